# Optimizing a Trainium2 kernel written in Bass

```python
import math
import jax, jax.numpy as jnp
from jax import lax
import numpy as np

D_MODEL = 2048
BATCH = 4
SEQ = 4096
DEPTH = 4

N_MIXERS = 4
ALPHA = (2.0 * DEPTH) ** 0.25
BETA = (8.0 * DEPTH) ** -0.25
LN_EPS = 1e-5

CONV_WIDTH = 31
S5_GROUP = 16
S5_GROUPS = D_MODEL // S5_GROUP
S5_STATE = 64
S5_DT_MIN = 1e-3
S5_DT_MAX = 1e-1
ML_HEADS = 8
ML_DV = D_MODEL // ML_HEADS
ML_DK = ML_DV // 2
ML_CHUNK = 64
ML_CONV = 4
GM_CHUNK = 128
GM_GROUPS = 8
GM_GROUP_DIM = D_MODEL // GM_GROUPS
D_FF = 5632
N_EXPERTS = 8
TOP_K = 2
D_FF_EXPERT = 5632
MOE_BLOCK = 256

N_A = (DEPTH + 3) // 4
N_B = (DEPTH + 2) // 4
N_C = (DEPTH + 1) // 4
N_D = DEPTH // 4
N_DENSE = (DEPTH + 1) // 2
N_MOE = DEPTH // 2

kernel_name = "hybrid_conv_s5_mlstm_gmlp_moe_deepnorm_adaln"


def layer_norm(x, g, b):
    xf = x.astype(jnp.float32)
    mu = jnp.mean(xf, -1, keepdims=True)
    var = jnp.mean(jnp.square(xf - mu), -1, keepdims=True)
    y = (xf - mu) * lax.rsqrt(var + LN_EPS) * g.astype(jnp.float32) + b.astype(jnp.float32)
    return y.astype(x.dtype)


def causal_depthwise_conv(x, w, b):
    k = w.shape[0]
    y = lax.conv_general_dilated(x, w[:, None, :].astype(x.dtype), window_strides=(1,),
                                 padding=[(k - 1, 0)], dimension_numbers=('NWC', 'WIO', 'NWC'),
                                 feature_group_count=x.shape[-1])
    return y + b


def conformer_conv(h, w_in, b_in, w_dw, b_dw, ln_g, ln_b, w_out, b_out):
    a, g = jnp.split(h @ w_in + b_in, 2, axis=-1)
    u = causal_depthwise_conv(a * jax.nn.sigmoid(g), w_dw, b_dw)
    u = jax.nn.silu(layer_norm(u, ln_g, ln_b))
    return u @ w_out + b_out


def _complex_affine_combine(e1, e2):
    a1r, a1i, b1r, b1i = e1
    a2r, a2i, b2r, b2i = e2
    ar = a2r * a1r - a2i * a1i
    ai = a2r * a1i + a2i * a1r
    br = a2r * b1r - a2i * b1i + b2r
    bi = a2r * b1i + a2i * b1r + b2i
    return ar, ai, br, bi


def s5_mixer(h, w_in, b_in, a_re, a_im, log_dt, b_re, b_im, c_re, c_im, d_skip, w_glu, b_glu):
    bsz, s, dm = h.shape
    G, P, N = S5_GROUPS, S5_GROUP, S5_STATE
    f32 = jnp.float32
    u = (h @ w_in + b_in).astype(f32).reshape(bsz, s, G, P)
    ar, ai = a_re.astype(f32), a_im.astype(f32)
    dt = jnp.exp(log_dt.astype(f32))[:, None]
    decay = jnp.exp(ar * dt)
    lr, li = decay * jnp.cos(ai * dt), decay * jnp.sin(ai * dt)
    den = ar * ar + ai * ai
    zr = ((lr - 1.0) * ar + li * ai) / den
    zi = (li * ar - (lr - 1.0) * ai) / den
    br, bi = b_re.astype(f32), b_im.astype(f32)
    bbr = zr[..., None] * br - zi[..., None] * bi
    bbi = zr[..., None] * bi + zi[..., None] * br
    xr = jnp.einsum('bsgp,gnp->bsgn', u, bbr)
    xi = jnp.einsum('bsgp,gnp->bsgn', u, bbi)
    lam_r = jnp.broadcast_to(lr, (1, s, G, N))
    lam_i = jnp.broadcast_to(li, (1, s, G, N))
    _, _, sr, si = lax.associative_scan(_complex_affine_combine, (lam_r, lam_i, xr, xi), axis=1)
    y = (jnp.einsum('bsgn,gpn->bsgp', sr, c_re.astype(f32))
         - jnp.einsum('bsgn,gpn->bsgp', si, c_im.astype(f32))
         + d_skip.astype(f32).reshape(G, P) * u)
    y = jax.nn.gelu(y.reshape(bsz, s, dm)).astype(h.dtype)
    a, g = jnp.split(y @ w_glu + b_glu, 2, axis=-1)
    return a * jax.nn.sigmoid(g)


def mlstm_mixer(h, w_in, b_in, w_conv, b_conv, mh_g, w_out, b_out):
    bsz, s, dm = h.shape
    H, DK, DV, L = ML_HEADS, ML_DK, ML_DV, ML_CHUNK
    nc = s // L
    f32 = jnp.float32
    z = h @ w_in + b_in
    split_at = np.cumsum([2 * H * DK, H * DV, H * DV, H]).tolist()
    qk, v, o, gi, gf = jnp.split(z, split_at, axis=-1)
    qk = jax.nn.silu(causal_depthwise_conv(qk, w_conv, b_conv))
    q, k = jnp.split(qk.astype(f32), 2, axis=-1)

    def to_chunks(t, d):
        return t.reshape(bsz, nc, L, H, d).transpose(1, 0, 3, 2, 4)

    def gate_chunks(t):
        return t.reshape(bsz, nc, L, H).transpose(1, 0, 3, 2)

    qc = to_chunks(q, DK)
    kc = to_chunks(k * (DK ** -0.5), DK)
    vc = to_chunks(v.astype(f32), DV)
    lic = gate_chunks(gi.astype(f32))
    lfc = gate_chunks(jax.nn.log_sigmoid(gf.astype(f32)))
    causal = jnp.tril(jnp.ones((L, L), bool))

    def step(carry, xs):
        C, n, m = carry
        qb, kb, vb, li_, lf_ = xs
        b = jnp.cumsum(lf_, axis=-1)
        dmat = jnp.where(causal, b[..., :, None] - b[..., None, :] + li_[..., None, :], -jnp.inf)
        inter = b + m[..., None]
        m_j = jnp.maximum(inter, jnp.max(dmat, axis=-1))
        w_intra = jnp.exp(dmat - m_j[..., None])
        w_inter = jnp.exp(inter - m_j)
        sc = jnp.einsum('bhjd,bhsd->bhjs', qb, kb) * w_intra
        num = w_inter[..., None] * jnp.einsum('bhjd,bhde->bhje', qb, C) + jnp.einsum('bhjs,bhse->bhje', sc, vb)
        den = w_inter * jnp.einsum('bhjd,bhd->bhj', qb, n) + jnp.sum(sc, axis=-1)
        hb = num / jnp.maximum(jnp.abs(den), jnp.exp(-m_j))[..., None]
        bl = b[..., -1]
        gsum = bl[..., None] - b + li_
        m_new = jnp.maximum(bl + m, jnp.max(gsum, axis=-1))
        wc = jnp.exp(bl + m - m_new)
        wk = jnp.exp(gsum - m_new[..., None])
        C_new = wc[..., None, None] * C + jnp.einsum('bhs,bhsd,bhse->bhde', wk, kb, vb)
        n_new = wc[..., None] * n + jnp.einsum('bhs,bhsd->bhd', wk, kb)
        return (C_new, n_new, m_new), hb

    init = (jnp.zeros((bsz, H, DK, DV), f32), jnp.zeros((bsz, H, DK), f32), jnp.zeros((bsz, H), f32))
    _, hs = lax.scan(step, init, (qc, kc, vc, lic, lfc))
    hs = hs.transpose(1, 0, 3, 2, 4).reshape(bsz, s, H, DV)
    hs = jax.nn.sigmoid(o.astype(f32)).reshape(bsz, s, H, DV) * hs
    mu = jnp.mean(hs, -1, keepdims=True)
    var = jnp.mean(jnp.square(hs - mu), -1, keepdims=True)
    hs = (hs - mu) * lax.rsqrt(var + LN_EPS) * mh_g.astype(f32).reshape(H, DV)
    return hs.reshape(bsz, s, dm).astype(h.dtype) @ w_out + b_out


def gmlp_mixer(h, w_in, b_in, ln_g, ln_b, w_sp, b_sp, w_out, b_out):
    bsz, s, dm = h.shape
    L, G, Dg = GM_CHUNK, GM_GROUPS, GM_GROUP_DIM
    zz = jax.nn.gelu(h @ w_in + b_in)
    u, v = jnp.split(zz, 2, axis=-1)
    v = layer_norm(v, ln_g, ln_b).reshape(bsz, s // L, L, G, Dg)
    w = w_sp * jnp.tril(jnp.ones((L, L), w_sp.dtype))
    sv = jnp.einsum('gts,bcsgd->bctgd', w, v) + b_sp.T[:, :, None]
    return (u * sv.reshape(bsz, s, dm)) @ w_out + b_out


def swiglu(h, w1, w3, w2):
    return (jax.nn.silu(h @ w1) * (h @ w3)) @ w2


def moe_swiglu(h, w_router, w1, w3, w2):
    bsz, s, dm = h.shape
    xt = h.reshape(-1, dm)
    n_tok = xt.shape[0]
    logits = (xt @ w_router).astype(jnp.float32)
    top_v, top_e = lax.top_k(logits, TOP_K)
    gates = jax.nn.softmax(top_v, axis=-1)
    e_flat = top_e.reshape(-1).astype(jnp.int32)
    tok_flat = jnp.repeat(jnp.arange(n_tok, dtype=jnp.int32), TOP_K)
    g_flat = gates.reshape(-1)
    order = jnp.argsort(e_flat)
    e_s, tok_s, g_s = e_flat[order], tok_flat[order], g_flat[order]
    counts = jnp.bincount(e_flat, length=N_EXPERTS).astype(jnp.int32)
    start = jnp.cumsum(counts) - counts
    padded = (counts + MOE_BLOCK - 1) // MOE_BLOCK * MOE_BLOCK
    pend = jnp.cumsum(padded)
    pstart = pend - padded
    dest = pstart[e_s] + (jnp.arange(n_tok * TOP_K, dtype=jnp.int32) - start[e_s])
    n_rows = n_tok * TOP_K + N_EXPERTS * MOE_BLOCK
    n_blocks = n_rows // MOE_BLOCK
    row_tok = jnp.full((n_rows,), n_tok, jnp.int32).at[dest].set(tok_s)
    row_gate = jnp.zeros((n_rows,), jnp.float32).at[dest].set(g_s)
    block_e = jnp.minimum(jnp.searchsorted(pend, jnp.arange(n_blocks, dtype=jnp.int32) * MOE_BLOCK,
                                           side='right'), N_EXPERTS - 1)
    x_pad = jnp.concatenate([xt, jnp.zeros((1, dm), xt.dtype)], axis=0)
    xb = x_pad[row_tok].reshape(n_blocks, MOE_BLOCK, dm)

    def expert_block(args):
        xblk, e = args
        return (jax.nn.silu(xblk @ w1[e]) * (xblk @ w3[e])) @ w2[e]

    yb = lax.map(expert_block, (xb, block_e))
    y_rows = yb.reshape(n_rows, dm) * row_gate[:, None].astype(xt.dtype)
    out = jnp.zeros((n_tok + 1, dm), xt.dtype).at[row_tok].add(y_rows)[:n_tok]
    return out.reshape(bsz, s, dm)


def setup_inputs(seed: int = 0) -> dict:
    key = jax.random.key(seed)
    ks = iter(jax.random.split(key, 96))
    f32 = jnp.float32
    D = D_MODEL

    def nrm(shape, scale):
        return jax.random.normal(next(ks), shape, f32) * scale

    x = nrm((BATCH, SEQ, D), 1.0)
    c = nrm((BATCH, D), 1.0)
    ada_w = nrm((DEPTH, D, 6 * D), 0.5 * D ** -0.5)
    ada_b = nrm((DEPTH, 6 * D), 0.02)
    ln1_g = 1.0 + nrm((DEPTH, D), 0.02)
    ln1_b = nrm((DEPTH, D), 0.02)
    ln2_g = 1.0 + nrm((DEPTH, D), 0.02)
    ln2_b = nrm((DEPTH, D), 0.02)
    a_w_in = nrm((N_A, D, 2 * D), D ** -0.5)
    a_b_in = nrm((N_A, 2 * D), 0.02)
    a_w_dw = nrm((N_A, CONV_WIDTH, D), CONV_WIDTH ** -0.5)
    a_b_dw = nrm((N_A, D), 0.02)
    a_ln_g = 1.0 + nrm((N_A, D), 0.02)
    a_ln_b = nrm((N_A, D), 0.02)
    a_w_out = nrm((N_A, D, D), BETA * D ** -0.5)
    a_b_out = nrm((N_A, D), 0.02)
    n_idx = jnp.arange(S5_STATE, dtype=f32)
    b_w_in = nrm((N_B, D, D), D ** -0.5)
    b_b_in = nrm((N_B, D), 0.02)
    b_a_re = -0.5 + nrm((N_B, S5_GROUPS, S5_STATE), 0.01)
    b_a_im = math.pi * n_idx + nrm((N_B, S5_GROUPS, S5_STATE), 0.01)
    b_log_dt = jax.random.uniform(next(ks), (N_B, S5_GROUPS), f32,
                                  minval=math.log(S5_DT_MIN), maxval=math.log(S5_DT_MAX))
    b_b_re = nrm((N_B, S5_GROUPS, S5_STATE, S5_GROUP), (2 * S5_GROUP) ** -0.5)
    b_b_im = nrm((N_B, S5_GROUPS, S5_STATE, S5_GROUP), (2 * S5_GROUP) ** -0.5)
    b_c_re = nrm((N_B, S5_GROUPS, S5_GROUP, S5_STATE), 0.5)
    b_c_im = nrm((N_B, S5_GROUPS, S5_GROUP, S5_STATE), 0.5)
    b_d = nrm((N_B, D), 1.0)
    b_w_glu = nrm((N_B, D, 2 * D), BETA * D ** -0.5)
    b_b_glu = nrm((N_B, 2 * D), 0.02)
    cw = 2 * ML_HEADS * ML_DK + 2 * ML_HEADS * ML_DV + 2 * ML_HEADS
    c_w_in = nrm((N_C, D, cw), D ** -0.5)
    c_b_in = jnp.concatenate([
        nrm((N_C, 2 * ML_HEADS * ML_DK + 2 * ML_HEADS * ML_DV), 0.02),
        nrm((N_C, ML_HEADS), 0.1),
        jnp.linspace(3.0, 6.0, ML_HEADS, dtype=f32)[None] + nrm((N_C, ML_HEADS), 0.1),
    ], axis=-1)
    c_w_conv = nrm((N_C, ML_CONV, 2 * ML_HEADS * ML_DK), ML_CONV ** -0.5)
    c_b_conv = nrm((N_C, 2 * ML_HEADS * ML_DK), 0.02)
    c_mh_g = 1.0 + nrm((N_C, D), 0.02)
    c_w_out = nrm((N_C, D, D), BETA * D ** -0.5)
    c_b_out = nrm((N_C, D), 0.02)
    d_w_in = nrm((N_D, D, 2 * D), D ** -0.5)
    d_b_in = nrm((N_D, 2 * D), 0.02)
    d_ln_g = 1.0 + nrm((N_D, D), 0.02)
    d_ln_b = nrm((N_D, D), 0.02)
    d_w_sp = nrm((N_D, GM_GROUPS, GM_CHUNK, GM_CHUNK), GM_CHUNK ** -0.5)
    d_b_sp = 1.0 + nrm((N_D, GM_GROUPS, GM_CHUNK), 0.02)
    d_w_out = nrm((N_D, D, D), BETA * D ** -0.5)
    d_b_out = nrm((N_D, D), 0.02)
    f_w1 = nrm((N_DENSE, D, D_FF), D ** -0.5)
    f_w3 = nrm((N_DENSE, D, D_FF), D ** -0.5)
    f_w2 = nrm((N_DENSE, D_FF, D), BETA * D_FF ** -0.5)
    m_router = nrm((N_MOE, D, N_EXPERTS), D ** -0.5)
    m_w1 = nrm((N_MOE, N_EXPERTS, D, D_FF_EXPERT), D ** -0.5)
    m_w3 = nrm((N_MOE, N_EXPERTS, D, D_FF_EXPERT), D ** -0.5)
    m_w2 = nrm((N_MOE, N_EXPERTS, D_FF_EXPERT, D), BETA * D_FF_EXPERT ** -0.5)
    return {
        "x": x, "c": c, "ada_w": ada_w, "ada_b": ada_b,
        "ln1_g": ln1_g, "ln1_b": ln1_b, "ln2_g": ln2_g, "ln2_b": ln2_b,
        "a_w_in": a_w_in, "a_b_in": a_b_in, "a_w_dw": a_w_dw, "a_b_dw": a_b_dw,
        "a_ln_g": a_ln_g, "a_ln_b": a_ln_b, "a_w_out": a_w_out, "a_b_out": a_b_out,
        "b_w_in": b_w_in, "b_b_in": b_b_in, "b_a_re": b_a_re, "b_a_im": b_a_im,
        "b_log_dt": b_log_dt, "b_b_re": b_b_re, "b_b_im": b_b_im, "b_c_re": b_c_re,
        "b_c_im": b_c_im, "b_d": b_d, "b_w_glu": b_w_glu, "b_b_glu": b_b_glu,
        "c_w_in": c_w_in, "c_b_in": c_b_in, "c_w_conv": c_w_conv, "c_b_conv": c_b_conv,
        "c_mh_g": c_mh_g, "c_w_out": c_w_out, "c_b_out": c_b_out,
        "d_w_in": d_w_in, "d_b_in": d_b_in, "d_ln_g": d_ln_g, "d_ln_b": d_ln_b,
        "d_w_sp": d_w_sp, "d_b_sp": d_b_sp, "d_w_out": d_w_out, "d_b_out": d_b_out,
        "f_w1": f_w1, "f_w3": f_w3, "f_w2": f_w2,
        "m_router": m_router, "m_w1": m_w1, "m_w3": m_w3, "m_w2": m_w2,
    }


def reference(x, c, ada_w, ada_b, ln1_g, ln1_b, ln2_g, ln2_b,
              a_w_in, a_b_in, a_w_dw, a_b_dw, a_ln_g, a_ln_b, a_w_out, a_b_out,
              b_w_in, b_b_in, b_a_re, b_a_im, b_log_dt, b_b_re, b_b_im, b_c_re,
              b_c_im, b_d, b_w_glu, b_b_glu,
              c_w_in, c_b_in, c_w_conv, c_b_conv, c_mh_g, c_w_out, c_b_out,
              d_w_in, d_b_in, d_ln_g, d_ln_b, d_w_sp, d_b_sp, d_w_out, d_b_out,
              f_w1, f_w3, f_w2,
              m_router, m_w1, m_w3, m_w2):
    cond = jax.nn.silu(c)
    ia = ib = ic = idd = 0
    i_dense = i_moe = 0
    for layer in range(DEPTH):
        mod = (cond @ ada_w[layer] + ada_b[layer])[:, None, :]
        sh1, sc1, g1, sh2, sc2, g2 = jnp.split(mod, 6, axis=-1)
        hm = x * (1.0 + sc1) + sh1
        kind = layer % N_MIXERS
        if kind == 0:
            y = conformer_conv(hm, a_w_in[ia], a_b_in[ia], a_w_dw[ia], a_b_dw[ia],
                               a_ln_g[ia], a_ln_b[ia], a_w_out[ia], a_b_out[ia])
            ia += 1
        elif kind == 1:
            y = s5_mixer(hm, b_w_in[ib], b_b_in[ib], b_a_re[ib], b_a_im[ib], b_log_dt[ib],
                         b_b_re[ib], b_b_im[ib], b_c_re[ib], b_c_im[ib], b_d[ib],
                         b_w_glu[ib], b_b_glu[ib])
            ib += 1
        elif kind == 2:
            y = mlstm_mixer(hm, c_w_in[ic], c_b_in[ic], c_w_conv[ic], c_b_conv[ic],
                            c_mh_g[ic], c_w_out[ic], c_b_out[ic])
            ic += 1
        else:
            y = gmlp_mixer(hm, d_w_in[idd], d_b_in[idd], d_ln_g[idd], d_ln_b[idd],
                           d_w_sp[idd], d_b_sp[idd], d_w_out[idd], d_b_out[idd])
            idd += 1
        x = layer_norm(ALPHA * x + g1 * y, ln1_g[layer], ln1_b[layer])
        hf = x * (1.0 + sc2) + sh2
        if layer % 2 == 0:
            y = swiglu(hf, f_w1[i_dense], f_w3[i_dense], f_w2[i_dense])
            i_dense += 1
        else:
            y = moe_swiglu(hf, m_router[i_moe], m_w1[i_moe], m_w3[i_moe], m_w2[i_moe])
            i_moe += 1
        x = layer_norm(ALPHA * x + g2 * y, ln2_g[layer], ln2_b[layer])
    return x
```

```python
import math
from contextlib import ExitStack
import numpy as np
import concourse.bass as bass
import concourse.mybir as mybir
from concourse.bass_utils import run_bass_kernel_spmd

AF = mybir.ActivationFunctionType
ALU = mybir.AluOpType
F32 = mybir.dt.float32
BF16 = mybir.dt.bfloat16

D = 2048
KC = 16
SEQ = 4096
NCORE = 8
TC = 2048
NT = 512
DFF = 5632
MC_FF = 44
ALPHA = 8.0 ** 0.25
LN_EPS = 1e-5
HALO = 32
ENGS = ["pe", "act", "dve", "pool", "sp"]


class Tl:
    __slots__ = ("ap", "name", "w", "rs", "rd", "dsem", "dcnt", "al")

    def __init__(self, ap, name):
        self.ap = ap
        self.name = name
        self.w = None
        self.rs = {}
        self.rd = []
        self.dsem = None
        self.dcnt = 0
        self.al = ()

    def __getitem__(self, idx):
        return self.ap[idx]


class Op:
    __slots__ = ("eng", "emit", "waits", "idx", "mark", "cnt", "is_dma", "dsem", "dval", "dtile", "inc")


class Prog:
    def __init__(self, nc, es):
        self.nc = nc
        self.es = es
        self.streams = {e: [] for e in ENGS}
        self.esem = {e: es.enter_context(nc.semaphore("S_" + e)) for e in ENGS}
        self.nsem = 5
        self.uid = 0
        self.wt = {}

    def sb(self, shape, dt, name):
        t = self.es.enter_context(self.nc.sbuf_tensor(name, list(shape), dt))
        return Tl(t, name)

    def ps(self, shape, dt, name):
        t = self.es.enter_context(self.nc.psum_tensor(name, list(shape), dt))
        return Tl(t, name)

    def view(self, ap, name):
        return Tl(ap, name)

    def _touch(self, tiles):
        out = []
        for t in tiles:
            out.append(t)
            out.extend(t.al)
        return out

    def add(self, eng, emit, reads=(), writes=(), dma_tile=None, inc=16):
        op = Op()
        op.inc = inc
        op.eng = eng
        op.emit = emit
        op.idx = len(self.streams[eng])
        op.mark = False
        op.cnt = 0
        op.is_dma = dma_tile is not None
        op.dtile = dma_tile
        op.dsem = None
        op.dval = 0
        reads = self._touch(reads)
        writes = self._touch(writes)
        deps = {}
        ddeps = []

        def dep(d):
            if d is None or d is op:
                return
            if d.is_dma:
                if op.is_dma and d.dtile is dma_tile:
                    return
                ddeps.append(d)
                return
            cur = deps.get(d.eng)
            if cur is None or cur.idx < d.idx:
                deps[d.eng] = d

        for t in reads:
            dep(t.w)
        for t in writes:
            dep(t.w)
            for r in t.rs.values():
                dep(r)
            for r in t.rd:
                dep(r)
        op.waits = []
        for d in ddeps:
            op.waits.append((d.dsem, d.dtile.dcnt))
        for e, d in deps.items():
            if e == eng and not op.is_dma:
                if eng == "pe":
                    continue
                if op.idx - d.idx >= 4:
                    continue
            d.mark = True
            op.waits.append(d)
        for t in reads:
            if op.is_dma:
                t.rd.append(op)
            else:
                t.rs[eng] = op
        for t in writes:
            t.w = op
            t.rs = {}
            t.rd = []
        if op.is_dma:
            if dma_tile.dsem is None:
                dma_tile.dsem = self.es.enter_context(self.nc.semaphore("D%d" % self.nsem))
                self.nsem += 1
            dma_tile.dcnt += inc
            op.dsem = dma_tile.dsem
            op.dval = dma_tile.dcnt
        self.streams[eng].append(op)
        return op

    def dma(self, q, out_t, out_ap, in_t, in_ap):
        if in_t is None:
            try:
                in_t = self.wt.get(in_ap.name)
            except Exception:
                in_t = None
        reads = [in_t] if in_t is not None else []
        writes = [out_t] if out_t is not None else []
        return self.add(q, lambda e: e.dma_start(out=out_ap, in_=in_ap), reads, writes, dma_tile=out_t)

    def gather_into(self, name, rows, cols, slot):
        nc = self.nc
        rs = rows // NCORE
        ext = nc.dram_tensor(name, [rs, cols], F32, kind="ExternalInput").ap()
        if "full" not in slot:
            slot["bnc"] = nc.dram_tensor(slot["nm"] + "_b", [rs, cols], F32)
            slot["full"] = nc.dram_tensor(slot["nm"] + "_f", [rows, cols], F32)
            slot["tb"] = Tl(None, slot["nm"] + "_b")
            slot["tf"] = Tl(None, slot["nm"] + "_f")
            self.wt[slot["full"].ap().name] = slot["tf"]
        bnc, full, tb, tf = slot["bnc"], slot["full"], slot["tb"], slot["tf"]
        step = max(1, (8 << 20) // (cols * 4))
        r = 0
        first = True
        while r < rs:
            r2 = min(rs, r + step)
            self.dma("pool", tb, bnc.ap()[r:r2, :], None, ext[r:r2, :])
            r = r2
        self.add("pool", lambda e: e.collective_compute("AllGather", ALU.bypass, replica_groups=[list(range(NCORE))],
                                                        ins=[bnc.ap()], outs=[full.ap()]),
                 [tb], [tf], dma_tile=tf, inc=1)
        return full.ap()

    def gathered(self, name, rows, cols):
        return self.nc.dram_tensor(name, [rows, cols], F32, kind="ExternalInput").ap()

    def gathered_cc(self, name, rows, cols):
        nc = self.nc
        rs = rows // NCORE
        ext = nc.dram_tensor(name, [rs, cols], F32, kind="ExternalInput").ap()
        bnc = nc.dram_tensor(name + "_b", [rs, cols], F32)
        full = nc.dram_tensor(name + "_f", [rows, cols], F32)
        tb = Tl(None, name + "_b")
        tf = Tl(None, name + "_f")
        step = max(1, (8 << 20) // (cols * 4))
        r = 0
        while r < rs:
            r2 = min(rs, r + step)
            self.dma("pool", tb, bnc.ap()[r:r2, :], None, ext[r:r2, :])
            r = r2
        self.add("pool", lambda e: e.collective_compute("AllGather", ALU.bypass, replica_groups=[list(range(NCORE))],
                                                        ins=[bnc.ap()], outs=[full.ap()]),
                 [tb], [tf], dma_tile=tf, inc=1)
        self.wt[full.ap().name] = tf
        return full.ap()

    def finish(self, tiles):
        self.add("sp", lambda e: e.nop(), reads=list(tiles), writes=[])

    def emit_all(self):
        nc = self.nc
        for e in ENGS:
            c = 0
            for op in self.streams[e]:
                if op.mark:
                    c += 1
                    op.cnt = c
        esem = self.esem
        streams = self.streams

        def run(eng_obj, e):
            waited = {}
            for op in streams[e]:
                for d in op.waits:
                    if isinstance(d, tuple):
                        sem, val = d
                    else:
                        sem, val = esem[d.eng], d.cnt
                    k = id(sem)
                    if waited.get(k, 0) >= val:
                        continue
                    eng_obj.wait_ge(sem, val)
                    waited[k] = val
                ins = op.emit(eng_obj)
                if op.is_dma:
                    ins.then_inc(op.dsem, op.inc)
                elif op.mark:
                    ins.then_inc(esem[e], 1)

        with nc.Block() as block:
            @block.tensor
            def _(x):
                run(x, "pe")

            @block.scalar
            def _(x):
                run(x, "act")

            @block.vector
            def _(x):
                run(x, "dve")

            @block.gpsimd
            def _(x):
                run(x, "pool")

            @block.sync
            def _(x):
                run(x, "sp")


class Ctx:
    def __init__(self, P, nslab=3, slab_elems=11264, ntmp=3):
        self.P = P
        self.slabs = [P.sb([128, slab_elems], BF16, "slab%d" % i) for i in range(nslab)]
        self.slab_i = 0
        self.psum = [P.ps([128, 512], F32, "ps%d" % i) for i in range(6)]
        self.ps_i = 0
        self.pstat = [P.ps([128, 512], F32, "pst%d" % i) for i in range(2)]
        self.ones = P.sb([128, 128], F32, "ones")
        P.add("dve", lambda e: e.memset(self.ones.ap[:], 1.0), [], [self.ones])
        self.tmp = [P.sb([128, 512], F32, "tmp%d" % i) for i in range(ntmp)]
        self.tmp_i = 0
        self.stat = [P.sb([128, 512], F32, "stat%d" % i) for i in range(3)]
        self.ev = 0

    def next_slab(self):
        s = self.slabs[self.slab_i % len(self.slabs)]
        self.slab_i += 1
        return s

    def next_ps(self):
        p = self.psum[self.ps_i % len(self.psum)]
        self.ps_i += 1
        return p

    def next_tmp(self):
        t = self.tmp[self.tmp_i % len(self.tmp)]
        self.tmp_i += 1
        return t


def linear(C, w_ap, K, col0, ncols, slabw, rhs_groups, epilogue, m_order=None):
    P = C.P
    kc = K // 128
    wv = w_ap.rearrange("(k p) n -> p k n", p=128)
    nslab = ncols // slabw
    for s in range(nslab):
        slab = C.next_slab()
        c0 = col0 + s * slabw
        sv = slab.ap[:, 0:kc * slabw].rearrange("p (k n) -> p k n", k=kc)
        P.dma("pool", slab, sv, None, wv[:, :, c0:c0 + slabw])
        for j in range(slabw // 128):
            mi = (c0 // 128) + j
            for gi, (rt, rf) in enumerate(rhs_groups):
                ps = C.next_ps()
                for k in range(kc):
                    lhs = sv[:, k, j * 128:(j + 1) * 128]
                    rhs = rf(k)
                    nn = rhs.shape[-1]
                    P.add("pe", (lambda e, o=ps.ap[:, 0:nn], l=lhs, r=rhs, st=(k == 0), sp=(k == kc - 1):
                                 e.matmul(o, l, r, start=st, stop=sp)),
                          [slab, rt[k]], [ps])
                epilogue(mi, gi, ps)


def ln_stats(C, ztiles, n, width_sel=None):
    P = C.P
    ps1, ps2 = C.pstat
    nk = len(ztiles)
    for k in range(nk):
        zt, za = ztiles[k]
        P.add("pe", (lambda e, o=ps1.ap[:, 0:n], r=za, st=(k == 0), sp=(k == nk - 1):
                     e.matmul(o, C.ones.ap[:], r, start=st, stop=sp)), [C.ones, zt], [ps1])
    for k in range(nk):
        zt, za = ztiles[k]
        sq = C.next_tmp()
        P.add("act", (lambda e, o=sq.ap[:, 0:n], i=za: e.activation(o, i, AF.Square)), [zt], [sq])
        P.add("pe", (lambda e, o=ps2.ap[:, 0:n], r=sq.ap[:, 0:n], st=(k == 0), sp=(k == nk - 1):
                     e.matmul(o, C.ones.ap[:], r, start=st, stop=sp)), [C.ones, sq], [ps2])
    mean, ex2, rstd = C.stat
    invd = 1.0 / (128.0 * nk)
    P.add("act", lambda e: e.activation(mean.ap[:, 0:n], ps1.ap[:, 0:n], AF.Copy, scale=invd), [ps1], [mean])
    P.add("act", lambda e: e.activation(ex2.ap[:, 0:n], ps2.ap[:, 0:n], AF.Copy, scale=invd), [ps2], [ex2])
    P.add("dve", lambda e: e.tensor_tensor(rstd.ap[:, 0:n], mean.ap[:, 0:n], mean.ap[:, 0:n], ALU.mult), [mean], [rstd])
    P.add("dve", lambda e: e.tensor_tensor(ex2.ap[:, 0:n], ex2.ap[:, 0:n], rstd.ap[:, 0:n], ALU.subtract), [ex2, rstd], [ex2])
    P.add("dve", lambda e: e.tensor_scalar(ex2.ap[:, 0:n], ex2.ap[:, 0:n], LN_EPS, None, ALU.add), [ex2], [ex2])
    P.add("act", lambda e: e.activation(ex2.ap[:, 0:n], ex2.ap[:, 0:n], AF.Sqrt), [ex2], [ex2])
    P.add("dve", lambda e: e.reciprocal(rstd.ap[:, 0:n], ex2.ap[:, 0:n]), [ex2], [rstd])
    return mean, rstd


def ln_apply(C, zt, za, outs, n, mean, rstd, gcol, bcol, func=AF.Identity, extra=None):
    P = C.P
    P.add("dve", lambda e: e.tensor_tensor(za, za, mean.ap[:, 0:n], ALU.subtract), [zt, mean], [zt])
    P.add("dve", lambda e: e.tensor_tensor(za, za, rstd.ap[:, 0:n], ALU.mult), [zt, rstd], [zt])
    ot, oa = outs
    P.add("act", lambda e: e.activation(oa, za, func, bias=bcol, scale=gcol), [zt], [ot])
    if extra is not None:
        xt, xa, sc, bc = extra
        P.add("act", lambda e: e.activation(xa, oa, AF.Identity, bias=bc, scale=sc), [ot], [xt])


def compute_mod(C, cvec_ap, ada_w_ap, ada_b_ap, which, modc):
    P = C.P
    craw = P.sb([128, KC], F32, "craw")
    cb = P.sb([128, KC], BF16, "cbf")
    P.dma("sp", craw, craw.ap[:], None, cvec_ap)
    P.add("act", lambda e: e.activation(cb.ap[:], craw.ap[:], AF.Silu), [craw], [cb])
    bcols = P.sb([128, 6 * KC], F32, "adab_cols")
    P.dma("sp", bcols, bcols.ap[:], None, ada_b_ap)
    one11 = P.sb([1, 1], F32, "one11")
    P.add("dve", lambda e: e.memset(one11.ap[:], 1.0), [], [one11])
    wv = ada_w_ap.rearrange("(k p) n -> p k n", p=128)
    i = 0
    for v in which:
        for s in range(4):
            slab = C.next_slab()
            c0 = v * D + s * 512
            sv = slab.ap[:, 0:KC * 512].rearrange("p (k n) -> p k n", k=KC)
            P.dma("pool", slab, sv, None, wv[:, :, c0:c0 + 512])
            ps = C.next_ps()
            for k in range(KC):
                P.add("pe", (lambda e, o=ps.ap[0:1, :], l=cb.ap[:, k:k + 1], r=sv[:, k, :], st=(k == 0), sp=(k == KC - 1):
                             e.matmul(o, l, r, start=st, stop=sp)), [slab, cb], [ps])
            mr = C.next_tmp()
            P.add("dve", (lambda e, o=mr.ap[0:1, :], a=ps.ap[0:1, :]: e.tensor_copy(o, a)), [ps], [mr])
            ps2 = C.next_ps()
            for k in range(4):
                P.add("pe", (lambda e, o=ps2.ap[:, k:k + 1], l=mr.ap[0:1, k * 128:(k + 1) * 128]:
                             e.matmul(o, l, one11.ap[0:1, 0:1], start=True, stop=True)), [mr, one11], [ps2])
            cc = v * KC + s * 4
            P.add("dve", (lambda e, o=modc.ap[:, cc:cc + 4], i_=ps2.ap[:, 0:4], b=bcols.ap[:, cc:cc + 4]:
                          e.tensor_tensor(o, i_, b, ALU.add)), [ps2, bcols], [modc])


def load_cols(P, name, ap_1d, n):
    t = P.sb([128, n], F32, name)
    P.dma("sp", t, t.ap[:], None, ap_1d)
    return t


def ffn_phase(C, HB, X, w1_ap, w3_ap, w2_ap, G, g2col, n, gate=None, ACC=None, last=True):
    P = C.P
    SW = 256
    for s in range(DFF // SW):
        sl1 = C.next_slab()
        sl3 = C.next_slab()
        c0 = s * SW
        v1 = sl1.ap[:, 0:KC * SW].rearrange("p (k n) -> p k n", k=KC)
        v3 = sl3.ap[:, 0:KC * SW].rearrange("p (k n) -> p k n", k=KC)
        P.dma("pool", sl1, v1, None, w1_ap.rearrange("(k p) n -> p k n", p=128)[:, :, c0:c0 + SW])
        P.dma("pool", sl3, v3, None, w3_ap.rearrange("(k p) n -> p k n", p=128)[:, :, c0:c0 + SW])
        for j in range(SW // 128):
            m = c0 // 128 + j
            p1 = C.next_ps()
            p3 = C.next_ps()
            for k in range(KC):
                P.add("pe", (lambda e, o=p1.ap[:, 0:n], l=v1[:, k, j * 128:(j + 1) * 128], r=HB[k].ap[:, 0:n], st=(k == 0), sp=(k == KC - 1):
                             e.matmul(o, l, r, start=st, stop=sp)), [sl1, HB[k]], [p1])
            for k in range(KC):
                P.add("pe", (lambda e, o=p3.ap[:, 0:n], l=v3[:, k, j * 128:(j + 1) * 128], r=HB[k].ap[:, 0:n], st=(k == 0), sp=(k == KC - 1):
                             e.matmul(o, l, r, start=st, stop=sp)), [sl3, HB[k]], [p3])
            t = C.next_tmp()
            P.add("act", (lambda e, o=t.ap[:, 0:n], i=p1.ap[:, 0:n]: e.activation(o, i, AF.Silu)), [p1], [t])
            if gate is None:
                P.add("dve", (lambda e, o=G[m].ap[:, 0:n], a=t.ap[:, 0:n], b=p3.ap[:, 0:n]: e.tensor_tensor(o, a, b, ALU.mult)), [t, p3], [G[m]])
            else:
                P.add("dve", (lambda e, o=t.ap[:, 0:n], a=t.ap[:, 0:n], b=p3.ap[:, 0:n]: e.tensor_tensor(o, a, b, ALU.mult)), [t, p3], [t])
                P.add("dve", (lambda e, o=G[m].ap[:, 0:n], a=t.ap[:, 0:n], b=gate.ap[:, 0:n]: e.tensor_tensor(o, a, b, ALU.mult)), [t, gate], [G[m]])
    SW2 = 256
    for s in range(D // SW2):
        sl = C.next_slab()
        c0 = s * SW2
        v2 = sl.ap[:, 0:MC_FF * SW2].rearrange("p (k n) -> p k n", k=MC_FF)
        P.dma("pool", sl, v2, None, w2_ap.rearrange("(k p) n -> p k n", p=128)[:, :, c0:c0 + SW2])
        for j in range(SW2 // 128):
            c = c0 // 128 + j
            ps = C.next_ps()
            for m in range(MC_FF):
                P.add("pe", (lambda e, o=ps.ap[:, 0:n], l=v2[:, m, j * 128:(j + 1) * 128], r=G[m].ap[:, 0:n], st=(m == 0), sp=(m == MC_FF - 1):
                             e.matmul(o, l, r, start=st, stop=sp)), [sl, G[m]], [ps])
            if gate is None:
                t = C.next_tmp()
                P.add("act", (lambda e, o=t.ap[:, 0:n], i=ps.ap[:, 0:n], c=c: e.activation(o, i, AF.Copy, scale=g2col.ap[:, c:c + 1])), [ps, g2col], [t])
                P.add("dve", (lambda e, o=X[c].ap[:, 0:n], a=X[c].ap[:, 0:n], b=t.ap[:, 0:n]:
                              e.scalar_tensor_tensor(o, a, ALPHA, b, ALU.mult, ALU.add)), [X[c], t], [X[c]])
            else:
                P.add("dve", (lambda e, o=X[c].ap[:, 0:n], p_=ps.ap[:, 0:n], c=c:
                              e.scalar_tensor_tensor(o, p_, g2col.ap[:, c:c + 1], o, ALU.mult, ALU.add)), [X[c], ps, g2col], [X[c]])


def ln_inplace(C, X, n, gcol, bcol, HB=None, sccol=None, shcol=None):
    mean, rstd = ln_stats(C, [(X[c], X[c].ap[:, 0:n]) for c in range(KC)], n)
    for c in range(KC):
        extra = None
        if HB is not None:
            extra = (HB[c], HB[c].ap[:, 0:n], sccol.ap[:, c:c + 1], shcol.ap[:, c:c + 1])
        ln_apply(C, X[c], X[c].ap[:, 0:n], (X[c], X[c].ap[:, 0:n]), n, mean, rstd,
                 gcol.ap[:, c:c + 1], bcol.ap[:, c:c + 1], AF.Identity, extra)


def build_layer0():
    nc = bass.Bass("TRN2", target_bir_lowering=False)
    dr = lambda name, shape: nc.dram_tensor(name, list(shape), F32, kind="ExternalInput").ap()
    xT = dr("xT", [D, HALO + TC])
    modc_d = dr("modc", [128, 6 * KC])
    hmask = dr("hmask", [128, 1])
    CV = [128, KC]
    ln1_g, ln1_b, ln2_g, ln2_b = dr("ln1_g", CV), dr("ln1_b", CV), dr("ln2_g", CV), dr("ln2_b", CV)
    b_in = dr("a_b_in", [128, 2 * KC])
    w_dw, b_dw = dr("a_w_dw", [128, KC, 31]), dr("a_b_dw", CV)
    aln_g, aln_b = dr("a_ln_g", CV), dr("a_ln_b", CV)
    b_out = dr("a_b_out", CV)
    outT = nc.dram_tensor("outT", [D, TC], F32, kind="ExternalOutput").ap()
    with ExitStack() as es:
        P = Prog(nc, es)
        w_in = P.gathered("a_w_in", D, 2 * D)
        w_out = P.gathered("a_w_out", D, D)
        w1, w3, w2 = P.gathered("f_w1", D, DFF), P.gathered("f_w3", D, DFF), P.gathered("f_w2", DFF, D)
        C = Ctx(P)
        sh1, sc1, g1, sh2, sc2, g2 = setup_common(P, C, modc_d)
        sc1p = P.sb([128, KC], F32, "sc1p")
        sc2p = P.sb([128, KC], F32, "sc2p")
        P.add("dve", lambda e: e.tensor_scalar(sc1p.ap[:], sc1.ap[:], 1.0, None, ALU.add), [sc1], [sc1p])
        P.add("dve", lambda e: e.tensor_scalar(sc2p.ap[:], sc2.ap[:], 1.0, None, ALU.add), [sc2], [sc2p])
        c_ln1g, c_ln1b = load_cols(P, "c_ln1g", ln1_g, KC), load_cols(P, "c_ln1b", ln1_b, KC)
        c_ln2g, c_ln2b = load_cols(P, "c_ln2g", ln2_g, KC), load_cols(P, "c_ln2b", ln2_b, KC)
        c_bin = load_cols(P, "c_bin", b_in, 2 * KC)
        c_bdw = load_cols(P, "c_bdw", b_dw, KC)
        c_alng, c_alnb = load_cols(P, "c_alng", aln_g, KC), load_cols(P, "c_alnb", aln_b, KC)
        c_bout = load_cols(P, "c_bout", b_out, KC)
        c_wdw = P.sb([128, KC, 31], F32, "c_wdw")
        P.dma("sp", c_wdw, c_wdw.ap[:], None, w_dw)
        c_mask = P.sb([128, 1], F32, "c_mask")
        P.dma("sp", c_mask, c_mask.ap[:], None, hmask)
        g1b = P.sb([128, KC], F32, "g1b")
        P.add("dve", lambda e: e.tensor_tensor(g1b.ap[:], g1.ap[:], c_bout.ap[:], ALU.mult), [g1, c_bout], [g1b])

        X = [P.sb([128, NT], F32, "X%d" % k) for k in range(KC)]
        XH = [P.sb([128, HALO], F32, "XH%d" % k) for k in range(KC)]
        HB = [P.sb([128, NT], BF16, "HB%d" % k) for k in range(KC)]
        HBH = [P.sb([128, HALO], BF16, "HBH%d" % k) for k in range(KC)]
        arena = P.sb([128, 16 * (HALO + NT) + 16 * NT], F32, "arena")
        Pp, U = [], []
        for k in range(KC):
            t = Tl(arena.ap[:, k * (HALO + NT):(k + 1) * (HALO + NT)], "Pp%d" % k)
            Pp.append(t)
        off = KC * (HALO + NT)
        for k in range(KC):
            t = Tl(arena.ap[:, off + k * NT:off + (k + 1) * NT], "U%d" % k)
            U.append(t)
        gview = arena.ap[:, 0:MC_FF * NT // 2].bitcast(BF16)
        G = []
        for m in range(MC_FF):
            t = Tl(gview[:, m * NT:(m + 1) * NT], "G%d" % m)
            G.append(t)
        spans = [(Pp[k], k * (HALO + NT), (k + 1) * (HALO + NT)) for k in range(KC)] + \
                [(U[k], off + k * NT, off + (k + 1) * NT) for k in range(KC)]
        for m in range(MC_FF):
            lo, hi = m * NT // 2, (m + 1) * NT // 2
            al = [t for (t, a, b) in spans if a < hi and lo < b]
            G[m].al = tuple(al)
            for t in al:
                t.al = tuple(list(t.al) + [G[m]])
        HALOS = [P.sb([128, HALO], F32, "HL%d" % k) for k in range(KC)]

        ntile = TC // NT
        for it in range(ntile):
            t0 = HALO + it * NT
            for k in range(KC):
                P.dma("sp", X[k], X[k].ap[:], None, xT[k * 128:(k + 1) * 128, t0:t0 + NT])
                P.add("act", (lambda e, o=HB[k].ap[:], i=X[k].ap[:], k=k:
                              e.activation(o, i, AF.Identity, bias=sh1.ap[:, k:k + 1], scale=sc1p.ap[:, k:k + 1])), [X[k], sh1, sc1p], [HB[k]])
            groups = [(HB, lambda k: HB[k].ap[:])]
            if it == 0:
                for k in range(KC):
                    P.dma("sp", XH[k], XH[k].ap[:], None, xT[k * 128:(k + 1) * 128, 0:HALO])
                    P.add("act", (lambda e, o=HBH[k].ap[:], i=XH[k].ap[:], k=k:
                                  e.activation(o, i, AF.Identity, bias=sh1.ap[:, k:k + 1], scale=sc1p.ap[:, k:k + 1])), [XH[k], sh1, sc1p], [HBH[k]])
                groups.append((HBH, lambda k: HBH[k].ap[:]))
            else:
                for k in range(KC):
                    P.add("pool", (lambda e, o=Pp[k].ap[:, 0:HALO], i=HALOS[k].ap[:]: e.tensor_copy(o, i)), [HALOS[k]], [Pp[k]])
            wv = w_in.rearrange("(k p) n -> p k n", p=128)
            for s in range(4):
                sa = C.next_slab()
                sg = C.next_slab()
                va = sa.ap[:, 0:KC * 512].rearrange("p (k n) -> p k n", k=KC)
                vg = sg.ap[:, 0:KC * 512].rearrange("p (k n) -> p k n", k=KC)
                P.dma("pool", sa, va, None, wv[:, :, s * 512:(s + 1) * 512])
                P.dma("pool", sg, vg, None, wv[:, :, D + s * 512:D + (s + 1) * 512])
                for j in range(4):
                    m = s * 4 + j
                    for gi, (rt, rf) in enumerate(groups):
                        nn = NT if gi == 0 else HALO
                        pa = C.next_ps()
                        pg = C.next_ps()
                        for k in range(KC):
                            P.add("pe", (lambda e, o=pa.ap[:, 0:nn], l=va[:, k, j * 128:(j + 1) * 128], r=rf(k), st=(k == 0), sp=(k == KC - 1):
                                         e.matmul(o, l, r, start=st, stop=sp)), [sa, rt[k]], [pa])
                        for k in range(KC):
                            P.add("pe", (lambda e, o=pg.ap[:, 0:nn], l=vg[:, k, j * 128:(j + 1) * 128], r=rf(k), st=(k == 0), sp=(k == KC - 1):
                                         e.matmul(o, l, r, start=st, stop=sp)), [sg, rt[k]], [pg])
                        t = C.next_tmp()
                        P.add("act", (lambda e, o=t.ap[:, 0:nn], i=pg.ap[:, 0:nn], m=m:
                                      e.activation(o, i, AF.Sigmoid, bias=c_bin.ap[:, KC + m:KC + m + 1])), [pg, c_bin], [t])
                        dst = Pp[m].ap[:, HALO:] if gi == 0 else Pp[m].ap[:, 0:HALO]
                        P.add("dve", (lambda e, o=dst, a=pa.ap[:, 0:nn], b=t.ap[:, 0:nn], m=m:
                                      e.scalar_tensor_tensor(o, a, c_bin.ap[:, m:m + 1], b, ALU.add, ALU.mult)), [pa, t, c_bin], [Pp[m]])
                        if gi == 1:
                            P.add("dve", (lambda e, o=dst: e.tensor_scalar(o, o, c_mask.ap[:, 0:1], None, ALU.mult)), [Pp[m], c_mask], [Pp[m]])
            for m in range(KC):
                eng = "dve"
                P.add(eng, (lambda e, o=U[m].ap[:], i=Pp[m].ap[:, 2:2 + NT], m=m:
                            e.tensor_scalar(o, i, c_wdw.ap[:, m, 0:1], c_bdw.ap[:, m:m + 1], ALU.mult, ALU.add)), [Pp[m], c_wdw, c_bdw], [U[m]])
                for j in range(1, 31):
                    P.add(eng, (lambda e, o=U[m].ap[:], i=Pp[m].ap[:, 2 + j:2 + j + NT], m=m, j=j:
                                e.scalar_tensor_tensor(o, i, c_wdw.ap[:, m, j:j + 1], o, ALU.mult, ALU.add)), [Pp[m], U[m], c_wdw], [U[m]])
                P.add("pool", (lambda e, o=HALOS[m].ap[:], i=Pp[m].ap[:, NT:NT + HALO]: e.tensor_copy(o, i)), [Pp[m]], [HALOS[m]])
            mean, rstd = ln_stats(C, [(U[m], U[m].ap[:]) for m in range(KC)], NT)
            for m in range(KC):
                ln_apply(C, U[m], U[m].ap[:], (HB[m], HB[m].ap[:]), NT, mean, rstd,
                         c_alng.ap[:, m:m + 1], c_alnb.ap[:, m:m + 1], AF.Silu)
            def epi_out(mi, gi, ps):
                t = C.next_tmp()
                P.add("act", (lambda e, o=t.ap[:], i=ps.ap[:]: e.activation(o, i, AF.Identity, bias=g1b.ap[:, mi:mi + 1], scale=g1.ap[:, mi:mi + 1])), [ps, g1b, g1], [t])
                P.add("dve", (lambda e, o=X[mi].ap[:], b=t.ap[:]: e.scalar_tensor_tensor(o, o, ALPHA, b, ALU.mult, ALU.add)), [X[mi], t], [X[mi]])
            linear(C, w_out, D, 0, D, 512, [(HB, lambda k: HB[k].ap[:])], epi_out)
            ln_inplace(C, X, NT, c_ln1g, c_ln1b, HB, sc2p, sh2)
            ffn_phase(C, HB, X, w1, w3, w2, G, g2, NT)
            ln_inplace(C, X, NT, c_ln2g, c_ln2b)
            outt = Tl(None, "out")
            if it == 0:
                OUT = outt
            for k in range(KC):
                P.dma("sp", OUT, outT[k * 128:(k + 1) * 128, it * NT:(it + 1) * NT], X[k], X[k].ap[:])
        P.finish([OUT])
        P.emit_all()
    return nc


def cols(v):
    v = np.asarray(v, np.float32).reshape(-1)
    return np.ascontiguousarray(v.reshape(-1, 128).T)


def shard_rows(w2d, core):
    w2d = np.asarray(w2d, np.float32)
    return w2d.reshape(-1, w2d.shape[-1])


def _run(nc, in_maps):
    res = run_bass_kernel_spmd(nc, in_maps, core_ids=list(range(NCORE)))
    return res.results


def run_layer0(inp, x):
    nc = build_layer0()
    in_maps = []
    for core in range(NCORE):
        b, h = core // 2, core % 2
        xT = np.zeros((D, HALO + TC), np.float32)
        t0 = h * TC
        xT[:, HALO:] = x[b, t0:t0 + TC, :].T
        if h == 1:
            xT[:, :HALO] = x[b, t0 - HALO:t0, :].T
        m = {
            "xT": xT, "modc": cols(inp["_mod"][0][b]),
            "hmask": np.full((128, 1), float(h), np.float32),
            "ln1_g": cols(inp["ln1_g"][0]), "ln1_b": cols(inp["ln1_b"][0]), "ln2_g": cols(inp["ln2_g"][0]), "ln2_b": cols(inp["ln2_b"][0]),
            "a_w_in": shard_rows(inp["a_w_in"][0], core), "a_b_in": cols(inp["a_b_in"][0]),
            "a_w_dw": inp["a_w_dw"][0].T.reshape(KC, 128, 31).transpose(1, 0, 2), "a_b_dw": cols(inp["a_b_dw"][0]),
            "a_ln_g": cols(inp["a_ln_g"][0]), "a_ln_b": cols(inp["a_ln_b"][0]), "a_w_out": shard_rows(inp["a_w_out"][0], core), "a_b_out": cols(inp["a_b_out"][0]),
            "f_w1": shard_rows(inp["f_w1"][0], core), "f_w3": shard_rows(inp["f_w3"][0], core), "f_w2": shard_rows(inp["f_w2"][0], core),
        }
        in_maps.append({k: np.ascontiguousarray(v, dtype=np.float32) for k, v in m.items()})
    res = _run(nc, in_maps)
    out = np.empty_like(x)
    for core in range(NCORE):
        b, h = core // 2, core % 2
        out[b, h * TC:(h + 1) * TC, :] = res[core]["outT"].T
    return out


def setup_common(P, C, modc_d):
    modc = P.sb([128, 6 * KC], F32, "modc_sb")
    P.dma("sp", modc, modc.ap[:], None, modc_d)
    vs = [Tl(modc.ap[:, v * KC:(v + 1) * KC], "m%d" % v) for v in range(6)]
    for t in vs:
        t.al = (modc,)
    return vs


def plus_one(P, t, name):
    o = P.sb([128, KC], F32, name)
    P.add("dve", lambda e: e.tensor_scalar(o.ap[:], t.ap[:], 1.0, None, ALU.add), [t], [o])
    return o


def build_head(ncols, layer_tag):
    nc = bass.Bass("TRN2", target_bir_lowering=False)
    dr = lambda name, shape: nc.dram_tensor(name, list(shape), F32, kind="ExternalInput").ap()
    xT = dr("xT", [D, TC])
    modc_d = dr("modc", [128, 6 * KC])
    nm = (ncols + 127) // 128
    ncp = nm * 128
    b = dr("b", [128, nm])
    outT = nc.dram_tensor("outT", [ncp, TC], F32, kind="ExternalOutput").ap()
    with ExitStack() as es:
        P = Prog(nc, es)
        w = P.gathered("w", D, ncp)
        C = Ctx(P)
        sh1, sc1, g1, sh2, sc2, g2 = setup_common(P, C, modc_d)
        sc1p = plus_one(P, sc1, "sc1p")
        c_b = load_cols(P, "c_b", b, nm)
        X = [P.sb([128, NT], F32, "X%d" % k) for k in range(KC)]
        HB = [P.sb([128, NT], BF16, "HB%d" % k) for k in range(KC)]
        O = [P.sb([128, NT], F32, "O%d" % k) for k in range(4)]
        OUT = Tl(None, "out")
        oi = [0]
        for it in range(TC // NT):
            for k in range(KC):
                P.dma("sp", X[k], X[k].ap[:], None, xT[k * 128:(k + 1) * 128, it * NT:(it + 1) * NT])
                P.add("act", (lambda e, o=HB[k].ap[:], i=X[k].ap[:], k=k:
                              e.activation(o, i, AF.Identity, bias=sh1.ap[:, k:k + 1], scale=sc1p.ap[:, k:k + 1])), [X[k], sh1, sc1p], [HB[k]])

            def epi(mi, gi, ps):
                o = O[oi[0] % 4]
                oi[0] += 1
                P.add("act", (lambda e, o_=o.ap[:], i=ps.ap[:]: e.activation(o_, i, AF.Identity, bias=c_b.ap[:, mi:mi + 1])), [ps, c_b], [o])
                P.dma("sp", OUT, outT[mi * 128:(mi + 1) * 128, it * NT:(it + 1) * NT], o, o.ap[:])
            full = (ncp // 512) * 512
            if full:
                linear(C, w, D, 0, full, 512, [(HB, lambda k: HB[k].ap[:])], epi)
            if ncp - full:
                linear(C, w, D, full, ncp - full, ncp - full, [(HB, lambda k: HB[k].ap[:])], epi)
        P.finish([OUT])
        P.emit_all()
    return nc


def moe_router(C, X, sc2p, sh2, wr_sb, n, hf_cb=None):
    P = C.P
    nch = n // 128
    pls = [C.next_ps() for _ in range(nch)]
    for k in range(KC):
        t = C.next_tmp()
        P.add("act", (lambda e, o=t.ap[:, 0:n], i=X[k].ap[:, 0:n], k=k:
                      e.activation(o, i, AF.Identity, bias=sh2.ap[:, k:k + 1], scale=sc2p.ap[:, k:k + 1])), [X[k], sh2, sc2p], [t])
        for j in range(nch):
            P.add("pe", (lambda e, o=pls[j].ap[:, 0:8], l=t.ap[:, j * 128:(j + 1) * 128], r=wr_sb.ap[:, k, :], st=(k == 0), sp=(k == KC - 1):
                         e.matmul(o, l, r, start=st, stop=sp)), [t, wr_sb], [pls[j]])
        if hf_cb is not None:
            hf_cb(k, t)
    if not hasattr(C, "rt"):
        C.rt = [P.sb([128, 4, 8], F32, nm) for nm in ("lg", "l2", "eq1", "eq2")] + [P.sb([128, 4], F32, nm) for nm in ("m1", "m2", "ga", "gb")]
    lg, l2, eq1, eq2, m1, m2, ga, gb = C.rt
    v3 = lambda t: t.ap[:, 0:nch, :]
    bc = lambda t: t.ap[:, 0:nch].unsqueeze(2).to_broadcast([128, nch, 8])
    for j in range(nch):
        P.add("dve", (lambda e, j=j: e.tensor_copy(lg.ap[:, j, :], pls[j].ap[:, 0:8])), [pls[j]], [lg])
    P.add("dve", lambda e: e.tensor_reduce(m1.ap[:, 0:nch], v3(lg), mybir.AxisListType.X, ALU.max), [lg], [m1])
    P.add("dve", lambda e: e.tensor_tensor(v3(eq1), v3(lg), bc(m1), ALU.is_equal), [lg, m1], [eq1])
    P.add("dve", lambda e: e.scalar_tensor_tensor(v3(l2), v3(eq1), -1e30, v3(lg), ALU.mult, ALU.add), [eq1, lg], [l2])
    P.add("dve", lambda e: e.tensor_reduce(m2.ap[:, 0:nch], v3(l2), mybir.AxisListType.X, ALU.max), [l2], [m2])
    P.add("dve", lambda e: e.tensor_tensor(v3(eq2), v3(l2), bc(m2), ALU.is_equal), [l2, m2], [eq2])
    P.add("dve", lambda e: e.tensor_tensor(gb.ap[:, 0:nch], m2.ap[:, 0:nch], m1.ap[:, 0:nch], ALU.subtract), [m1, m2], [gb])
    P.add("act", lambda e: e.activation(gb.ap[:, 0:nch], gb.ap[:, 0:nch], AF.Exp), [gb], [gb])
    P.add("dve", lambda e: e.tensor_scalar(ga.ap[:, 0:nch], gb.ap[:, 0:nch], 1.0, None, ALU.add), [gb], [ga])
    P.add("dve", lambda e: e.reciprocal(ga.ap[:, 0:nch], ga.ap[:, 0:nch]), [ga], [ga])
    P.add("dve", lambda e: e.tensor_tensor(gb.ap[:, 0:nch], gb.ap[:, 0:nch], ga.ap[:, 0:nch], ALU.mult), [ga, gb], [gb])
    P.add("dve", lambda e: e.tensor_tensor(v3(eq1), v3(eq1), bc(ga), ALU.mult), [eq1, ga], [eq1])
    P.add("dve", lambda e: e.tensor_tensor(v3(eq2), v3(eq2), bc(gb), ALU.mult), [eq2, gb], [eq2])
    P.add("dve", lambda e: e.tensor_tensor(v3(eq1), v3(eq1), v3(eq2), ALU.add), [eq1, eq2], [eq1])
    return eq1

GELU_C = 2.0 * math.sqrt(2.0 / math.pi)


def gelu_ops(C, xb_t, xb, out_t, out, n):
    P = C.P
    g = C.next_tmp()
    ga = g.ap[:, 0:n]
    P.add("act", lambda e: e.activation(ga, xb, AF.Square), [xb_t], [g])
    P.add("dve", lambda e: e.tensor_scalar(ga, ga, 0.044715, 1.0, ALU.mult, ALU.add), [g], [g])
    P.add("pool", lambda e: e.tensor_tensor(ga, ga, xb, ALU.mult), [g, xb_t], [g])
    P.add("act", lambda e: e.activation(ga, ga, AF.Sigmoid, scale=GELU_C), [g], [g])
    P.add("pool", lambda e: e.tensor_tensor(out, xb, ga, ALU.mult), [g, xb_t], [out_t])


DBG_TAIL = False


def build_tail(glu, moe, gmlp=False):
    nc = bass.Bass("TRN2", target_bir_lowering=False)
    dr = lambda name, shape: nc.dram_tensor(name, list(shape), F32, kind="ExternalInput").ap()
    xT = dr("xT", [D, TC])
    if not gmlp:
        yT = dr("yT", [D, TC])
    modc_d = dr("modc", [128, 6 * KC])
    CV = [128, KC]
    ln1_g, ln1_b, ln2_g, ln2_b = dr("ln1_g", CV), dr("ln1_b", CV), dr("ln2_g", CV), dr("ln2_b", CV)
    if gmlp:
        d_bi = dr("d_bi", [128, KC])
        d_bv = dr("d_bv", [128, D])
        d_lng, d_lnb = dr("d_lng", CV), dr("d_lnb", CV)
        d_wspT = dr("d_wspT", [128, 8, 128])
        d_mask = dr("d_mask", [128, 128])
        d_bsp = dr("d_bsp", [128, 8, 128])
    ncol = 2 * D if glu else D
    b = dr("b", [128, ncol // 128])
    if moe:
        wr = dr("wr", [128, KC, 8])
        idn = dr("idn", [128, 128])
    if not moe:
        outT = nc.dram_tensor("outT", [D, TC], F32, kind="ExternalOutput").ap()
    with ExitStack() as es:
        P = Prog(nc, es)
        w = P.gathered("w", D, ncol)
        if gmlp:
            w_gin = P.gathered("w_gin", D, 2 * D)
        if moe:
            x1T = nc.dram_tensor("x1T", [D, TC], F32, kind="ExternalOutput").ap()
            hfT = nc.dram_tensor("hfT", [D, TC], F32, kind="ExternalOutput").ap()
            gm_d = nc.dram_tensor("gm", [TC, 8], F32, kind="ExternalOutput").ap()
        else:
            w1, w3, w2 = P.gathered("w1", D, DFF), P.gathered("w3", D, DFF), P.gathered("w2", DFF, D)
        C = Ctx(P, nslab=(2 if gmlp else 3), ntmp=(4 if gmlp else 3))
        sh1, sc1, g1, sh2, sc2, g2 = setup_common(P, C, modc_d)
        sc2p = plus_one(P, sc2, "sc2p")
        if gmlp:
            sc1p = plus_one(P, sc1, "sc1p")
        c_ln1g, c_ln1b = load_cols(P, "c_ln1g", ln1_g, KC), load_cols(P, "c_ln1b", ln1_b, KC)
        c_ln2g, c_ln2b = load_cols(P, "c_ln2g", ln2_g, KC), load_cols(P, "c_ln2b", ln2_b, KC)
        c_b = load_cols(P, "c_b", b, ncol // 128)
        X = [P.sb([128, NT], F32, "X%d" % k) for k in range(KC)]
        HB = [P.sb([128, NT], BF16, "HB%d" % k) for k in range(KC)]
        if not gmlp:
            G = [P.sb([128, NT], BF16, "G%d" % m) for m in range(MC_FF)]
        else:
            arena = P.sb([128, MC_FF * NT], BF16, "arena")
            G = [Tl(arena.ap[:, m * NT:(m + 1) * NT], "G%d" % m) for m in range(MC_FF)]
            vview = arena.ap[:, 0:16384].bitcast(F32)
            V = [Tl(vview[:, j * D:(j + 1) * D], "V%d" % j) for j in range(4)]
            VB = [Tl(arena.ap[:, 16384 + j * D:16384 + (j + 1) * D], "VB%d" % j) for j in range(3)] + [Tl(arena.ap[:, 0:D], "VB3")]
            for j in range(4):
                V[j].al = tuple(G[8 * j:8 * j + 8]) + ((VB[3],) if j == 0 else ())
            for j in range(3):
                VB[j].al = tuple(G[32 + 4 * j:32 + 4 * j + 4])
            VB[3].al = tuple(G[0:4]) + (V[0],)
            for m in range(MC_FF):
                al = []
                if m < 32:
                    al.append(V[m // 8])
                    if m < 4:
                        al.append(VB[3])
                else:
                    al.append(VB[(m - 32) // 4])
                G[m].al = tuple(al)
            U = [P.sb([128, NT], BF16, "U%d" % k) for k in range(KC)]
            c_dbi = load_cols(P, "c_dbi", d_bi, KC)
            c_dlng, c_dlnb = load_cols(P, "c_dlng", d_lng, KC), load_cols(P, "c_dlnb", d_lnb, KC)
            BR = P.sb([128, D], F32, "BR")
            P.dma("sp", BR, BR.ap[:], None, d_bv)
            WTf = P.sb([128, 8, 128], F32, "WTf")
            P.dma("sp", WTf, WTf.ap[:], None, d_wspT)
            MK = P.sb([128, 128], F32, "MK")
            P.dma("sp", MK, MK.ap[:], None, d_mask)
            BSP = P.sb([128, 8, 128], F32, "BSP")
            P.dma("sp", BSP, BSP.ap[:], None, d_bsp)
            WT = P.sb([128, 8, 128], BF16, "WT")
            RS = P.sb([128, 8, 128], F32, "RS")
            for g_ in range(8):
                P.add("dve", (lambda e, g_=g_: e.tensor_tensor(WTf.ap[:, g_, :], WTf.ap[:, g_, :], MK.ap[:], ALU.mult)), [WTf, MK], [WTf])
            P.add("dve", lambda e: e.tensor_copy(WT.ap[:], WTf.ap[:]), [WTf], [WT])
            for g_ in range(0, 8, 4):
                pr_ = C.next_ps()
                P.add("pe", (lambda e, o=pr_.ap[:], r=WTf.ap[:, g_:g_ + 4, :]: e.matmul(o, C.ones.ap[:], r, start=True, stop=True)), [C.ones, WTf], [pr_])
                P.add("dve", (lambda e, o=RS.ap[:, g_:g_ + 4, :], i=pr_.ap[:].rearrange("p (g t) -> p g t", g=4): e.tensor_copy(o, i)), [pr_], [RS])
            lnc = [P.sb([128, 4], F32, "lnc%d" % i) for i in range(3)]
            lns = [P.sb([128, 1], F32, "lns%d" % i) for i in range(3)]
            t1s = [P.sb([128, 128], F32, "t1s%d" % i) for i in range(2)]
            t2s = [P.sb([128, 128], F32, "t2s%d" % i) for i in range(2)]
        if moe:
            ACC = None
            wr_sb = P.sb([128, KC, 8], F32, "wr_sb")
            P.dma("sp", wr_sb, wr_sb.ap[:], None, wr)
            ident = P.sb([128, 128], F32, "ident")
            P.dma("sp", ident, ident.ap[:], None, idn)
            dg = [P.sb([128, 128], F32, "dg%d" % i) for i in range(2)]
        if not glu:
            g1b = P.sb([128, KC], F32, "g1b")
            P.add("dve", lambda e: e.tensor_tensor(g1b.ap[:], g1.ap[:], c_b.ap[:], ALU.mult), [g1, c_b], [g1b])
        OUT = Tl(None, "out")
        if DBG_TAIL:
            dbgT = nc.dram_tensor("dbgT", [D, TC], F32, kind="ExternalOutput").ap()
            DBG = Tl(None, "dbg")
        for it in range(TC // NT):
            for k in range(KC):
                P.dma("sp", X[k], X[k].ap[:], None, xT[k * 128:(k + 1) * 128, it * NT:(it + 1) * NT])
                if not gmlp:
                    P.dma("pool", HB[k], HB[k].ap[:], None, yT[k * 128:(k + 1) * 128, it * NT:(it + 1) * NT])
                else:
                    P.add("act", (lambda e, o=HB[k].ap[:], i=X[k].ap[:], k=k:
                                  e.activation(o, i, AF.Identity, bias=sh1.ap[:, k:k + 1], scale=sc1p.ap[:, k:k + 1])), [X[k], sh1, sc1p], [HB[k]])
            if gmlp:
                def epi_u(mi, gi, ps):
                    xb = C.next_tmp()
                    P.add("act", (lambda e, o=xb.ap[:], i=ps.ap[:]: e.activation(o, i, AF.Identity, bias=c_dbi.ap[:, mi:mi + 1])), [ps, c_dbi], [xb])
                    gelu_ops(C, xb, xb.ap[:], U[mi], U[mi].ap[:], NT)
                linear(C, w_gin, D, 0, D, 512, [(HB, lambda k: HB[k].ap[:])], epi_u)
                wvv = w_gin.rearrange("(k p) n -> p k n", p=128)
                for s_ in range(4):
                    slab = C.next_slab()
                    sv = slab.ap[:, 0:KC * 512].rearrange("p (k n) -> p k n", k=KC)
                    P.dma("pool", slab, sv, None, wvv[:, :, D + s_ * 512:D + (s_ + 1) * 512])
                    for j in range(4):
                        ps = C.next_ps()
                        for k in range(KC):
                            P.add("pe", (lambda e, o=ps.ap[:], l=HB[k].ap[:, j * 128:(j + 1) * 128], r=sv[:, k, :], st=(k == 0), sp=(k == KC - 1):
                                         e.matmul(o, l, r, start=st, stop=sp)), [slab, HB[k]], [ps])
                        xb = C.next_tmp()
                        P.add("dve", (lambda e, o=xb.ap[:], a=ps.ap[:], b_=BR.ap[:, s_ * 512:(s_ + 1) * 512]: e.tensor_tensor(o, a, b_, ALU.add)), [ps, BR], [xb])
                        gelu_ops(C, xb, xb.ap[:], V[j], V[j].ap[:, s_ * 512:(s_ + 1) * 512], NT)
                for j in range(4):
                    c4, s1, s2 = lnc[j % 3], lns[j % 3], lns[(j + 1) % 3]
                    P.add("dve", (lambda e, o=s1.ap[:], i=V[j].ap[:]: e.tensor_reduce(o, i, mybir.AxisListType.X, ALU.add)), [V[j]], [s1])
                    P.add("dve", (lambda e, o=s1.ap[:]: e.tensor_scalar(o, o, -1.0 / D, None, ALU.mult)), [s1], [s1])
                    P.add("dve", (lambda e, o=V[j].ap[:], sc_=s1.ap[:, 0:1]: e.tensor_scalar(o, o, sc_, None, ALU.add)), [V[j], s1], [V[j]])
                    for s_ in range(4):
                        sq = C.next_tmp()
                        P.add("act", (lambda e, o=sq.ap[:], i=V[j].ap[:, s_ * 512:(s_ + 1) * 512]: e.activation(o, i, AF.Square)), [V[j]], [sq])
                        P.add("dve", (lambda e, o=c4.ap[:, s_:s_ + 1], i=sq.ap[:]: e.tensor_reduce(o, i, mybir.AxisListType.X, ALU.add)), [sq], [c4])
                    P.add("dve", (lambda e, o=s2.ap[:], i=c4.ap[:]: e.tensor_reduce(o, i, mybir.AxisListType.X, ALU.add)), [c4], [s2])
                    P.add("dve", (lambda e, o=s2.ap[:]: e.tensor_scalar(o, o, 1.0 / D, LN_EPS, ALU.mult, ALU.add)), [s2], [s2])
                    P.add("act", (lambda e, o=s2.ap[:]: e.activation(o, o, AF.Sqrt)), [s2], [s2])
                    P.add("dve", (lambda e, o=s2.ap[:]: e.reciprocal(o, o)), [s2], [s2])
                    P.add("dve", (lambda e, o=VB[j].ap[:], i=V[j].ap[:], sc_=s2.ap[:, 0:1]: e.tensor_scalar(o, i, sc_, None, ALU.mult)), [V[j], s2], [VB[j]])
                for c in range(KC):
                    g_ = c // 2
                    t1 = t1s[c % 2]
                    P.add("dve", (lambda e, o=t1.ap[:], r=RS.ap[:, g_, :], b_=BSP.ap[:, g_, :], c=c:
                                  e.scalar_tensor_tensor(o, r, c_dlnb.ap[:, c:c + 1], b_, ALU.mult, ALU.add)), [RS, BSP, c_dlnb], [t1])
                    for j in range(4):
                        ps = C.next_ps()
                        P.add("pe", (lambda e, o=ps.ap[:, 0:128], l=VB[j].ap[:, c * 128:(c + 1) * 128], r=WT.ap[:, g_, :]:
                                     e.matmul(o, l, r, start=True, stop=True)), [VB[j], WT], [ps])
                        t2 = t2s[j % 2]
                        P.add("dve", (lambda e, o=t2.ap[:], p_=ps.ap[:, 0:128], t1_=t1.ap[:], c=c:
                                      e.scalar_tensor_tensor(o, p_, c_dlng.ap[:, c:c + 1], t1_, ALU.mult, ALU.add)), [ps, t1, c_dlng], [t2])
                        P.add("pool", (lambda e, o=HB[c].ap[:, j * 128:(j + 1) * 128], a=t2.ap[:], u_=U[c].ap[:, j * 128:(j + 1) * 128]:
                                       e.tensor_tensor(o, a, u_, ALU.mult)), [t2, U[c]], [HB[c]])
            if glu:
                wv = w.rearrange("(k p) n -> p k n", p=128)
                for s in range(4):
                    sa = C.next_slab()
                    sg = C.next_slab()
                    va = sa.ap[:, 0:KC * 512].rearrange("p (k n) -> p k n", k=KC)
                    vg = sg.ap[:, 0:KC * 512].rearrange("p (k n) -> p k n", k=KC)
                    P.dma("pool", sa, va, None, wv[:, :, s * 512:(s + 1) * 512])
                    P.dma("pool", sg, vg, None, wv[:, :, D + s * 512:D + (s + 1) * 512])
                    for j in range(4):
                        m = s * 4 + j
                        pa = C.next_ps()
                        pg = C.next_ps()
                        for k in range(KC):
                            P.add("pe", (lambda e, o=pa.ap[:], l=va[:, k, j * 128:(j + 1) * 128], r=HB[k].ap[:], st=(k == 0), sp=(k == KC - 1):
                                         e.matmul(o, l, r, start=st, stop=sp)), [sa, HB[k]], [pa])
                        for k in range(KC):
                            P.add("pe", (lambda e, o=pg.ap[:], l=vg[:, k, j * 128:(j + 1) * 128], r=HB[k].ap[:], st=(k == 0), sp=(k == KC - 1):
                                         e.matmul(o, l, r, start=st, stop=sp)), [sg, HB[k]], [pg])
                        t = C.next_tmp()
                        P.add("act", (lambda e, o=t.ap[:], i=pg.ap[:], m=m:
                                      e.activation(o, i, AF.Sigmoid, bias=c_b.ap[:, KC + m:KC + m + 1])), [pg, c_b], [t])
                        P.add("dve", (lambda e, o=t.ap[:], a=pa.ap[:], m=m:
                                      e.scalar_tensor_tensor(o, a, c_b.ap[:, m:m + 1], o, ALU.add, ALU.mult)), [pa, t, c_b], [t])
                        P.add("act", (lambda e, o=t.ap[:], m=m: e.activation(o, o, AF.Copy, scale=g1.ap[:, m:m + 1])), [t, g1], [t])
                        P.add("dve", (lambda e, o=X[m].ap[:], b_=t.ap[:]: e.scalar_tensor_tensor(o, o, ALPHA, b_, ALU.mult, ALU.add)), [X[m], t], [X[m]])
            else:
                def epi_out(mi, gi, ps):
                    t = C.next_tmp()
                    P.add("act", (lambda e, o=t.ap[:], i=ps.ap[:]: e.activation(o, i, AF.Identity, bias=g1b.ap[:, mi:mi + 1], scale=g1.ap[:, mi:mi + 1])), [ps, g1b, g1], [t])
                    P.add("dve", (lambda e, o=X[mi].ap[:], b_=t.ap[:]: e.scalar_tensor_tensor(o, o, ALPHA, b_, ALU.mult, ALU.add)), [X[mi], t], [X[mi]])
                linear(C, w, D, 0, D, 512, [(HB, lambda k: HB[k].ap[:])], epi_out)
            ln_inplace(C, X, NT, c_ln1g, c_ln1b, HB, sc2p, sh2)
            if DBG_TAIL:
                for k in range(KC):
                    P.dma("sp", DBG, dbgT[k * 128:(k + 1) * 128, it * NT:(it + 1) * NT], X[k], X[k].ap[:])
            if not moe:
                ffn_phase(C, HB, X, w1, w3, w2, G, g2, NT)
            else:
                def hf_out(k, t):
                    P.dma("sp", OUT, hfT[k * 128:(k + 1) * 128, it * NT:(it + 1) * NT], t, t.ap[:])
                Gm = moe_router(C, X, sc2p, sh2, wr_sb, NT, hf_out)
                P.dma("sp", OUT, gm_d[it * NT:(it + 1) * NT, :].rearrange("(j p) e -> p j e", p=128), Gm, Gm.ap[:])
                for k in range(KC):
                    P.dma("sp", OUT, x1T[k * 128:(k + 1) * 128, it * NT:(it + 1) * NT], X[k], X[k].ap[:])
                continue
            ln_inplace(C, X, NT, c_ln2g, c_ln2b)
            for k in range(KC):
                P.dma("sp", OUT, outT[k * 128:(k + 1) * 128, it * NT:(it + 1) * NT], X[k], X[k].ap[:])
        P.finish([OUT] + ([DBG] if DBG_TAIL else []))
        P.emit_all()
    return nc


def common_maps(inp, layer, core):
    b = core // 2
    return {"modc": cols(inp["_mod"][layer][b])}


def tok_T(x, core):
    b, h = core // 2, core % 2
    return np.ascontiguousarray(x[b, h * TC:(h + 1) * TC, :].T)


def from_T(res, key, width):
    out = np.empty((4, SEQ, width), np.float32)
    for core in range(NCORE):
        b, h = core // 2, core % 2
        out[b, h * TC:(h + 1) * TC, :] = res[core][key].T[:, :width]
    return out


def run_head(inp, x, layer, w, bvec):
    ncols = w.shape[1]
    nm = (ncols + 127) // 128
    wp = np.zeros((D, nm * 128), np.float32)
    wp[:, :ncols] = w
    bp = np.zeros((nm * 128,), np.float32)
    bp[:ncols] = bvec
    nc = build_head(ncols, layer)
    maps = []
    for core in range(NCORE):
        m = common_maps(inp, layer, core)
        m.update({"xT": tok_T(x, core), "w": shard_rows(wp, core), "b": cols(bp)})
        maps.append({k: np.ascontiguousarray(v, dtype=np.float32) for k, v in m.items()})
    res = _run(nc, maps)
    return from_T(res, "outT", ncols)


def run_tail(inp, x, y, layer, w, bvec, glu, moe, gmlp=False):
    nc = build_tail(glu, moe, gmlp)
    maps = []
    fi = layer // 2
    for core in range(NCORE):
        m = common_maps(inp, layer, core)
        if gmlp:
            m.update({"w_gin": shard_rows(inp["d_w_in"][0], core), "d_bi": cols(inp["d_b_in"][0][:D]),
                      "d_bv": np.tile(inp["d_b_in"][0][D:][None, :], (128, 1)),
                      "d_lng": cols(inp["d_ln_g"][0]), "d_lnb": cols(inp["d_ln_b"][0]),
                      "d_wspT": inp["d_w_sp"][0].transpose(2, 0, 1),
                      "d_mask": np.triu(np.ones((128, 128), np.float32)),
                      "d_bsp": np.tile(inp["d_b_sp"][0][None, :, :], (128, 1, 1))})
        else:
            m["yT"] = tok_T(y, core)
        m.update({"xT": tok_T(x, core), "w": shard_rows(w, core), "b": cols(bvec),
                  "ln1_g": cols(inp["ln1_g"][layer]), "ln1_b": cols(inp["ln1_b"][layer]),
                  "ln2_g": cols(inp["ln2_g"][layer]), "ln2_b": cols(inp["ln2_b"][layer])})
        if moe:
            m.update({"wr": inp["m_router"][fi].reshape(KC, 128, 8).transpose(1, 0, 2),
                      "idn": np.eye(128, dtype=np.float32)})
        else:
            m.update({"w1": shard_rows(inp["f_w1"][fi], core), "w3": shard_rows(inp["f_w3"][fi], core), "w2": shard_rows(inp["f_w2"][fi], core)})
        maps.append({k: np.ascontiguousarray(v, dtype=np.float32) for k, v in m.items()})
    res = _run(nc, maps)
    if moe:
        gm = np.empty((4, SEQ, 8), np.float32)
        for core in range(NCORE):
            b, h = core // 2, core % 2
            gm[b, h * TC:(h + 1) * TC] = res[core]["gm"]
        return from_T(res, "x1T", D), from_T(res, "hfT", D), gm
    return from_T(res, "outT", D)


S5_TB = 512
TWO_PI = 2.0 * math.pi

MAGIC = 12582912.0
INV2PI = 1.0 / (2.0 * math.pi)
PI_SAFE = 3.14159


def sincos(P, ang, sn, cs, w1, w2, aa, sa, ca):
    A = P.add
    for (dst, da, shift) in ((sn, sa, 0.0), (cs, ca, 0.25)):
        A("dve", lambda e, sh=shift: e.tensor_scalar(aa(w1), aa(ang), INV2PI, sh, ALU.mult, ALU.add), [ang], [w1])
        A("dve", lambda e: e.tensor_scalar(aa(w1), aa(w1), MAGIC, None, ALU.add), [w1], [w1])
        A("dve", lambda e: e.tensor_scalar(aa(w1), aa(w1), -MAGIC, None, ALU.add), [w1], [w1])
        A("dve", lambda e: e.scalar_tensor_tensor(aa(w2), aa(w1), -TWO_PI, aa(ang), ALU.mult, ALU.add), [w1, ang], [w2])
        A("dve", lambda e, sh=shift: e.tensor_scalar(aa(w2), aa(w2), sh * TWO_PI, PI_SAFE, ALU.add, ALU.min), [w2], [w2])
        A("dve", lambda e: e.tensor_scalar(aa(w2), aa(w2), -PI_SAFE, None, ALU.max), [w2], [w2])
        A("act", lambda e, d_=dst, da_=da: e.activation(da_(d_), aa(w2), AF.Sin), [w2], [dst])


def build_s5core():
    nc = bass.Bass("TRN2", target_bir_lowering=False)
    dr = lambda name, shape: nc.dram_tensor(name, list(shape), F32, kind="ExternalInput").ap()
    NP_ = 32
    NCH = 8
    uT = dr("uT", [NCH * 128, SEQ])
    are_c, aim_c, ldt_c = dr("are_c", [128, NP_]), dr("aim_c", [128, NP_]), dr("ldt_c", [128, NP_])
    bre_l, bim_l = dr("bre_l", [128, NP_, 128]), dr("bim_l", [128, NP_, 128])
    cre_l, cim_l = dr("cre_l", [128, NP_, 128]), dr("cim_l", [128, NP_, 128])
    d_c = dr("d_c", [128, NCH])
    iota_d = dr("iota", [128, S5_TB])
    yT = nc.dram_tensor("yT", [NCH * 128, SEQ], F32, kind="ExternalOutput").ap()
    TB = S5_TB
    NB = SEQ // TB
    with ExitStack() as es:
        P = Prog(nc, es)
        pss = [P.ps([128, 512], F32, "ps%d" % i) for i in range(8)]
        psi = [0]

        def nps():
            p = pss[psi[0] % 8]
            psi[0] += 1
            return p
        ld = lambda name, ap, shape: (lambda t: (P.dma("sp", t, t.ap[:], None, ap), t)[1])(P.sb(shape, F32, name))
        are = ld("are", are_c, [128, NP_])
        aim = ld("aim", aim_c, [128, NP_])
        ldt = ld("ldt", ldt_c, [128, NP_])
        dcol = ld("dcol", d_c, [128, NCH])
        iota = ld("iota_sb", iota_d, [128, TB])
        brel = P.sb([128, NP_, 128], BF16, "brel")
        biml = P.sb([128, NP_, 128], BF16, "biml")
        P.dma("pool", brel, brel.ap[:], None, bre_l)
        P.dma("pool", biml, biml.ap[:], None, bim_l)
        sm = lambda name: P.sb([128, NP_], F32, name)
        dt, rho, th, sn, cs, lr, li, den, zr, zi, t1, t2 = [sm(n) for n in ("dt", "rho", "th", "sn0", "cs0", "lr", "li", "den", "zr", "zi", "t1s", "t2s")]
        negpi = P.sb([128, 1], F32, "negpi")
        P.add("dve", lambda e: e.memset(negpi.ap[:], -math.pi), [], [negpi])
        A = lambda eng, f, r, w: P.add(eng, f, r, w)
        A("act", lambda e: e.activation(dt.ap[:], ldt.ap[:], AF.Exp), [ldt], [dt])
        A("dve", lambda e: e.tensor_tensor(t1.ap[:], are.ap[:], dt.ap[:], ALU.mult), [are, dt], [t1])
        A("act", lambda e: e.activation(rho.ap[:], t1.ap[:], AF.Exp), [t1], [rho])
        A("dve", lambda e: e.tensor_tensor(th.ap[:], aim.ap[:], dt.ap[:], ALU.mult), [aim, dt], [th])
        full = lambda t: t.ap[:]
        sincos(P, th, sn, cs, t1, t2, full, full, full)
        thb, snB, csB = sm("thb"), sm("snB"), sm("csB")
        A("dve", lambda e: e.tensor_scalar(thb.ap[:], th.ap[:], float(S5_TB), None, ALU.mult), [th], [thb])
        sincos(P, thb, snB, csB, t1, t2, full, full, full)
        A("dve", lambda e: e.tensor_tensor(lr.ap[:], rho.ap[:], cs.ap[:], ALU.mult), [rho, cs], [lr])
        A("dve", lambda e: e.tensor_tensor(li.ap[:], rho.ap[:], sn.ap[:], ALU.mult), [rho, sn], [li])
        A("dve", lambda e: e.tensor_tensor(den.ap[:], are.ap[:], are.ap[:], ALU.mult), [are], [den])
        A("dve", lambda e: e.tensor_tensor(t1.ap[:], aim.ap[:], aim.ap[:], ALU.mult), [aim], [t1])
        A("dve", lambda e: e.tensor_tensor(den.ap[:], den.ap[:], t1.ap[:], ALU.add), [den, t1], [den])
        A("dve", lambda e: e.reciprocal(den.ap[:], den.ap[:]), [den], [den])
        A("dve", lambda e: e.tensor_scalar(lr.ap[:], lr.ap[:], -1.0, None, ALU.add), [lr], [lr])
        A("dve", lambda e: e.tensor_tensor(t1.ap[:], lr.ap[:], are.ap[:], ALU.mult), [lr, are], [t1])
        A("dve", lambda e: e.tensor_tensor(t2.ap[:], li.ap[:], aim.ap[:], ALU.mult), [li, aim], [t2])
        A("dve", lambda e: e.tensor_tensor(zr.ap[:], t1.ap[:], t2.ap[:], ALU.add), [t1, t2], [zr])
        A("dve", lambda e: e.tensor_tensor(zr.ap[:], zr.ap[:], den.ap[:], ALU.mult), [zr, den], [zr])
        A("dve", lambda e: e.tensor_tensor(t1.ap[:], li.ap[:], are.ap[:], ALU.mult), [li, are], [t1])
        A("dve", lambda e: e.tensor_tensor(t2.ap[:], lr.ap[:], aim.ap[:], ALU.mult), [lr, aim], [t2])
        A("dve", lambda e: e.tensor_tensor(zi.ap[:], t1.ap[:], t2.ap[:], ALU.subtract), [t1, t2], [zi])
        A("dve", lambda e: e.tensor_tensor(zi.ap[:], zi.ap[:], den.ap[:], ALU.mult), [zi, den], [zi])
        nzi = sm("nzi")
        A("dve", lambda e: e.tensor_scalar(nzi.ap[:], zi.ap[:], -1.0, None, ALU.mult), [zi], [nzi])
        cpr = P.sb([128, NP_, 128], BF16, "cpr")
        ncpi = P.sb([128, NP_, 128], BF16, "ncpi")
        ctmp = [P.sb([128, 128], F32, "ctmp%d" % i) for i in range(4)]
        for j in range(NP_):
            cr_t, ci_t, w1_t, w2_t = ctmp
            P.dma("sp", cr_t, cr_t.ap[:], None, cre_l[:, j, :])
            P.dma("sp", ci_t, ci_t.ap[:], None, cim_l[:, j, :])
            A("dve", lambda e, j=j: e.tensor_scalar(w1_t.ap[:], ci_t.ap[:], nzi.ap[:, j:j + 1], None, ALU.mult), [ci_t, nzi], [w1_t])
            A("dve", lambda e, j=j: e.scalar_tensor_tensor(cpr.ap[:, j, :], cr_t.ap[:], zr.ap[:, j:j + 1], w1_t.ap[:], ALU.mult, ALU.add), [cr_t, zr, w1_t], [cpr])
            A("dve", lambda e, j=j: e.tensor_scalar(w2_t.ap[:], ci_t.ap[:], zr.ap[:, j:j + 1], None, ALU.mult), [ci_t, zr], [w2_t])
            A("dve", lambda e, j=j: e.scalar_tensor_tensor(w2_t.ap[:], cr_t.ap[:], zi.ap[:, j:j + 1], w2_t.ap[:], ALU.mult, ALU.add), [cr_t, zi, w2_t], [w2_t])
            A("dve", lambda e, j=j: e.tensor_scalar(ncpi.ap[:, j, :], w2_t.ap[:], -1.0, None, ALU.mult), [w2_t], [ncpi])
        W = lambda name: P.sb([128, TB], F32, name)
        ub = P.sb([128, SEQ], BF16, "ub")
        uf = P.sb([128, SEQ], F32, "uf")
        Sr = [P.sb([128, SEQ], BF16, "Sr%d" % i) for i in range(4)]
        Si = [P.sb([128, SEQ], BF16, "Si%d" % i) for i in range(4)]
        rho_t = W("rho_t")
        ang, r1, r2, sn_t, cs_t = W("ang"), W("r1"), W("r2"), W("sn_t"), W("cs_t")
        arS, aiS = W("arS"), W("aiS")
        q1, q2, q3, q4 = W("q1"), W("q2"), W("q3"), W("q4")
        xr, xi = W("xr"), W("xi")
        sr2 = [W("sra"), W("srb")]
        si2 = [W("sia"), W("sib")]
        car = [P.sb([128, 1], F32, "car%d" % i) for i in range(4)]
        zero_c = P.sb([128, 1], F32, "zero_c")
        A("dve", lambda e: e.memset(zero_c.ap[:], 0.0), [], [zero_c])
        yo = [W("yo0"), W("yo1")]
        g1t, g2t = W("g1t"), W("g2t")
        OUT = Tl(None, "out")
        for c in range(NCH):
            P.dma("sp", uf, uf.ap[:], None, uT[c * 128:(c + 1) * 128, :])
            P.dma("pool", ub, ub.ap[:], None, uT[c * 128:(c + 1) * 128, :])
            for jj in range(4):
                j = c * 4 + jj
                A("dve", lambda e, j=j: e.tensor_scalar(rho_t.ap[:], iota.ap[:], 0.0, rho.ap[:, j:j + 1], ALU.mult, ALU.add), [iota, rho], [rho_t])
                A("dve", lambda e, j=j: e.tensor_scalar(ang.ap[:], iota.ap[:], th.ap[:, j:j + 1], None, ALU.mult), [iota, th], [ang])
                sincos(P, ang, sn_t, cs_t, r1, r2, full, full, full)
                for k in range(NB):
                    pr, pi_ = nps(), nps()
                    A("pe", lambda e, o=pr.ap[:], l=brel.ap[:, j, :], r=ub.ap[:, k * TB:(k + 1) * TB]: e.matmul(o, l, r, start=True, stop=True), [brel, ub], [pr])
                    A("pe", lambda e, o=pi_.ap[:], l=biml.ap[:, j, :], r=ub.ap[:, k * TB:(k + 1) * TB]: e.matmul(o, l, r, start=True, stop=True), [biml, ub], [pi_])
                    A("act", lambda e, o=arS.ap[:], i=pr.ap[:]: e.activation(o, i, AF.Copy), [pr], [arS])
                    A("act", lambda e, o=aiS.ap[:], i=pi_.ap[:]: e.activation(o, i, AF.Copy), [pi_], [aiS])
                    A("dve", lambda e: e.tensor_tensor(q1.ap[:], arS.ap[:], cs_t.ap[:], ALU.mult), [arS, cs_t], [q1])
                    A("dve", lambda e: e.tensor_tensor(q2.ap[:], aiS.ap[:], sn_t.ap[:], ALU.mult), [aiS, sn_t], [q2])
                    A("pool", lambda e: e.tensor_tensor(q3.ap[:], aiS.ap[:], cs_t.ap[:], ALU.mult), [aiS, cs_t], [q3])
                    A("pool", lambda e: e.tensor_tensor(q4.ap[:], arS.ap[:], sn_t.ap[:], ALU.mult), [arS, sn_t], [q4])
                    A("dve", lambda e: e.tensor_tensor(xr.ap[:], q1.ap[:], q2.ap[:], ALU.add), [q1, q2], [xr])
                    A("pool", lambda e: e.tensor_tensor(xi.ap[:], q3.ap[:], q4.ap[:], ALU.subtract), [q3, q4], [xi])
                    srt, sit = sr2[k % 2], si2[k % 2]
                    if k == 0:
                        ir, ii_, rd = zero_c.ap[:, 0:1], zero_c.ap[:, 0:1], [zero_c]
                    else:
                        pr_ = sr2[(k - 1) % 2]
                        pi2_ = si2[(k - 1) % 2]
                        A("dve", lambda e, j=j, p_=pi2_: e.tensor_scalar(car[2].ap[:], p_.ap[:, TB - 1:TB], snB.ap[:, j:j + 1], None, ALU.mult), [pi2_, snB], [car[2]])
                        A("dve", lambda e, j=j, p_=pr_: e.scalar_tensor_tensor(car[0].ap[:], p_.ap[:, TB - 1:TB], csB.ap[:, j:j + 1], car[2].ap[:], ALU.mult, ALU.subtract), [pr_, csB, car[2]], [car[0]])
                        A("dve", lambda e, j=j, p_=pi2_: e.tensor_scalar(car[3].ap[:], p_.ap[:, TB - 1:TB], csB.ap[:, j:j + 1], None, ALU.mult), [pi2_, csB], [car[3]])
                        A("dve", lambda e, j=j, p_=pr_: e.scalar_tensor_tensor(car[1].ap[:], p_.ap[:, TB - 1:TB], snB.ap[:, j:j + 1], car[3].ap[:], ALU.mult, ALU.add), [pr_, snB, car[3]], [car[1]])
                        ir, ii_, rd = car[0].ap[:, 0:1], car[1].ap[:, 0:1], [car[0], car[1]]
                    A("dve", lambda e, o=srt.ap[:], i0=ir: e.tensor_tensor_scan(o, rho_t.ap[:], xr.ap[:], i0, ALU.mult, ALU.add), [rho_t, xr] + rd, [srt])
                    A("dve", lambda e, o=sit.ap[:], i0=ii_: e.tensor_tensor_scan(o, rho_t.ap[:], xi.ap[:], i0, ALU.mult, ALU.add), [rho_t, xi] + rd, [sit])
                    A("dve", lambda e, s_=srt: e.tensor_tensor(q1.ap[:], s_.ap[:], cs_t.ap[:], ALU.mult), [srt, cs_t], [q1])
                    A("dve", lambda e, s_=sit: e.tensor_tensor(q2.ap[:], s_.ap[:], sn_t.ap[:], ALU.mult), [sit, sn_t], [q2])
                    A("pool", lambda e, s_=srt: e.tensor_tensor(q3.ap[:], s_.ap[:], sn_t.ap[:], ALU.mult), [srt, sn_t], [q3])
                    A("pool", lambda e, s_=sit: e.tensor_tensor(q4.ap[:], s_.ap[:], cs_t.ap[:], ALU.mult), [sit, cs_t], [q4])
                    A("dve", lambda e, o=Sr[jj].ap[:, k * TB:(k + 1) * TB]: e.tensor_tensor(o, q1.ap[:], q2.ap[:], ALU.subtract), [q1, q2], [Sr[jj]])
                    A("pool", lambda e, o=Si[jj].ap[:, k * TB:(k + 1) * TB]: e.tensor_tensor(o, q3.ap[:], q4.ap[:], ALU.add), [q3, q4], [Si[jj]])
            for k in range(NB):
                ps = nps()
                for jj in range(4):
                    j = c * 4 + jj
                    A("pe", lambda e, o=ps.ap[:], l=cpr.ap[:, j, :], r=Sr[jj].ap[:, k * TB:(k + 1) * TB], st=(jj == 0): e.matmul(o, l, r, start=st, stop=False), [cpr, Sr[jj]], [ps])
                    A("pe", lambda e, o=ps.ap[:], l=ncpi.ap[:, j, :], r=Si[jj].ap[:, k * TB:(k + 1) * TB], sp=(jj == 3): e.matmul(o, l, r, start=False, stop=sp), [ncpi, Si[jj]], [ps])
                y = yo[k % 2]
                A("dve", lambda e, o=y.ap[:], u_=uf.ap[:, k * TB:(k + 1) * TB], p_=ps.ap[:], c=c: e.scalar_tensor_tensor(o, u_, dcol.ap[:, c:c + 1], p_, ALU.mult, ALU.add), [uf, dcol, ps], [y])
                A("act", lambda e, o=g1t.ap[:], i=y.ap[:]: e.activation(o, i, AF.Square), [y], [g1t])
                A("dve", lambda e: e.tensor_scalar(g1t.ap[:], g1t.ap[:], 0.044715, 1.0, ALU.mult, ALU.add), [g1t], [g1t])
                A("pool", lambda e, i=y.ap[:]: e.tensor_tensor(g2t.ap[:], g1t.ap[:], i, ALU.mult), [g1t, y], [g2t])
                A("act", lambda e: e.activation(g2t.ap[:], g2t.ap[:], AF.Sigmoid, scale=2.0 * math.sqrt(2.0 / math.pi)), [g2t], [g2t])
                A("pool", lambda e, o=y.ap[:]: e.tensor_tensor(o, o, g2t.ap[:], ALU.mult), [y, g2t], [y])
                P.dma("sp", OUT, yT[c * 128:(c + 1) * 128, k * TB:(k + 1) * TB], y, y.ap[:])
        P.finish([OUT])
        P.emit_all()
    return nc


def run_s5core(inp, u):
    nc = build_s5core()
    are, aim, ldt = inp["b_a_re"][0], inp["b_a_im"][0], inp["b_log_dt"][0]
    bre, bim, cre, cim = inp["b_b_re"][0], inp["b_b_im"][0], inp["b_c_re"][0], inp["b_c_im"][0]
    dvec = inp["b_d"][0]
    maps = []
    for core in range(NCORE):
        b, gh = core // 2, core % 2
        g0 = gh * 64
        pc = lambda a: np.ascontiguousarray(a[g0:g0 + 64].reshape(32, 128).T)
        ldt_rep = np.repeat(ldt[:, None], 64, axis=1)
        bre_l = np.zeros((128, 32, 128), np.float32)
        bim_l = np.zeros((128, 32, 128), np.float32)
        cre_l = np.zeros((128, 32, 128), np.float32)
        cim_l = np.zeros((128, 32, 128), np.float32)
        for j in range(32):
            for gi in range(2):
                g = g0 + 2 * j + gi
                r0 = 16 * ((2 * j + gi) % 8)
                bre_l[r0:r0 + 16, j, gi * 64:(gi + 1) * 64] = bre[g].T
                bim_l[r0:r0 + 16, j, gi * 64:(gi + 1) * 64] = bim[g].T
                cre_l[gi * 64:(gi + 1) * 64, j, r0:r0 + 16] = cre[g].T
                cim_l[gi * 64:(gi + 1) * 64, j, r0:r0 + 16] = cim[g].T
        m = {"uT": np.ascontiguousarray(u[b, :, gh * 1024:(gh + 1) * 1024].T),
             "are_c": pc(are), "aim_c": pc(aim), "ldt_c": pc(ldt_rep),
             "bre_l": bre_l, "bim_l": bim_l, "cre_l": cre_l, "cim_l": cim_l,
             "d_c": np.ascontiguousarray(dvec[gh * 1024:(gh + 1) * 1024].reshape(8, 128).T),
             "iota": np.tile(np.arange(S5_TB, dtype=np.float32)[None, :], (128, 1))}
        maps.append({k: np.ascontiguousarray(v, dtype=np.float32) for k, v in m.items()})
    res = _run(nc, maps)
    y = np.empty((4, SEQ, D), np.float32)
    for core in range(NCORE):
        b, gh = core // 2, core % 2
        y[b, :, gh * 1024:(gh + 1) * 1024] = res[core]["yT"].T
    return y


ML_T = 128
DKS = 128.0 ** -0.5


def build_mlstm():
    nc = bass.Bass("TRN2", target_bir_lowering=False)
    dr = lambda name, shape: nc.dram_tensor(name, list(shape), F32, kind="ExternalInput").ap()
    qkT = dr("qkT", [8, 128, SEQ])
    wcv = dr("wcv", [128, 8, 4])
    bcv = dr("bcv", [128, 8])
    v_d = dr("v_tok", [SEQ, 4, 256])
    o_d = dr("o_tok", [SEQ, 1024])
    gi_d = dr("gi_tok", [SEQ, 4])
    gf_d = dr("gf_tok", [SEQ, 4])
    mhg_d = dr("mhg", [128, 1024])
    tri_d, blk_d, mkc_d, cm0_d, cm1_d, idn_d = [dr(n, [128, 128]) for n in ("tri", "blk", "mkc", "cm0", "cm1", "idn")]
    hs_d = nc.dram_tensor("hs", [SEQ, 1024], F32, kind="ExternalOutput").ap()
    NTL = SEQ // ML_T
    with ExitStack() as es:
        P = Prog(nc, es)
        A = P.add
        ldc = lambda name, ap, shape, dt=F32, q="sp": (lambda t: (P.dma(q, t, t.ap[:], None, ap), t)[1])(P.sb(shape, dt, name))
        TRI = ldc("TRI", tri_d, [128, 128])
        BLK = ldc("BLK", blk_d, [128, 128])
        MKC = ldc("MKC", mkc_d, [128, 128])
        CM0 = ldc("CM0", cm0_d, [128, 128])
        CM1 = ldc("CM1", cm1_d, [128, 128])
        IDB = ldc("IDB", idn_d, [128, 128], BF16, "pool")
        MHG = ldc("MHG", mhg_d, [128, 1024])
        WCV = ldc("WCV", wcv, [128, 8, 4])
        BCV = ldc("BCV", bcv, [128, 8])
        ONES = P.sb([128, 128], F32, "ONES")
        A("dve", lambda e: e.memset(ONES.ap[:], 1.0), [], [ONES])
        lnk = P.sb([128, 1], F32, "lnk")
        A("dve", lambda e: e.memset(lnk.ap[:], math.log(DKS)), [], [lnk])
        CF = [P.sb([128, 257], F32, "CF%d" % h) for h in range(4)]
        CA = [P.sb([128, 257], BF16, "CA%d" % h) for h in range(4)]
        CAm = [P.sb([128, 257], BF16, "CAm%d" % h) for h in range(4)]
        for h in range(4):
            A("dve", lambda e, h=h: e.memset(CF[h].ap[:], 0.0), [], [CF[h]])
            A("dve", lambda e, h=h: e.memset(CA[h].ap[:], 0.0), [], [CA[h]])
        QKR = [P.sb([128, 8, 3 + ML_T], F32, "QKR%d" % i) for i in range(2)]
        A("dve", lambda e: e.memset(QKR[1].ap[:], 0.0), [], [QKR[1]])
        QKC = [P.sb([128, 8, ML_T], BF16, "QKC%d" % i) for i in range(2)]
        cacc = [P.sb([128, ML_T], F32, "cacc%d" % i) for i in range(2)]
        VA = [P.sb([128, 4, 257], BF16, "VA%d" % i) for i in range(2)]
        for i in range(2):
            A("dve", lambda e, i=i: e.memset(VA[i].ap[:, :, 256:257], 1.0), [], [VA[i]])
        OT = [P.sb([128, 1024], F32, "OT%d" % i) for i in range(2)]
        GI = [P.sb([128, 4], F32, "GI%d" % i) for i in range(2)]
        GF = [P.sb([128, 4], F32, "GF%d" % i) for i in range(2)]
        LF = P.sb([128, 4], F32, "LF")
        BC, AC, BL, WK = [P.sb([128, 4], F32, n) for n in ("BCc", "ACc", "BLc", "WKc")]
        LFB = [P.sb([128, 128], F32, "LFB%d" % i) for i in range(2)]
        BBS = P.sb([128, 4, 128], F32, "BBS")
        EBC = P.sb([128, 4, 128], F32, "EBC")
        E0 = [P.sb([128, 128], F32, "E0_%d" % i) for i in range(2)]
        E1 = [P.sb([128, 128], F32, "E1_%d" % i) for i in range(2)]
        Q0 = [P.sb([128, 128], BF16, "Q0_%d" % i) for i in range(2)]
        Q1 = [P.sb([128, 128], BF16, "Q1_%d" % i) for i in range(2)]
        WTT = [P.sb([128, 128], F32, "WTT%d" % i) for i in range(2)]
        SB = [P.sb([128, 128], BF16, "SB%d" % i) for i in range(2)]
        KW = [P.sb([128, 128], BF16, "KW%d" % i) for i in range(2)]
        DN = [P.sb([128, 1], F32, "DN%d" % i) for i in range(2)]
        S1 = [P.sb([128, 1], F32, "S1_%d" % i) for i in range(2)]
        S2 = [P.sb([128, 1], F32, "S2_%d" % i) for i in range(2)]
        OG = [P.sb([128, 256], F32, "OG%d" % i) for i in range(2)]
        HG = [P.sb([128, 256], F32, "HG%d" % i) for i in range(2)]
        SQ = [P.sb([128, 256], F32, "SQ%d" % i) for i in range(2)]
        OUTT = [P.sb([128, 1024], F32, "OUTT%d" % i) for i in range(2)]
        p_bb = P.ps([128, 512], F32, "p_bb")
        p_st = P.ps([128, 512], F32, "p_st")
        p_kt = P.ps([128, 512], BF16, "p_kt")
        p_sm = P.ps([128, 512], F32, "p_sm")
        p_c = [P.ps([128, 512], F32, "p_c%d" % i) for i in range(2)]
        p_o = [P.ps([128, 512], F32, "p_o%d" % i) for i in range(2)]
        OUT = Tl(None, "out")
        pcs = [0]

        def do_tile(i):
            t0 = i * ML_T
            qr, qp = QKR[i % 2], QKR[(i + 1) % 2]
            qc = QKC[i % 2]
            va, ot, gi, gf = VA[i % 2], OT[i % 2], GI[i % 2], GF[i % 2]
            P.dma("sp", qr, qr.ap[:, :, 3:3 + ML_T], None, qkT[:, :, t0:t0 + ML_T].rearrange("g p t -> p g t"))
            P.dma("pool", va, va.ap[:, :, 0:256], None, v_d[t0:t0 + ML_T, :, :])
            P.dma("sp", ot, ot.ap[:], None, o_d[t0:t0 + ML_T, :])
            P.dma("sp", gi, gi.ap[:], None, gi_d[t0:t0 + ML_T, :])
            P.dma("sp", gf, gf.ap[:], None, gf_d[t0:t0 + ML_T, :])
            A("pool", lambda e, o=qr.ap[:, :, 0:3], s_=qp.ap[:, :, ML_T:ML_T + 3]: e.tensor_copy(o, s_), [qp], [qr])
            for g in range(8):
                ca = cacc[g % 2]
                A("dve", lambda e, g=g, o=ca.ap[:]: e.tensor_scalar(o, qr.ap[:, g, 0:ML_T], WCV.ap[:, g, 0:1], BCV.ap[:, g:g + 1], ALU.mult, ALU.add), [qr, WCV, BCV], [ca])
                for j in range(1, 4):
                    A("dve", lambda e, g=g, j=j, o=ca.ap[:]: e.scalar_tensor_tensor(o, qr.ap[:, g, j:j + ML_T], WCV.ap[:, g, j:j + 1], o, ALU.mult, ALU.add), [qr, WCV, ca], [ca])
                A("act", lambda e, g=g, i_=ca.ap[:]: e.activation(qc.ap[:, g, :], i_, AF.Silu), [ca], [qc])
            A("act", lambda e: e.activation(LF.ap[:], gf.ap[:], AF.Exp, scale=-1.0), [gf], [LF])
            A("act", lambda e: e.activation(LF.ap[:], LF.ap[:], AF.Ln, bias=1.0), [LF], [LF])
            A("dve", lambda e: e.tensor_scalar(LF.ap[:], LF.ap[:], -1.0, None, ALU.mult), [LF], [LF])
            A("pe", lambda e: e.matmul(p_sm.ap[:, 0:4], TRI.ap[:], LF.ap[:], start=True, stop=True), [TRI, LF], [p_sm])
            A("pe", lambda e: e.matmul(p_sm.ap[:, 4:8], BLK.ap[:], LF.ap[:], start=True, stop=True), [BLK, LF], [p_sm])
            A("dve", lambda e: e.tensor_copy(BC.ap[:], p_sm.ap[:, 0:4]), [p_sm], [BC])
            A("dve", lambda e: e.tensor_tensor(AC.ap[:], gi.ap[:], BC.ap[:], ALU.subtract), [gi, BC], [AC])
            A("dve", lambda e: e.tensor_tensor(BL.ap[:], p_sm.ap[:, 4:8], AC.ap[:], ALU.add), [p_sm, AC], [BL])
            A("act", lambda e: e.activation(WK.ap[:], BL.ap[:], AF.Exp, bias=lnk.ap[:, 0:1]), [BL, lnk], [WK])
            for h in range(4):
                lb = LFB[h % 2]
                A("dve", lambda e, h=h, o=lb.ap[:]: e.tensor_scalar(o, ONES.ap[:], LF.ap[:, h:h + 1], None, ALU.mult), [ONES, LF], [lb])
                A("pe", lambda e, h=h, l=lb.ap[:]: e.matmul(p_bb.ap[:, h * 128:(h + 1) * 128], l, TRI.ap[:], start=True, stop=True), [lb, TRI], [p_bb])
            A("act", lambda e: e.activation(BBS.ap[:], p_bb.ap[:].rearrange("p (h t) -> p h t", h=4), AF.Copy), [p_bb], [BBS])
            A("act", lambda e: e.activation(EBC.ap[:], BBS.ap[:], AF.Exp), [BBS], [EBC])
            for h in range(4):
                do_head(i, h, qc, va, ot)
            P.dma("sp", OUT, hs_d[t0:t0 + ML_T, :], OUTT[i % 2], OUTT[i % 2].ap[:])

        def do_head(i, h, qc, va, ot):
            if True:
                qh = qc.ap[:, h, :]
                kh = qc.ap[:, 4 + h, :]
                A("pe", lambda e, h=h, kh=kh, qh=qh: e.matmul(p_st.ap[:, h * 128:(h + 1) * 128], kh, qh, start=True, stop=True), [qc], [p_st])
                wt = WTT[h % 2]
                A("act", lambda e, h=h, o=wt.ap[:]: e.activation(o, BBS.ap[:, h, :], AF.Exp, bias=AC.ap[:, h:h + 1]), [BBS, AC], [wt])
                A("pool", lambda e, o=wt.ap[:]: e.tensor_tensor(o, o, MKC.ap[:], ALU.mult), [wt, MKC], [wt])
                sb = SB[h % 2]
                A("dve", lambda e, h=h, o=sb.ap[:], w_=wt.ap[:]: e.tensor_tensor(o, p_st.ap[:, h * 128:(h + 1) * 128], w_, ALU.mult), [p_st, wt], [sb])
                A("pe", lambda e, h=h, kh=kh: e.transpose(p_kt.ap[:, h * 128:(h + 1) * 128], kh, IDB.ap[:]), [qc, IDB], [p_kt])
                kw = KW[h % 2]
                A("act", lambda e, h=h, o=kw.ap[:]: e.activation(o, p_kt.ap[:, h * 128:(h + 1) * 128], AF.Copy, scale=WK.ap[:, h:h + 1]), [p_kt, WK], [kw])
                e0, e1, q0, q1 = E0[h % 2], E1[h % 2], Q0[h % 2], Q1[h % 2]
                A("pool", lambda e, h=h, o=e0.ap[:]: e.tensor_tensor(o, EBC.ap[:, h, :], CM0.ap[:], ALU.mult), [EBC, CM0], [e0])
                A("pool", lambda e, h=h, o=e1.ap[:]: e.tensor_tensor(o, EBC.ap[:, h, :], CM1.ap[:], ALU.mult), [EBC, CM1], [e1])
                A("dve", lambda e, o=q0.ap[:], qh=qh, e_=e0.ap[:]: e.tensor_tensor(o, qh, e_, ALU.mult), [qc, e0], [q0])
                A("dve", lambda e, o=q1.ap[:], qh=qh, e_=e1.ap[:]: e.tensor_tensor(o, qh, e_, ALU.mult), [qc, e1], [q1])
                po = p_o[h % 2]
                A("pe", lambda e, h=h, o=po.ap[:, 0:257], l=sb.ap[:]: e.matmul(o, l, va.ap[:, h, :], start=True, stop=False), [sb, va], [po])
                A("pe", lambda e, h=h, o=po.ap[:, 0:257], l=q0.ap[:]: e.matmul(o, l, CA[h].ap[:], start=False, stop=False), [q0, CA[h]], [po])
                for cc in range(2):
                    pc = p_c[pcs[0] % 2]
                    pcs[0] += 1
                    A("pe", lambda e, h=h, cc=cc, o=pc.ap[:, 0:257], l=kw.ap[64 * cc:64 * cc + 64, :]: e.matmul(o, l, va.ap[64 * cc:64 * cc + 64, h, :], start=True, stop=True), [kw, va], [pc])
                    A("dve", lambda e, h=h, cc=cc, p_=pc.ap[:, 0:257]: e.scalar_tensor_tensor(CF[h].ap[:], CF[h].ap[:], EBC.ap[:, h, 64 * cc + 63:64 * cc + 64], p_, ALU.mult, ALU.add), [CF[h], EBC, pc], [CF[h]])
                    if cc == 0:
                        A("act", lambda e, h=h: e.activation(CAm[h].ap[:], CF[h].ap[:], AF.Copy), [CF[h]], [CAm[h]])
                        A("pe", lambda e, h=h, o=po.ap[:, 0:257], l=q1.ap[:]: e.matmul(o, l, CAm[h].ap[:], start=False, stop=True), [q1, CAm[h]], [po])
                    else:
                        A("act", lambda e, h=h: e.activation(CA[h].ap[:], CF[h].ap[:], AF.Copy), [CF[h]], [CA[h]])
                dn, s1, s2, og, hg, sq = DN[h % 2], S1[h % 2], S2[h % 2], OG[h % 2], HG[h % 2], SQ[h % 2]
                A("act", lambda e, o=dn.ap[:], p_=po.ap[:, 256:257]: e.activation(o, p_, AF.Abs), [po], [dn])
                A("dve", lambda e, o=dn.ap[:]: e.tensor_scalar(o, o, 1.0, None, ALU.max), [dn], [dn])
                A("dve", lambda e, o=dn.ap[:]: e.reciprocal(o, o), [dn], [dn])
                A("act", lambda e, h=h, o=og.ap[:]: e.activation(o, ot.ap[:, h * 256:(h + 1) * 256], AF.Sigmoid), [ot], [og])
                A("dve", lambda e, o=hg.ap[:], p_=po.ap[:, 0:256], d_=dn.ap[:, 0:1], g_=og.ap[:]: e.scalar_tensor_tensor(o, p_, d_, g_, ALU.mult, ALU.mult), [po, dn, og], [hg])
                A("dve", lambda e, o=s1.ap[:], i_=hg.ap[:]: e.tensor_reduce(o, i_, mybir.AxisListType.X, ALU.add), [hg], [s1])
                A("dve", lambda e, o=s1.ap[:]: e.tensor_scalar(o, o, -1.0 / 256.0, None, ALU.mult), [s1], [s1])
                A("pool", lambda e, o=hg.ap[:], c_=s1.ap[:, 0:1]: e.tensor_scalar(o, o, c_, None, ALU.add), [hg, s1], [hg])
                A("act", lambda e, o=sq.ap[:], i_=hg.ap[:]: e.activation(o, i_, AF.Square), [hg], [sq])
                A("dve", lambda e, o=s2.ap[:], i_=sq.ap[:]: e.tensor_reduce(o, i_, mybir.AxisListType.X, ALU.add), [sq], [s2])
                A("dve", lambda e, o=s2.ap[:]: e.tensor_scalar(o, o, 1.0 / 256.0, LN_EPS, ALU.mult, ALU.add), [s2], [s2])
                A("act", lambda e, o=s2.ap[:]: e.activation(o, o, AF.Sqrt), [s2], [s2])
                A("dve", lambda e, o=s2.ap[:]: e.reciprocal(o, o), [s2], [s2])
                outt = OUTT[i % 2]
                A("dve", lambda e, h=h, o=outt.ap[:, h * 256:(h + 1) * 256], i_=hg.ap[:], c_=s2.ap[:, 0:1]: e.scalar_tensor_tensor(o, i_, c_, MHG.ap[:, h * 256:(h + 1) * 256], ALU.mult, ALU.mult), [hg, s2, MHG], [outt])
        for i in range(NTL):
            do_tile(i)
        P.finish([OUT])
        P.emit_all()
    return nc


def run_mlstm(inp, z):
    nc = build_mlstm()
    wc, bc, mhg = inp["c_w_conv"][0], inp["c_b_conv"][0], inp["c_mh_g"][0]
    ii = np.arange(128)
    same = (ii[:, None] // 64) == (ii[None, :] // 64)
    tri = (same & (ii[:, None] <= ii[None, :])).astype(np.float32)
    consts = {"tri": tri, "blk": same.astype(np.float32), "mkc": tri * np.float32(DKS),
              "cm0": np.tile((ii < 64).astype(np.float32)[None, :], (128, 1)),
              "cm1": np.tile((ii >= 64).astype(np.float32)[None, :], (128, 1)),
              "idn": np.eye(128, dtype=np.float32)}
    maps = []
    for core in range(NCORE):
        b, hh = core // 2, core % 2
        heads = [4 * hh + h for h in range(4)]
        chans = [np.arange(128 * h_, 128 * h_ + 128) for h_ in heads] + [np.arange(1024 + 128 * h_, 1024 + 128 * h_ + 128) for h_ in heads]
        qk = np.stack([z[b][:, ch].T for ch in chans], 0)
        wcv = np.stack([wc[:, ch].T for ch in chans], 1)
        bcv = np.stack([bc[ch] for ch in chans], 1)
        v = z[b][:, 2048 + 1024 * hh:2048 + 1024 * (hh + 1)].reshape(SEQ, 4, 256)
        o = z[b][:, 4096 + 1024 * hh:4096 + 1024 * (hh + 1)]
        gi = z[b][:, 6144 + 4 * hh:6144 + 4 * hh + 4]
        gf = z[b][:, 6152 + 4 * hh:6152 + 4 * hh + 4]
        m = {"qkT": qk, "wcv": wcv, "bcv": bcv, "v_tok": v, "o_tok": o, "gi_tok": gi, "gf_tok": gf,
             "mhg": np.tile(mhg[1024 * hh:1024 * (hh + 1)][None, :], (128, 1))}
        m.update(consts)
        maps.append({k: np.ascontiguousarray(v_, dtype=np.float32) for k, v_ in m.items()})
    res = _run(nc, maps)
    hs = np.empty((4, SEQ, D), np.float32)
    for core in range(NCORE):
        b, hh = core // 2, core % 2
        hs[b, :, 1024 * hh:1024 * (hh + 1)] = res[core]["hs"]
    return hs


def _dbg(name, arr):
    import os
    d = os.environ.get("KDEBUG_DIR")
    if d:
        np.save(os.path.join(d, name + ".npy"), arr[0])


def kernel(**inputs):
    inp = {k: np.asarray(v) for k, v in inputs.items()}
    inp["_mod"] = run_mod(inp)
    x = np.asarray(inp["x"], np.float32)
    x = run_layer0(inp, x)
    _dbg("x0", x)
    u = run_head(inp, x, 1, inp["b_w_in"][0], inp["b_b_in"][0])
    y = run_s5core(inp, u)
    _dbg("y1", y)
    del u
    x1, hf, gm = run_tail(inp, x, y, 1, inp["b_w_glu"][0], inp["b_b_glu"][0], True, True)
    del y, x
    ya, yb = run_experts(inp, 0, hf, gm)
    x = run_combine(inp, 1, x1, ya, yb)
    _dbg("x1", x)
    del x1, hf, ya, yb
    z = run_head(inp, x, 2, inp["c_w_in"][0], inp["c_b_in"][0])
    hs = run_mlstm(inp, z)
    _dbg("hs2", hs)
    del z
    x = run_tail(inp, x, hs, 2, inp["c_w_out"][0], inp["c_b_out"][0], False, False)
    _dbg("x2", x)
    del hs
    x1, hf, gm = run_tail(inp, x, None, 3, inp["d_w_out"][0], inp["d_b_out"][0], False, True, gmlp=True)
    del x
    ya, yb = run_experts(inp, 1, hf, gm)
    x = run_combine(inp, 3, x1, ya, yb)
    return np.ascontiguousarray(x, dtype=np.float32)


MODW = 6 * D // NCORE


def build_mod():
    nc = bass.Bass("TRN2", target_bir_lowering=False)
    dr = lambda name, shape: nc.dram_tensor(name, list(shape), F32, kind="ExternalInput").ap()
    c_d = dr("c_all", [128, KC, 4])
    aw = dr("adw", [4 * D, MODW])
    ab = dr("adb", [4, 4, MODW])
    out = nc.dram_tensor("modr", [4, 4, MODW], F32, kind="ExternalOutput").ap()
    with ExitStack() as es:
        P = Prog(nc, es)
        C = Ctx(P)
        craw = P.sb([128, KC, 4], F32, "craw")
        cb = P.sb([128, KC, 4], BF16, "cbf")
        P.dma("sp", craw, craw.ap[:], None, c_d)
        P.add("act", lambda e: e.activation(cb.ap[:], craw.ap[:], AF.Silu), [craw], [cb])
        bt = [P.sb([4, MODW], F32, "bt%d" % l) for l in range(4)]
        ot = [P.sb([4, 512], F32, "ot%d" % i) for i in range(2)]
        OUT = Tl(None, "out")
        i = 0
        for l in range(4):
            P.dma("sp", bt[l], bt[l].ap[:], None, ab[l])
            wv = aw[l * D:(l + 1) * D, :].rearrange("(k p) n -> p k n", p=128)
            for s_ in range(MODW // 512):
                slab = C.next_slab()
                sv = slab.ap[:, 0:KC * 512].rearrange("p (k n) -> p k n", k=KC)
                P.dma("pool", slab, sv, None, wv[:, :, s_ * 512:(s_ + 1) * 512])
                ps = C.next_ps()
                for k in range(KC):
                    P.add("pe", (lambda e, o=ps.ap[0:4, :], l_=cb.ap[:, k, :], r=sv[:, k, :], st=(k == 0), sp=(k == KC - 1):
                                 e.matmul(o, l_, r, start=st, stop=sp)), [slab, cb], [ps])
                o_ = ot[i % 2]
                i += 1
                P.add("dve", (lambda e, o=o_.ap[:], a=ps.ap[0:4, :], b_=bt[l].ap[:, s_ * 512:(s_ + 1) * 512]: e.tensor_tensor(o, a, b_, ALU.add)), [ps, bt[l]], [o_])
                P.dma("sp", OUT, out[l, :, s_ * 512:(s_ + 1) * 512], o_, o_.ap[:])
        P.finish([OUT])
        P.emit_all()
    return nc


def run_mod(inp):
    nc = build_mod()
    c_all = np.stack([cols(inp["c"][b]) for b in range(4)], -1)
    maps = []
    for core in range(NCORE):
        cs = slice(core * MODW, (core + 1) * MODW)
        m = {"c_all": c_all, "adw": inp["ada_w"][:, :, cs].reshape(4 * D, MODW),
             "adb": np.tile(inp["ada_b"][:, None, cs], (1, 4, 1))}
        maps.append({k: np.ascontiguousarray(v, dtype=np.float32) for k, v in m.items()})
    res = _run(nc, maps)
    mod = np.empty((4, 4, 6 * D), np.float32)
    for core in range(NCORE):
        mod[:, :, core * MODW:(core + 1) * MODW] = res[core]["modr"]
    return mod


def build_expert(ntile):
    n = ntile * NT
    nc = bass.Bass("TRN2", target_bir_lowering=False)
    dr = lambda name, shape: nc.dram_tensor(name, list(shape), F32, kind="ExternalInput").ap()
    xT = dr("xT", [D, n])
    grow = dr("grow", [1, n])
    w1, w3, w2 = dr("w1", [D, DFF]), dr("w3", [D, DFF]), dr("w2", [DFF, D])
    yT = nc.dram_tensor("yT", [D, n], F32, kind="ExternalOutput").ap()
    with ExitStack() as es:
        P = Prog(nc, es)
        C = Ctx(P)
        X = [P.sb([128, NT], F32, "X%d" % k) for k in range(KC)]
        HB = [P.sb([128, NT], BF16, "HB%d" % k) for k in range(KC)]
        G = [P.sb([128, NT], BF16, "G%d" % m) for m in range(MC_FF)]
        onec = P.sb([128, KC], F32, "onec")
        P.add("dve", lambda e: e.memset(onec.ap[:], 1.0), [], [onec])
        gr = [P.sb([1, NT], F32, "gr%d" % i) for i in range(2)]
        OUT = Tl(None, "out")
        for it in range(ntile):
            g_ = gr[it % 2]
            P.dma("sp", g_, g_.ap[:], None, grow[:, it * NT:(it + 1) * NT])
            for k in range(KC):
                P.dma("pool", HB[k], HB[k].ap[:], None, xT[k * 128:(k + 1) * 128, it * NT:(it + 1) * NT])
                P.add("pool", (lambda e, o=X[k].ap[:]: e.memset(o, 0.0)), [], [X[k]])
            gate = C.pstat[1]
            P.add("pe", (lambda e, o=gate.ap[:], r=g_.ap[0:1, :]: e.matmul(o, C.ones.ap[0:1, :], r, start=True, stop=True)), [C.ones, g_], [gate])
            ffn_phase(C, HB, X, w1, w3, w2, G, onec, NT, gate=gate, ACC=1)
            for k in range(KC):
                P.dma("sp", OUT, yT[k * 128:(k + 1) * 128, it * NT:(it + 1) * NT], X[k], X[k].ap[:])
        P.finish([OUT])
        P.emit_all()
    return nc


def run_experts(inp, fi, hf, gm):
    hff = hf.reshape(-1, D)
    gmf = gm.reshape(-1, 8)
    idxs = [np.nonzero(gmf[:, e] != 0)[0] for e in range(8)]
    ntile = max(1, max((len(ix) + NT - 1) // NT for ix in idxs))
    n = ntile * NT
    nc = build_expert(ntile)
    maps = []
    for e in range(8):
        ix = idxs[e]
        xe = np.zeros((D, n), np.float32)
        xe[:, :len(ix)] = hff[ix].T
        ge = np.zeros((1, n), np.float32)
        ge[0, :len(ix)] = gmf[ix, e]
        maps.append({"xT": xe, "grow": ge, "w1": np.ascontiguousarray(inp["m_w1"][fi][e], dtype=np.float32),
                     "w3": np.ascontiguousarray(inp["m_w3"][fi][e], dtype=np.float32),
                     "w2": np.ascontiguousarray(inp["m_w2"][fi][e], dtype=np.float32)})
    res = _run(nc, maps)
    ya = np.zeros_like(hff)
    yb = np.zeros_like(hff)
    filled = np.zeros((hff.shape[0],), np.int32)
    for e in range(8):
        ix = idxs[e]
        ye = res[e]["yT"][:, :len(ix)].T
        first = filled[ix] == 0
        ya[ix[first]] = ye[first]
        yb[ix[~first]] = ye[~first]
        filled[ix] += 1
    return ya.reshape(hf.shape), yb.reshape(hf.shape)


def build_combine():
    nc = bass.Bass("TRN2", target_bir_lowering=False)
    dr = lambda name, shape: nc.dram_tensor(name, list(shape), F32, kind="ExternalInput").ap()
    x1T, yaT, ybT = dr("x1T", [D, TC]), dr("yaT", [D, TC]), dr("ybT", [D, TC])
    modc_d = dr("modc", [128, 6 * KC])
    ln2_g, ln2_b = dr("ln2_g", [128, KC]), dr("ln2_b", [128, KC])
    outT = nc.dram_tensor("outT", [D, TC], F32, kind="ExternalOutput").ap()
    with ExitStack() as es:
        P = Prog(nc, es)
        C = Ctx(P, nslab=1, slab_elems=64)
        sh1, sc1, g1, sh2, sc2, g2 = setup_common(P, C, modc_d)
        c_g, c_b = load_cols(P, "c_ln2g", ln2_g, KC), load_cols(P, "c_ln2b", ln2_b, KC)
        X = [P.sb([128, NT], F32, "X%d" % k) for k in range(KC)]
        YA = [P.sb([128, NT], F32, "YA%d" % k) for k in range(KC)]
        YB = [P.sb([128, NT], F32, "YB%d" % k) for k in range(KC)]
        OUT = Tl(None, "out")
        for it in range(TC // NT):
            sl = slice(it * NT, (it + 1) * NT)
            for k in range(KC):
                P.dma("sp", X[k], X[k].ap[:], None, x1T[k * 128:(k + 1) * 128, sl])
                P.dma("sp", YA[k], YA[k].ap[:], None, yaT[k * 128:(k + 1) * 128, sl])
                P.dma("sp", YB[k], YB[k].ap[:], None, ybT[k * 128:(k + 1) * 128, sl])
                P.add("pool", (lambda e, o=YA[k].ap[:], b_=YB[k].ap[:]: e.tensor_tensor(o, o, b_, ALU.add)), [YA[k], YB[k]], [YA[k]])
                P.add("act", (lambda e, o=YA[k].ap[:], k=k: e.activation(o, o, AF.Copy, scale=g2.ap[:, k:k + 1])), [YA[k], g2], [YA[k]])
                P.add("dve", (lambda e, o=X[k].ap[:], b_=YA[k].ap[:]: e.scalar_tensor_tensor(o, o, ALPHA, b_, ALU.mult, ALU.add)), [X[k], YA[k]], [X[k]])
            ln_inplace(C, X, NT, c_g, c_b)
            for k in range(KC):
                P.dma("sp", OUT, outT[k * 128:(k + 1) * 128, sl], X[k], X[k].ap[:])
        P.finish([OUT])
        P.emit_all()
    return nc


def run_combine(inp, layer, x1, ya, yb):
    nc = build_combine()
    maps = []
    for core in range(NCORE):
        m = common_maps(inp, layer, core)
        m.update({"x1T": tok_T(x1, core), "yaT": tok_T(ya, core), "ybT": tok_T(yb, core),
                  "ln2_g": cols(inp["ln2_g"][layer]), "ln2_b": cols(inp["ln2_b"][layer])})
        maps.append({k: np.ascontiguousarray(v, dtype=np.float32) for k, v in m.items()})
    res = _run(nc, maps)
    return from_T(res, "outT", D)
```

```python
import math
from contextlib import ExitStack
import numpy as np
import concourse.bass as bass
import concourse.mybir as mybir
from concourse.bass_utils import run_bass_kernel_spmd

AF = mybir.ActivationFunctionType
ALU = mybir.AluOpType
F32 = mybir.dt.float32
BF16 = mybir.dt.bfloat16

D = 2048
KC = 16
SEQ = 4096
NCORE = 8
TC = 2048
NT = 512
DFF = 5632
MC_FF = 44
ALPHA = 8.0 ** 0.25
LN_EPS = 1e-5
HALO = 32
ENGS = ["pe", "act", "dve", "pool", "sp"]


class Tl:
    __slots__ = ("ap", "name", "w", "rs", "rd", "dsem", "dcnt", "al")

    def __init__(self, ap, name):
        self.ap = ap
        self.name = name
        self.w = None
        self.rs = {}
        self.rd = []
        self.dsem = None
        self.dcnt = 0
        self.al = ()

    def __getitem__(self, idx):
        return self.ap[idx]


class Op:
    __slots__ = ("eng", "emit", "waits", "idx", "mark", "cnt", "is_dma", "dsem", "dval", "dtile", "inc")


class Prog:
    def __init__(self, nc, es):
        self.nc = nc
        self.es = es
        self.streams = {e: [] for e in ENGS}
        self.esem = {e: es.enter_context(nc.semaphore("S_" + e)) for e in ENGS}
        self.nsem = 5
        self.uid = 0
        self.wt = {}

    def sb(self, shape, dt, name):
        t = self.es.enter_context(self.nc.sbuf_tensor(name, list(shape), dt))
        return Tl(t, name)

    def ps(self, shape, dt, name):
        t = self.es.enter_context(self.nc.psum_tensor(name, list(shape), dt))
        return Tl(t, name)

    def view(self, ap, name):
        return Tl(ap, name)

    def _touch(self, tiles):
        out = []
        for t in tiles:
            out.append(t)
            out.extend(t.al)
        return out

    def add(self, eng, emit, reads=(), writes=(), dma_tile=None, inc=16):
        op = Op()
        op.inc = inc
        op.eng = eng
        op.emit = emit
        op.idx = len(self.streams[eng])
        op.mark = False
        op.cnt = 0
        op.is_dma = dma_tile is not None
        op.dtile = dma_tile
        op.dsem = None
        op.dval = 0
        reads = self._touch(reads)
        writes = self._touch(writes)
        deps = {}
        ddeps = []

        def dep(d):
            if d is None or d is op:
                return
            if d.is_dma:
                if op.is_dma and d.dtile is dma_tile:
                    return
                ddeps.append(d)
                return
            cur = deps.get(d.eng)
            if cur is None or cur.idx < d.idx:
                deps[d.eng] = d

        for t in reads:
            dep(t.w)
        for t in writes:
            dep(t.w)
            for r in t.rs.values():
                dep(r)
            for r in t.rd:
                dep(r)
        op.waits = []
        for d in ddeps:
            op.waits.append((d.dsem, d.dtile.dcnt))
        for e, d in deps.items():
            if e == eng and not op.is_dma:
                if eng == "pe":
                    continue
                if op.idx - d.idx >= 4:
                    continue
            d.mark = True
            op.waits.append(d)
        for t in reads:
            if op.is_dma:
                t.rd.append(op)
            else:
                t.rs[eng] = op
        for t in writes:
            t.w = op
            t.rs = {}
            t.rd = []
        if op.is_dma:
            if dma_tile.dsem is None:
                dma_tile.dsem = self.es.enter_context(self.nc.semaphore("D%d" % self.nsem))
                self.nsem += 1
            dma_tile.dcnt += inc
            op.dsem = dma_tile.dsem
            op.dval = dma_tile.dcnt
        self.streams[eng].append(op)
        return op

    def dma(self, q, out_t, out_ap, in_t, in_ap):
        if in_t is None:
            try:
                in_t = self.wt.get(in_ap.name)
            except Exception:
                in_t = None
        reads = [in_t] if in_t is not None else []
        writes = [out_t] if out_t is not None else []
        return self.add(q, lambda e: e.dma_start(out=out_ap, in_=in_ap), reads, writes, dma_tile=out_t)

    def gather_into(self, name, rows, cols, slot):
        nc = self.nc
        rs = rows // NCORE
        ext = nc.dram_tensor(name, [rs, cols], F32, kind="ExternalInput").ap()
        if "full" not in slot:
            slot["bnc"] = nc.dram_tensor(slot["nm"] + "_b", [rs, cols], F32)
            slot["full"] = nc.dram_tensor(slot["nm"] + "_f", [rows, cols], F32)
            slot["tb"] = Tl(None, slot["nm"] + "_b")
            slot["tf"] = Tl(None, slot["nm"] + "_f")
            self.wt[slot["full"].ap().name] = slot["tf"]
        bnc, full, tb, tf = slot["bnc"], slot["full"], slot["tb"], slot["tf"]
        step = max(1, (8 << 20) // (cols * 4))
        r = 0
        first = True
        while r < rs:
            r2 = min(rs, r + step)
            self.dma("pool", tb, bnc.ap()[r:r2, :], None, ext[r:r2, :])
            r = r2
        self.add("pool", lambda e: e.collective_compute("AllGather", ALU.bypass, replica_groups=[list(range(NCORE))],
                                                        ins=[bnc.ap()], outs=[full.ap()]),
                 [tb], [tf], dma_tile=tf, inc=1)
        return full.ap()

    def gathered(self, name, rows, cols):
        return self.nc.dram_tensor(name, [rows, cols], F32, kind="ExternalInput").ap()

    def gathered_cc(self, name, rows, cols):
        nc = self.nc
        rs = rows // NCORE
        ext = nc.dram_tensor(name, [rs, cols], F32, kind="ExternalInput").ap()
        bnc = nc.dram_tensor(name + "_b", [rs, cols], F32)
        full = nc.dram_tensor(name + "_f", [rows, cols], F32)
        tb = Tl(None, name + "_b")
        tf = Tl(None, name + "_f")
        step = max(1, (8 << 20) // (cols * 4))
        r = 0
        while r < rs:
            r2 = min(rs, r + step)
            self.dma("pool", tb, bnc.ap()[r:r2, :], None, ext[r:r2, :])
            r = r2
        self.add("pool", lambda e: e.collective_compute("AllGather", ALU.bypass, replica_groups=[list(range(NCORE))],
                                                        ins=[bnc.ap()], outs=[full.ap()]),
                 [tb], [tf], dma_tile=tf, inc=1)
        self.wt[full.ap().name] = tf
        return full.ap()

    def finish(self, tiles):
        self.add("sp", lambda e: e.nop(), reads=list(tiles), writes=[])

    def emit_all(self):
        nc = self.nc
        for e in ENGS:
            c = 0
            for op in self.streams[e]:
                if op.mark:
                    c += 1
                    op.cnt = c
        esem = self.esem
        streams = self.streams

        def run(eng_obj, e):
            waited = {}
            for op in streams[e]:
                for d in op.waits:
                    if isinstance(d, tuple):
                        sem, val = d
                    else:
                        sem, val = esem[d.eng], d.cnt
                    k = id(sem)
                    if waited.get(k, 0) >= val:
                        continue
                    eng_obj.wait_ge(sem, val)
                    waited[k] = val
                ins = op.emit(eng_obj)
                if op.is_dma:
                    ins.then_inc(op.dsem, op.inc)
                elif op.mark:
                    ins.then_inc(esem[e], 1)

        with nc.Block() as block:
            @block.tensor
            def _(x):
                run(x, "pe")

            @block.scalar
            def _(x):
                run(x, "act")

            @block.vector
            def _(x):
                run(x, "dve")

            @block.gpsimd
            def _(x):
                run(x, "pool")

            @block.sync
            def _(x):
                run(x, "sp")


class Ctx:
    def __init__(self, P, nslab=3, slab_elems=11264, ntmp=3, nsmall=0):
        self.P = P
        self.slabs = [P.sb([128, slab_elems], BF16, "slab%d" % i) for i in range(nslab)]
        self.slab_i = 0
        self.small = [P.sb([128, 4096], BF16, "sslab%d" % i) for i in range(nsmall)]
        self.small_i = 0
        self.psum = [P.ps([128, 512], F32, "ps%d" % i) for i in range(6)]
        self.ps_i = 0
        self.pstat = [P.ps([128, 512], F32, "pst%d" % i) for i in range(2)]
        self.ones = P.sb([128, 128], F32, "ones")
        P.add("dve", lambda e: e.memset(self.ones.ap[:], 1.0), [], [self.ones])
        self.tmp = [P.sb([128, 512], F32, "tmp%d" % i) for i in range(ntmp)]
        self.tmp_i = 0
        self.stat = [P.sb([128, 512], F32, "stat%d" % i) for i in range(3)]
        self.ev = 0

    def next_slab(self):
        s = self.slabs[self.slab_i % len(self.slabs)]
        self.slab_i += 1
        return s

    def next_small(self):
        if not self.small:
            return self.next_slab()
        s = self.small[self.small_i % len(self.small)]
        self.small_i += 1
        return s

    def next_ps(self):
        p = self.psum[self.ps_i % len(self.psum)]
        self.ps_i += 1
        return p

    def next_tmp(self):
        t = self.tmp[self.tmp_i % len(self.tmp)]
        self.tmp_i += 1
        return t


def linear(C, w_ap, K, col0, ncols, slabw, rhs_groups, epilogue, m_order=None):
    P = C.P
    kc = K // 128
    wv = w_ap.rearrange("(k p) n -> p k n", p=128)
    nslab = ncols // slabw
    for s in range(nslab):
        slab = C.next_slab()
        c0 = col0 + s * slabw
        sv = slab.ap[:, 0:kc * slabw].rearrange("p (k n) -> p k n", k=kc)
        P.dma("pool", slab, sv, None, wv[:, :, c0:c0 + slabw])
        for j in range(slabw // 128):
            mi = (c0 // 128) + j
            for gi, (rt, rf) in enumerate(rhs_groups):
                ps = C.next_ps()
                for k in range(kc):
                    lhs = sv[:, k, j * 128:(j + 1) * 128]
                    rhs = rf(k)
                    nn = rhs.shape[-1]
                    P.add("pe", (lambda e, o=ps.ap[:, 0:nn], l=lhs, r=rhs, st=(k == 0), sp=(k == kc - 1):
                                 e.matmul(o, l, r, start=st, stop=sp)),
                          [slab, rt[k]], [ps])
                epilogue(mi, gi, ps)


def ln_stats(C, ztiles, n, width_sel=None):
    P = C.P
    ps1, ps2 = C.pstat
    nk = len(ztiles)
    for k in range(nk):
        zt, za = ztiles[k]
        P.add("pe", (lambda e, o=ps1.ap[:, 0:n], r=za, st=(k == 0), sp=(k == nk - 1):
                     e.matmul(o, C.ones.ap[:], r, start=st, stop=sp)), [C.ones, zt], [ps1])
    for k in range(nk):
        zt, za = ztiles[k]
        sq = C.next_tmp()
        P.add("act", (lambda e, o=sq.ap[:, 0:n], i=za: e.activation(o, i, AF.Square)), [zt], [sq])
        P.add("pe", (lambda e, o=ps2.ap[:, 0:n], r=sq.ap[:, 0:n], st=(k == 0), sp=(k == nk - 1):
                     e.matmul(o, C.ones.ap[:], r, start=st, stop=sp)), [C.ones, sq], [ps2])
    mean, ex2, rstd = C.stat
    invd = 1.0 / (128.0 * nk)
    P.add("act", lambda e: e.activation(mean.ap[:, 0:n], ps1.ap[:, 0:n], AF.Copy, scale=invd), [ps1], [mean])
    P.add("act", lambda e: e.activation(ex2.ap[:, 0:n], ps2.ap[:, 0:n], AF.Copy, scale=invd), [ps2], [ex2])
    P.add("dve", lambda e: e.tensor_tensor(rstd.ap[:, 0:n], mean.ap[:, 0:n], mean.ap[:, 0:n], ALU.mult), [mean], [rstd])
    P.add("dve", lambda e: e.tensor_tensor(ex2.ap[:, 0:n], ex2.ap[:, 0:n], rstd.ap[:, 0:n], ALU.subtract), [ex2, rstd], [ex2])
    P.add("dve", lambda e: e.tensor_scalar(ex2.ap[:, 0:n], ex2.ap[:, 0:n], LN_EPS, None, ALU.add), [ex2], [ex2])
    P.add("act", lambda e: e.activation(ex2.ap[:, 0:n], ex2.ap[:, 0:n], AF.Sqrt), [ex2], [ex2])
    P.add("dve", lambda e: e.reciprocal(rstd.ap[:, 0:n], ex2.ap[:, 0:n]), [ex2], [rstd])
    return mean, rstd


def ln_apply(C, zt, za, outs, n, mean, rstd, gcol, bcol, func=AF.Identity, extra=None):
    P = C.P
    P.add("dve", lambda e: e.tensor_tensor(za, za, mean.ap[:, 0:n], ALU.subtract), [zt, mean], [zt])
    P.add("dve", lambda e: e.tensor_tensor(za, za, rstd.ap[:, 0:n], ALU.mult), [zt, rstd], [zt])
    ot, oa = outs
    P.add("act", lambda e: e.activation(oa, za, func, bias=bcol, scale=gcol), [zt], [ot])
    if extra is not None:
        xt, xa, sc, bc = extra
        P.add("act", lambda e: e.activation(xa, oa, AF.Identity, bias=bc, scale=sc), [ot], [xt])


def compute_mod(C, cvec_ap, ada_w_ap, ada_b_ap, which, modc):
    P = C.P
    craw = P.sb([128, KC], F32, "craw")
    cb = P.sb([128, KC], BF16, "cbf")
    P.dma("sp", craw, craw.ap[:], None, cvec_ap)
    P.add("act", lambda e: e.activation(cb.ap[:], craw.ap[:], AF.Silu), [craw], [cb])
    bcols = P.sb([128, 6 * KC], F32, "adab_cols")
    P.dma("sp", bcols, bcols.ap[:], None, ada_b_ap)
    one11 = P.sb([1, 1], F32, "one11")
    P.add("dve", lambda e: e.memset(one11.ap[:], 1.0), [], [one11])
    wv = ada_w_ap.rearrange("(k p) n -> p k n", p=128)
    i = 0
    for v in which:
        for s in range(4):
            slab = C.next_slab()
            c0 = v * D + s * 512
            sv = slab.ap[:, 0:KC * 512].rearrange("p (k n) -> p k n", k=KC)
            P.dma("pool", slab, sv, None, wv[:, :, c0:c0 + 512])
            ps = C.next_ps()
            for k in range(KC):
                P.add("pe", (lambda e, o=ps.ap[0:1, :], l=cb.ap[:, k:k + 1], r=sv[:, k, :], st=(k == 0), sp=(k == KC - 1):
                             e.matmul(o, l, r, start=st, stop=sp)), [slab, cb], [ps])
            mr = C.next_tmp()
            P.add("dve", (lambda e, o=mr.ap[0:1, :], a=ps.ap[0:1, :]: e.tensor_copy(o, a)), [ps], [mr])
            ps2 = C.next_ps()
            for k in range(4):
                P.add("pe", (lambda e, o=ps2.ap[:, k:k + 1], l=mr.ap[0:1, k * 128:(k + 1) * 128]:
                             e.matmul(o, l, one11.ap[0:1, 0:1], start=True, stop=True)), [mr, one11], [ps2])
            cc = v * KC + s * 4
            P.add("dve", (lambda e, o=modc.ap[:, cc:cc + 4], i_=ps2.ap[:, 0:4], b=bcols.ap[:, cc:cc + 4]:
                          e.tensor_tensor(o, i_, b, ALU.add)), [ps2, bcols], [modc])


def load_cols(P, name, ap_1d, n):
    t = P.sb([128, n], F32, name)
    P.dma("sp", t, t.ap[:], None, ap_1d)
    return t


def ffn_phase(C, HB, X, w1_ap, w3_ap, w2_ap, G, g2col, n, gate=None, ACC=None, last=True):
    P = C.P
    SW = 256
    for s in range(DFF // SW):
        sl1 = C.next_small()
        sl3 = C.next_small()
        c0 = s * SW
        v1 = sl1.ap[:, 0:KC * SW].rearrange("p (k n) -> p k n", k=KC)
        v3 = sl3.ap[:, 0:KC * SW].rearrange("p (k n) -> p k n", k=KC)
        P.dma("pool", sl1, v1, None, w1_ap.rearrange("(k p) n -> p k n", p=128)[:, :, c0:c0 + SW])
        P.dma("pool", sl3, v3, None, w3_ap.rearrange("(k p) n -> p k n", p=128)[:, :, c0:c0 + SW])
        for j in range(SW // 128):
            m = c0 // 128 + j
            p1 = C.next_ps()
            p3 = C.next_ps()
            for k in range(KC):
                P.add("pe", (lambda e, o=p1.ap[:, 0:n], l=v1[:, k, j * 128:(j + 1) * 128], r=HB[k].ap[:, 0:n], st=(k == 0), sp=(k == KC - 1):
                             e.matmul(o, l, r, start=st, stop=sp)), [sl1, HB[k]], [p1])
            for k in range(KC):
                P.add("pe", (lambda e, o=p3.ap[:, 0:n], l=v3[:, k, j * 128:(j + 1) * 128], r=HB[k].ap[:, 0:n], st=(k == 0), sp=(k == KC - 1):
                             e.matmul(o, l, r, start=st, stop=sp)), [sl3, HB[k]], [p3])
            t = C.next_tmp()
            P.add("act", (lambda e, o=t.ap[:, 0:n], i=p1.ap[:, 0:n]: e.activation(o, i, AF.Silu)), [p1], [t])
            if gate is None:
                P.add("dve", (lambda e, o=G[m].ap[:, 0:n], a=t.ap[:, 0:n], b=p3.ap[:, 0:n]: e.tensor_tensor(o, a, b, ALU.mult)), [t, p3], [G[m]])
            else:
                P.add("dve", (lambda e, o=t.ap[:, 0:n], a=t.ap[:, 0:n], b=p3.ap[:, 0:n]: e.tensor_tensor(o, a, b, ALU.mult)), [t, p3], [t])
                P.add("dve", (lambda e, o=G[m].ap[:, 0:n], a=t.ap[:, 0:n], b=gate.ap[:, 0:n]: e.tensor_tensor(o, a, b, ALU.mult)), [t, gate], [G[m]])
    SW2 = 256
    for s in range(D // SW2):
        sl = C.next_slab()
        c0 = s * SW2
        v2 = sl.ap[:, 0:MC_FF * SW2].rearrange("p (k n) -> p k n", k=MC_FF)
        P.dma("pool", sl, v2, None, w2_ap.rearrange("(k p) n -> p k n", p=128)[:, :, c0:c0 + SW2])
        for j in range(SW2 // 128):
            c = c0 // 128 + j
            ps = C.next_ps()
            for m in range(MC_FF):
                P.add("pe", (lambda e, o=ps.ap[:, 0:n], l=v2[:, m, j * 128:(j + 1) * 128], r=G[m].ap[:, 0:n], st=(m == 0), sp=(m == MC_FF - 1):
                             e.matmul(o, l, r, start=st, stop=sp)), [sl, G[m]], [ps])
            if gate is None:
                t = C.next_tmp()
                P.add("act", (lambda e, o=t.ap[:, 0:n], i=ps.ap[:, 0:n], c=c: e.activation(o, i, AF.Copy, scale=g2col.ap[:, c:c + 1])), [ps, g2col], [t])
                P.add("dve", (lambda e, o=X[c].ap[:, 0:n], a=X[c].ap[:, 0:n], b=t.ap[:, 0:n]:
                              e.scalar_tensor_tensor(o, a, ALPHA, b, ALU.mult, ALU.add)), [X[c], t], [X[c]])
            else:
                P.add("dve", (lambda e, o=X[c].ap[:, 0:n], p_=ps.ap[:, 0:n], c=c:
                              e.scalar_tensor_tensor(o, p_, g2col.ap[:, c:c + 1], o, ALU.mult, ALU.add)), [X[c], ps, g2col], [X[c]])


def ln_inplace(C, X, n, gcol, bcol, HB=None, sccol=None, shcol=None):
    mean, rstd = ln_stats(C, [(X[c], X[c].ap[:, 0:n]) for c in range(KC)], n)
    for c in range(KC):
        extra = None
        if HB is not None:
            extra = (HB[c], HB[c].ap[:, 0:n], sccol.ap[:, c:c + 1], shcol.ap[:, c:c + 1])
        ln_apply(C, X[c], X[c].ap[:, 0:n], (X[c], X[c].ap[:, 0:n]), n, mean, rstd,
                 gcol.ap[:, c:c + 1], bcol.ap[:, c:c + 1], AF.Identity, extra)


def build_layer0():
    nc = bass.Bass("TRN2", target_bir_lowering=False)
    dr = lambda name, shape: nc.dram_tensor(name, list(shape), F32, kind="ExternalInput").ap()
    xT = dr("xT", [D, HALO + TC])
    modc_d = dr("modc", [128, 6 * KC])
    hmask = dr("hmask", [128, 1])
    CV = [128, KC]
    ln1_g, ln1_b, ln2_g, ln2_b = dr("ln1_g", CV), dr("ln1_b", CV), dr("ln2_g", CV), dr("ln2_b", CV)
    b_in = dr("a_b_in", [128, 2 * KC])
    w_dw, b_dw = dr("a_w_dw", [128, KC, 31]), dr("a_b_dw", CV)
    aln_g, aln_b = dr("a_ln_g", CV), dr("a_ln_b", CV)
    b_out = dr("a_b_out", CV)
    outT = nc.dram_tensor("outT", [D, TC], F32, kind="ExternalOutput").ap()
    with ExitStack() as es:
        P = Prog(nc, es)
        w_in = P.gathered("a_w_in", D, 2 * D)
        w_out = P.gathered("a_w_out", D, D)
        w1, w3, w2 = P.gathered("f_w1", D, DFF), P.gathered("f_w3", D, DFF), P.gathered("f_w2", DFF, D)
        C = Ctx(P)
        sh1, sc1, g1, sh2, sc2, g2 = setup_common(P, C, modc_d)
        sc1p = P.sb([128, KC], F32, "sc1p")
        sc2p = P.sb([128, KC], F32, "sc2p")
        P.add("dve", lambda e: e.tensor_scalar(sc1p.ap[:], sc1.ap[:], 1.0, None, ALU.add), [sc1], [sc1p])
        P.add("dve", lambda e: e.tensor_scalar(sc2p.ap[:], sc2.ap[:], 1.0, None, ALU.add), [sc2], [sc2p])
        c_ln1g, c_ln1b = load_cols(P, "c_ln1g", ln1_g, KC), load_cols(P, "c_ln1b", ln1_b, KC)
        c_ln2g, c_ln2b = load_cols(P, "c_ln2g", ln2_g, KC), load_cols(P, "c_ln2b", ln2_b, KC)
        c_bin = load_cols(P, "c_bin", b_in, 2 * KC)
        c_bdw = load_cols(P, "c_bdw", b_dw, KC)
        c_alng, c_alnb = load_cols(P, "c_alng", aln_g, KC), load_cols(P, "c_alnb", aln_b, KC)
        c_bout = load_cols(P, "c_bout", b_out, KC)
        c_wdw = P.sb([128, KC, 31], F32, "c_wdw")
        P.dma("sp", c_wdw, c_wdw.ap[:], None, w_dw)
        c_mask = P.sb([128, 1], F32, "c_mask")
        P.dma("sp", c_mask, c_mask.ap[:], None, hmask)
        g1b = P.sb([128, KC], F32, "g1b")
        P.add("dve", lambda e: e.tensor_tensor(g1b.ap[:], g1.ap[:], c_bout.ap[:], ALU.mult), [g1, c_bout], [g1b])

        X = [P.sb([128, NT], F32, "X%d" % k) for k in range(KC)]
        XH = [P.sb([128, HALO], F32, "XH%d" % k) for k in range(KC)]
        HB = [P.sb([128, NT], BF16, "HB%d" % k) for k in range(KC)]
        HBH = [P.sb([128, HALO], BF16, "HBH%d" % k) for k in range(KC)]
        arena = P.sb([128, 16 * (HALO + NT) + 16 * NT], F32, "arena")
        Pp, U = [], []
        for k in range(KC):
            t = Tl(arena.ap[:, k * (HALO + NT):(k + 1) * (HALO + NT)], "Pp%d" % k)
            Pp.append(t)
        off = KC * (HALO + NT)
        for k in range(KC):
            t = Tl(arena.ap[:, off + k * NT:off + (k + 1) * NT], "U%d" % k)
            U.append(t)
        gview = arena.ap[:, 0:MC_FF * NT // 2].bitcast(BF16)
        G = []
        for m in range(MC_FF):
            t = Tl(gview[:, m * NT:(m + 1) * NT], "G%d" % m)
            G.append(t)
        spans = [(Pp[k], k * (HALO + NT), (k + 1) * (HALO + NT)) for k in range(KC)] + \
                [(U[k], off + k * NT, off + (k + 1) * NT) for k in range(KC)]
        for m in range(MC_FF):
            lo, hi = m * NT // 2, (m + 1) * NT // 2
            al = [t for (t, a, b) in spans if a < hi and lo < b]
            G[m].al = tuple(al)
            for t in al:
                t.al = tuple(list(t.al) + [G[m]])
        HALOS = [P.sb([128, HALO], F32, "HL%d" % k) for k in range(KC)]

        ntile = TC // NT
        for it in range(ntile):
            t0 = HALO + it * NT
            for k in range(KC):
                P.dma("sp", X[k], X[k].ap[:], None, xT[k * 128:(k + 1) * 128, t0:t0 + NT])
                P.add("act", (lambda e, o=HB[k].ap[:], i=X[k].ap[:], k=k:
                              e.activation(o, i, AF.Identity, bias=sh1.ap[:, k:k + 1], scale=sc1p.ap[:, k:k + 1])), [X[k], sh1, sc1p], [HB[k]])
            groups = [(HB, lambda k: HB[k].ap[:])]
            if it == 0:
                for k in range(KC):
                    P.dma("sp", XH[k], XH[k].ap[:], None, xT[k * 128:(k + 1) * 128, 0:HALO])
                    P.add("act", (lambda e, o=HBH[k].ap[:], i=XH[k].ap[:], k=k:
                                  e.activation(o, i, AF.Identity, bias=sh1.ap[:, k:k + 1], scale=sc1p.ap[:, k:k + 1])), [XH[k], sh1, sc1p], [HBH[k]])
                groups.append((HBH, lambda k: HBH[k].ap[:]))
            else:
                for k in range(KC):
                    P.add("pool", (lambda e, o=Pp[k].ap[:, 0:HALO], i=HALOS[k].ap[:]: e.tensor_copy(o, i)), [HALOS[k]], [Pp[k]])
            wv = w_in.rearrange("(k p) n -> p k n", p=128)
            for s in range(4):
                sa = C.next_slab()
                sg = C.next_slab()
                va = sa.ap[:, 0:KC * 512].rearrange("p (k n) -> p k n", k=KC)
                vg = sg.ap[:, 0:KC * 512].rearrange("p (k n) -> p k n", k=KC)
                P.dma("pool", sa, va, None, wv[:, :, s * 512:(s + 1) * 512])
                P.dma("pool", sg, vg, None, wv[:, :, D + s * 512:D + (s + 1) * 512])
                for j in range(4):
                    m = s * 4 + j
                    for gi, (rt, rf) in enumerate(groups):
                        nn = NT if gi == 0 else HALO
                        pa = C.next_ps()
                        pg = C.next_ps()
                        for k in range(KC):
                            P.add("pe", (lambda e, o=pa.ap[:, 0:nn], l=va[:, k, j * 128:(j + 1) * 128], r=rf(k), st=(k == 0), sp=(k == KC - 1):
                                         e.matmul(o, l, r, start=st, stop=sp)), [sa, rt[k]], [pa])
                        for k in range(KC):
                            P.add("pe", (lambda e, o=pg.ap[:, 0:nn], l=vg[:, k, j * 128:(j + 1) * 128], r=rf(k), st=(k == 0), sp=(k == KC - 1):
                                         e.matmul(o, l, r, start=st, stop=sp)), [sg, rt[k]], [pg])
                        t = C.next_tmp()
                        P.add("act", (lambda e, o=t.ap[:, 0:nn], i=pg.ap[:, 0:nn], m=m:
                                      e.activation(o, i, AF.Sigmoid, bias=c_bin.ap[:, KC + m:KC + m + 1])), [pg, c_bin], [t])
                        dst = Pp[m].ap[:, HALO:] if gi == 0 else Pp[m].ap[:, 0:HALO]
                        P.add("dve", (lambda e, o=dst, a=pa.ap[:, 0:nn], b=t.ap[:, 0:nn], m=m:
                                      e.scalar_tensor_tensor(o, a, c_bin.ap[:, m:m + 1], b, ALU.add, ALU.mult)), [pa, t, c_bin], [Pp[m]])
                        if gi == 1:
                            P.add("dve", (lambda e, o=dst: e.tensor_scalar(o, o, c_mask.ap[:, 0:1], None, ALU.mult)), [Pp[m], c_mask], [Pp[m]])
            for m in range(KC):
                eng = "dve"
                P.add(eng, (lambda e, o=U[m].ap[:], i=Pp[m].ap[:, 2:2 + NT], m=m:
                            e.tensor_scalar(o, i, c_wdw.ap[:, m, 0:1], c_bdw.ap[:, m:m + 1], ALU.mult, ALU.add)), [Pp[m], c_wdw, c_bdw], [U[m]])
                for j in range(1, 31):
                    P.add(eng, (lambda e, o=U[m].ap[:], i=Pp[m].ap[:, 2 + j:2 + j + NT], m=m, j=j:
                                e.scalar_tensor_tensor(o, i, c_wdw.ap[:, m, j:j + 1], o, ALU.mult, ALU.add)), [Pp[m], U[m], c_wdw], [U[m]])
                P.add("pool", (lambda e, o=HALOS[m].ap[:], i=Pp[m].ap[:, NT:NT + HALO]: e.tensor_copy(o, i)), [Pp[m]], [HALOS[m]])
            mean, rstd = ln_stats(C, [(U[m], U[m].ap[:]) for m in range(KC)], NT)
            for m in range(KC):
                ln_apply(C, U[m], U[m].ap[:], (HB[m], HB[m].ap[:]), NT, mean, rstd,
                         c_alng.ap[:, m:m + 1], c_alnb.ap[:, m:m + 1], AF.Silu)
            def epi_out(mi, gi, ps):
                t = C.next_tmp()
                P.add("act", (lambda e, o=t.ap[:], i=ps.ap[:]: e.activation(o, i, AF.Identity, bias=g1b.ap[:, mi:mi + 1], scale=g1.ap[:, mi:mi + 1])), [ps, g1b, g1], [t])
                P.add("dve", (lambda e, o=X[mi].ap[:], b=t.ap[:]: e.scalar_tensor_tensor(o, o, ALPHA, b, ALU.mult, ALU.add)), [X[mi], t], [X[mi]])
            linear(C, w_out, D, 0, D, 512, [(HB, lambda k: HB[k].ap[:])], epi_out)
            ln_inplace(C, X, NT, c_ln1g, c_ln1b, HB, sc2p, sh2)
            ffn_phase(C, HB, X, w1, w3, w2, G, g2, NT)
            ln_inplace(C, X, NT, c_ln2g, c_ln2b)
            outt = Tl(None, "out")
            if it == 0:
                OUT = outt
            for k in range(KC):
                P.dma("sp", OUT, outT[k * 128:(k + 1) * 128, it * NT:(it + 1) * NT], X[k], X[k].ap[:])
        P.finish([OUT])
        P.emit_all()
    return nc


def cols(v):
    v = np.asarray(v, np.float32).reshape(-1)
    return np.ascontiguousarray(v.reshape(-1, 128).T)


def shard_rows(w2d, core):
    w2d = np.asarray(w2d, np.float32)
    return w2d.reshape(-1, w2d.shape[-1])


def _run(nc, in_maps):
    res = run_bass_kernel_spmd(nc, in_maps, core_ids=list(range(NCORE)))
    return res.results


def run_layer0(inp, x):
    nc = build_layer0()
    in_maps = []
    for core in range(NCORE):
        b, h = core // 2, core % 2
        xT = np.zeros((D, HALO + TC), np.float32)
        t0 = h * TC
        xT[:, HALO:] = x[b, t0:t0 + TC, :].T
        if h == 1:
            xT[:, :HALO] = x[b, t0 - HALO:t0, :].T
        m = {
            "xT": xT, "modc": cols(inp["_mod"][0][b]),
            "hmask": np.full((128, 1), float(h), np.float32),
            "ln1_g": cols(inp["ln1_g"][0]), "ln1_b": cols(inp["ln1_b"][0]), "ln2_g": cols(inp["ln2_g"][0]), "ln2_b": cols(inp["ln2_b"][0]),
            "a_w_in": shard_rows(inp["a_w_in"][0], core), "a_b_in": cols(inp["a_b_in"][0]),
            "a_w_dw": inp["a_w_dw"][0].T.reshape(KC, 128, 31).transpose(1, 0, 2), "a_b_dw": cols(inp["a_b_dw"][0]),
            "a_ln_g": cols(inp["a_ln_g"][0]), "a_ln_b": cols(inp["a_ln_b"][0]), "a_w_out": shard_rows(inp["a_w_out"][0], core), "a_b_out": cols(inp["a_b_out"][0]),
            "f_w1": shard_rows(inp["f_w1"][0], core), "f_w3": shard_rows(inp["f_w3"][0], core), "f_w2": shard_rows(inp["f_w2"][0], core),
        }
        in_maps.append({k: np.ascontiguousarray(v, dtype=np.float32) for k, v in m.items()})
    res = _run(nc, in_maps)
    out = np.empty_like(x)
    for core in range(NCORE):
        b, h = core // 2, core % 2
        out[b, h * TC:(h + 1) * TC, :] = res[core]["outT"].T
    return out


def setup_common(P, C, modc_d):
    modc = P.sb([128, 6 * KC], F32, "modc_sb")
    P.dma("sp", modc, modc.ap[:], None, modc_d)
    vs = [Tl(modc.ap[:, v * KC:(v + 1) * KC], "m%d" % v) for v in range(6)]
    for t in vs:
        t.al = (modc,)
    return vs


def plus_one(P, t, name):
    o = P.sb([128, KC], F32, name)
    P.add("dve", lambda e: e.tensor_scalar(o.ap[:], t.ap[:], 1.0, None, ALU.add), [t], [o])
    return o


def build_head(ncols, layer_tag):
    nc = bass.Bass("TRN2", target_bir_lowering=False)
    dr = lambda name, shape: nc.dram_tensor(name, list(shape), F32, kind="ExternalInput").ap()
    xT = dr("xT", [D, TC])
    modc_d = dr("modc", [128, 6 * KC])
    nm = (ncols + 127) // 128
    ncp = nm * 128
    b = dr("b", [128, nm])
    outT = nc.dram_tensor("outT", [ncp, TC], F32, kind="ExternalOutput").ap()
    with ExitStack() as es:
        P = Prog(nc, es)
        w = P.gathered("w", D, ncp)
        C = Ctx(P)
        sh1, sc1, g1, sh2, sc2, g2 = setup_common(P, C, modc_d)
        sc1p = plus_one(P, sc1, "sc1p")
        c_b = load_cols(P, "c_b", b, nm)
        X = [P.sb([128, NT], F32, "X%d" % k) for k in range(KC)]
        HB = [P.sb([128, NT], BF16, "HB%d" % k) for k in range(KC)]
        O = [P.sb([128, NT], F32, "O%d" % k) for k in range(4)]
        OUT = Tl(None, "out")
        oi = [0]
        for it in range(TC // NT):
            for k in range(KC):
                P.dma("sp", X[k], X[k].ap[:], None, xT[k * 128:(k + 1) * 128, it * NT:(it + 1) * NT])
                P.add("act", (lambda e, o=HB[k].ap[:], i=X[k].ap[:], k=k:
                              e.activation(o, i, AF.Identity, bias=sh1.ap[:, k:k + 1], scale=sc1p.ap[:, k:k + 1])), [X[k], sh1, sc1p], [HB[k]])

            def epi(mi, gi, ps):
                o = O[oi[0] % 4]
                oi[0] += 1
                P.add("act", (lambda e, o_=o.ap[:], i=ps.ap[:]: e.activation(o_, i, AF.Identity, bias=c_b.ap[:, mi:mi + 1])), [ps, c_b], [o])
                P.dma("sp", OUT, outT[mi * 128:(mi + 1) * 128, it * NT:(it + 1) * NT], o, o.ap[:])
            full = (ncp // 512) * 512
            if full:
                linear(C, w, D, 0, full, 512, [(HB, lambda k: HB[k].ap[:])], epi)
            if ncp - full:
                linear(C, w, D, full, ncp - full, ncp - full, [(HB, lambda k: HB[k].ap[:])], epi)
        P.finish([OUT])
        P.emit_all()
    return nc


def moe_router(C, X, sc2p, sh2, wr_sb, n, hf_cb=None):
    P = C.P
    nch = n // 128
    pls = [C.next_ps() for _ in range(nch)]
    for k in range(KC):
        t = C.next_tmp()
        P.add("act", (lambda e, o=t.ap[:, 0:n], i=X[k].ap[:, 0:n], k=k:
                      e.activation(o, i, AF.Identity, bias=sh2.ap[:, k:k + 1], scale=sc2p.ap[:, k:k + 1])), [X[k], sh2, sc2p], [t])
        for j in range(nch):
            P.add("pe", (lambda e, o=pls[j].ap[:, 0:8], l=t.ap[:, j * 128:(j + 1) * 128], r=wr_sb.ap[:, k, :], st=(k == 0), sp=(k == KC - 1):
                         e.matmul(o, l, r, start=st, stop=sp)), [t, wr_sb], [pls[j]])
        if hf_cb is not None:
            hf_cb(k, t)
    if not hasattr(C, "rt"):
        C.rt = [P.sb([128, 4, 8], F32, nm) for nm in ("lg", "l2", "eq1", "eq2")] + [P.sb([128, 4], F32, nm) for nm in ("m1", "m2", "ga", "gb")]
    lg, l2, eq1, eq2, m1, m2, ga, gb = C.rt
    v3 = lambda t: t.ap[:, 0:nch, :]
    bc = lambda t: t.ap[:, 0:nch].unsqueeze(2).to_broadcast([128, nch, 8])
    for j in range(nch):
        P.add("dve", (lambda e, j=j: e.tensor_copy(lg.ap[:, j, :], pls[j].ap[:, 0:8])), [pls[j]], [lg])
    P.add("dve", lambda e: e.tensor_reduce(m1.ap[:, 0:nch], v3(lg), mybir.AxisListType.X, ALU.max), [lg], [m1])
    P.add("dve", lambda e: e.tensor_tensor(v3(eq1), v3(lg), bc(m1), ALU.is_equal), [lg, m1], [eq1])
    P.add("dve", lambda e: e.scalar_tensor_tensor(v3(l2), v3(eq1), -1e30, v3(lg), ALU.mult, ALU.add), [eq1, lg], [l2])
    P.add("dve", lambda e: e.tensor_reduce(m2.ap[:, 0:nch], v3(l2), mybir.AxisListType.X, ALU.max), [l2], [m2])
    P.add("dve", lambda e: e.tensor_tensor(v3(eq2), v3(l2), bc(m2), ALU.is_equal), [l2, m2], [eq2])
    P.add("dve", lambda e: e.tensor_tensor(gb.ap[:, 0:nch], m2.ap[:, 0:nch], m1.ap[:, 0:nch], ALU.subtract), [m1, m2], [gb])
    P.add("act", lambda e: e.activation(gb.ap[:, 0:nch], gb.ap[:, 0:nch], AF.Exp), [gb], [gb])
    P.add("dve", lambda e: e.tensor_scalar(ga.ap[:, 0:nch], gb.ap[:, 0:nch], 1.0, None, ALU.add), [gb], [ga])
    P.add("dve", lambda e: e.reciprocal(ga.ap[:, 0:nch], ga.ap[:, 0:nch]), [ga], [ga])
    P.add("dve", lambda e: e.tensor_tensor(gb.ap[:, 0:nch], gb.ap[:, 0:nch], ga.ap[:, 0:nch], ALU.mult), [ga, gb], [gb])
    P.add("dve", lambda e: e.tensor_tensor(v3(eq1), v3(eq1), bc(ga), ALU.mult), [eq1, ga], [eq1])
    P.add("dve", lambda e: e.tensor_tensor(v3(eq2), v3(eq2), bc(gb), ALU.mult), [eq2, gb], [eq2])
    P.add("dve", lambda e: e.tensor_tensor(v3(eq1), v3(eq1), v3(eq2), ALU.add), [eq1, eq2], [eq1])
    return eq1

GELU_C = 2.0 * math.sqrt(2.0 / math.pi)


def gelu_ops(C, xb_t, xb, out_t, out, n):
    P = C.P
    g = C.next_tmp()
    ga = g.ap[:, 0:n]
    P.add("act", lambda e: e.activation(ga, xb, AF.Square), [xb_t], [g])
    P.add("dve", lambda e: e.tensor_scalar(ga, ga, 0.044715, 1.0, ALU.mult, ALU.add), [g], [g])
    P.add("pool", lambda e: e.tensor_tensor(ga, ga, xb, ALU.mult), [g, xb_t], [g])
    P.add("act", lambda e: e.activation(ga, ga, AF.Sigmoid, scale=GELU_C), [g], [g])
    P.add("pool", lambda e: e.tensor_tensor(out, xb, ga, ALU.mult), [g, xb_t], [out_t])


DBG_TAIL = False


def build_tail(glu, moe, gmlp=False):
    nc = bass.Bass("TRN2", target_bir_lowering=False)
    dr = lambda name, shape: nc.dram_tensor(name, list(shape), F32, kind="ExternalInput").ap()
    xT = dr("xT", [D, TC])
    if not gmlp:
        yT = dr("yT", [D, TC])
    modc_d = dr("modc", [128, 6 * KC])
    CV = [128, KC]
    ln1_g, ln1_b, ln2_g, ln2_b = dr("ln1_g", CV), dr("ln1_b", CV), dr("ln2_g", CV), dr("ln2_b", CV)
    if gmlp:
        d_bi = dr("d_bi", [128, KC])
        d_bv = dr("d_bv", [128, D])
        d_lng, d_lnb = dr("d_lng", CV), dr("d_lnb", CV)
        d_wspT = dr("d_wspT", [128, 8, 128])
        d_mask = dr("d_mask", [128, 128])
        d_bsp = dr("d_bsp", [128, 8, 128])
    ncol = 2 * D if glu else D
    b = dr("b", [128, ncol // 128])
    if moe:
        wr = dr("wr", [128, KC, 8])
        idn = dr("idn", [128, 128])
    if not moe:
        outT = nc.dram_tensor("outT", [D, TC], F32, kind="ExternalOutput").ap()
    with ExitStack() as es:
        P = Prog(nc, es)
        w = P.gathered("w", D, ncol)
        if gmlp:
            w_gin = P.gathered("w_gin", D, 2 * D)
        if moe:
            x1T = nc.dram_tensor("x1T", [D, TC], F32, kind="ExternalOutput").ap()
            hfT = nc.dram_tensor("hfT", [D, TC], F32, kind="ExternalOutput").ap()
            gm_d = nc.dram_tensor("gm", [TC, 8], F32, kind="ExternalOutput").ap()
        else:
            w1, w3, w2 = P.gathered("w1", D, DFF), P.gathered("w3", D, DFF), P.gathered("w2", DFF, D)
        C = Ctx(P, nslab=(2 if (gmlp or not moe) else 3), ntmp=(4 if gmlp else 3), nsmall=(6 if (not moe and not gmlp) else 0))
        sh1, sc1, g1, sh2, sc2, g2 = setup_common(P, C, modc_d)
        sc2p = plus_one(P, sc2, "sc2p")
        if gmlp:
            sc1p = plus_one(P, sc1, "sc1p")
        c_ln1g, c_ln1b = load_cols(P, "c_ln1g", ln1_g, KC), load_cols(P, "c_ln1b", ln1_b, KC)
        c_ln2g, c_ln2b = load_cols(P, "c_ln2g", ln2_g, KC), load_cols(P, "c_ln2b", ln2_b, KC)
        c_b = load_cols(P, "c_b", b, ncol // 128)
        X = [P.sb([128, NT], F32, "X%d" % k) for k in range(KC)]
        HB = [P.sb([128, NT], BF16, "HB%d" % k) for k in range(KC)]
        if not gmlp:
            G = [P.sb([128, NT], BF16, "G%d" % m) for m in range(MC_FF)]
        else:
            arena = P.sb([128, MC_FF * NT], BF16, "arena")
            G = [Tl(arena.ap[:, m * NT:(m + 1) * NT], "G%d" % m) for m in range(MC_FF)]
            vview = arena.ap[:, 0:16384].bitcast(F32)
            V = [Tl(vview[:, j * D:(j + 1) * D], "V%d" % j) for j in range(4)]
            VB = [Tl(arena.ap[:, 16384 + j * D:16384 + (j + 1) * D], "VB%d" % j) for j in range(3)] + [Tl(arena.ap[:, 0:D], "VB3")]
            for j in range(4):
                V[j].al = tuple(G[8 * j:8 * j + 8]) + ((VB[3],) if j == 0 else ())
            for j in range(3):
                VB[j].al = tuple(G[32 + 4 * j:32 + 4 * j + 4])
            VB[3].al = tuple(G[0:4]) + (V[0],)
            for m in range(MC_FF):
                al = []
                if m < 32:
                    al.append(V[m // 8])
                    if m < 4:
                        al.append(VB[3])
                else:
                    al.append(VB[(m - 32) // 4])
                G[m].al = tuple(al)
            U = [P.sb([128, NT], BF16, "U%d" % k) for k in range(KC)]
            c_dbi = load_cols(P, "c_dbi", d_bi, KC)
            c_dlng, c_dlnb = load_cols(P, "c_dlng", d_lng, KC), load_cols(P, "c_dlnb", d_lnb, KC)
            BR = P.sb([128, D], F32, "BR")
            P.dma("sp", BR, BR.ap[:], None, d_bv)
            WTf = P.sb([128, 8, 128], F32, "WTf")
            P.dma("sp", WTf, WTf.ap[:], None, d_wspT)
            MK = P.sb([128, 128], F32, "MK")
            P.dma("sp", MK, MK.ap[:], None, d_mask)
            BSP = P.sb([128, 8, 128], F32, "BSP")
            P.dma("sp", BSP, BSP.ap[:], None, d_bsp)
            WT = P.sb([128, 8, 128], BF16, "WT")
            RS = P.sb([128, 8, 128], F32, "RS")
            for g_ in range(8):
                P.add("dve", (lambda e, g_=g_: e.tensor_tensor(WTf.ap[:, g_, :], WTf.ap[:, g_, :], MK.ap[:], ALU.mult)), [WTf, MK], [WTf])
            P.add("dve", lambda e: e.tensor_copy(WT.ap[:], WTf.ap[:]), [WTf], [WT])
            for g_ in range(0, 8, 4):
                pr_ = C.next_ps()
                P.add("pe", (lambda e, o=pr_.ap[:], r=WTf.ap[:, g_:g_ + 4, :]: e.matmul(o, C.ones.ap[:], r, start=True, stop=True)), [C.ones, WTf], [pr_])
                P.add("dve", (lambda e, o=RS.ap[:, g_:g_ + 4, :], i=pr_.ap[:].rearrange("p (g t) -> p g t", g=4): e.tensor_copy(o, i)), [pr_], [RS])
            lnc = [P.sb([128, 4], F32, "lnc%d" % i) for i in range(3)]
            lns = [P.sb([128, 1], F32, "lns%d" % i) for i in range(3)]
            t1s = [P.sb([128, 128], F32, "t1s%d" % i) for i in range(2)]
            t2s = [P.sb([128, 128], F32, "t2s%d" % i) for i in range(2)]
        if moe:
            ACC = None
            wr_sb = P.sb([128, KC, 8], F32, "wr_sb")
            P.dma("sp", wr_sb, wr_sb.ap[:], None, wr)
            ident = P.sb([128, 128], F32, "ident")
            P.dma("sp", ident, ident.ap[:], None, idn)
            dg = [P.sb([128, 128], F32, "dg%d" % i) for i in range(2)]
        if not glu:
            g1b = P.sb([128, KC], F32, "g1b")
            P.add("dve", lambda e: e.tensor_tensor(g1b.ap[:], g1.ap[:], c_b.ap[:], ALU.mult), [g1, c_b], [g1b])
        OUT = Tl(None, "out")
        if DBG_TAIL:
            dbgT = nc.dram_tensor("dbgT", [D, TC], F32, kind="ExternalOutput").ap()
            DBG = Tl(None, "dbg")
        for it in range(TC // NT):
            for k in range(KC):
                P.dma("sp", X[k], X[k].ap[:], None, xT[k * 128:(k + 1) * 128, it * NT:(it + 1) * NT])
                if not gmlp:
                    P.dma("pool", HB[k], HB[k].ap[:], None, yT[k * 128:(k + 1) * 128, it * NT:(it + 1) * NT])
                else:
                    P.add("act", (lambda e, o=HB[k].ap[:], i=X[k].ap[:], k=k:
                                  e.activation(o, i, AF.Identity, bias=sh1.ap[:, k:k + 1], scale=sc1p.ap[:, k:k + 1])), [X[k], sh1, sc1p], [HB[k]])
            if gmlp:
                def epi_u(mi, gi, ps):
                    xb = C.next_tmp()
                    P.add("act", (lambda e, o=xb.ap[:], i=ps.ap[:]: e.activation(o, i, AF.Identity, bias=c_dbi.ap[:, mi:mi + 1])), [ps, c_dbi], [xb])
                    gelu_ops(C, xb, xb.ap[:], U[mi], U[mi].ap[:], NT)
                linear(C, w_gin, D, 0, D, 512, [(HB, lambda k: HB[k].ap[:])], epi_u)
                wvv = w_gin.rearrange("(k p) n -> p k n", p=128)
                for s_ in range(4):
                    slab = C.next_slab()
                    sv = slab.ap[:, 0:KC * 512].rearrange("p (k n) -> p k n", k=KC)
                    P.dma("pool", slab, sv, None, wvv[:, :, D + s_ * 512:D + (s_ + 1) * 512])
                    for j in range(4):
                        ps = C.next_ps()
                        for k in range(KC):
                            P.add("pe", (lambda e, o=ps.ap[:], l=HB[k].ap[:, j * 128:(j + 1) * 128], r=sv[:, k, :], st=(k == 0), sp=(k == KC - 1):
                                         e.matmul(o, l, r, start=st, stop=sp)), [slab, HB[k]], [ps])
                        xb = C.next_tmp()
                        P.add("dve", (lambda e, o=xb.ap[:], a=ps.ap[:], b_=BR.ap[:, s_ * 512:(s_ + 1) * 512]: e.tensor_tensor(o, a, b_, ALU.add)), [ps, BR], [xb])
                        gelu_ops(C, xb, xb.ap[:], V[j], V[j].ap[:, s_ * 512:(s_ + 1) * 512], NT)
                for j in range(4):
                    c4, s1, s2 = lnc[j % 3], lns[j % 3], lns[(j + 1) % 3]
                    P.add("dve", (lambda e, o=s1.ap[:], i=V[j].ap[:]: e.tensor_reduce(o, i, mybir.AxisListType.X, ALU.add)), [V[j]], [s1])
                    P.add("dve", (lambda e, o=s1.ap[:]: e.tensor_scalar(o, o, -1.0 / D, None, ALU.mult)), [s1], [s1])
                    P.add("dve", (lambda e, o=V[j].ap[:], sc_=s1.ap[:, 0:1]: e.tensor_scalar(o, o, sc_, None, ALU.add)), [V[j], s1], [V[j]])
                    for s_ in range(4):
                        sq = C.next_tmp()
                        P.add("act", (lambda e, o=sq.ap[:], i=V[j].ap[:, s_ * 512:(s_ + 1) * 512]: e.activation(o, i, AF.Square)), [V[j]], [sq])
                        P.add("dve", (lambda e, o=c4.ap[:, s_:s_ + 1], i=sq.ap[:]: e.tensor_reduce(o, i, mybir.AxisListType.X, ALU.add)), [sq], [c4])
                    P.add("dve", (lambda e, o=s2.ap[:], i=c4.ap[:]: e.tensor_reduce(o, i, mybir.AxisListType.X, ALU.add)), [c4], [s2])
                    P.add("dve", (lambda e, o=s2.ap[:]: e.tensor_scalar(o, o, 1.0 / D, LN_EPS, ALU.mult, ALU.add)), [s2], [s2])
                    P.add("act", (lambda e, o=s2.ap[:]: e.activation(o, o, AF.Sqrt)), [s2], [s2])
                    P.add("dve", (lambda e, o=s2.ap[:]: e.reciprocal(o, o)), [s2], [s2])
                    P.add("dve", (lambda e, o=VB[j].ap[:], i=V[j].ap[:], sc_=s2.ap[:, 0:1]: e.tensor_scalar(o, i, sc_, None, ALU.mult)), [V[j], s2], [VB[j]])
                for c in range(KC):
                    g_ = c // 2
                    t1 = t1s[c % 2]
                    P.add("dve", (lambda e, o=t1.ap[:], r=RS.ap[:, g_, :], b_=BSP.ap[:, g_, :], c=c:
                                  e.scalar_tensor_tensor(o, r, c_dlnb.ap[:, c:c + 1], b_, ALU.mult, ALU.add)), [RS, BSP, c_dlnb], [t1])
                    for j in range(4):
                        ps = C.next_ps()
                        P.add("pe", (lambda e, o=ps.ap[:, 0:128], l=VB[j].ap[:, c * 128:(c + 1) * 128], r=WT.ap[:, g_, :]:
                                     e.matmul(o, l, r, start=True, stop=True)), [VB[j], WT], [ps])
                        t2 = t2s[j % 2]
                        P.add("dve", (lambda e, o=t2.ap[:], p_=ps.ap[:, 0:128], t1_=t1.ap[:], c=c:
                                      e.scalar_tensor_tensor(o, p_, c_dlng.ap[:, c:c + 1], t1_, ALU.mult, ALU.add)), [ps, t1, c_dlng], [t2])
                        P.add("pool", (lambda e, o=HB[c].ap[:, j * 128:(j + 1) * 128], a=t2.ap[:], u_=U[c].ap[:, j * 128:(j + 1) * 128]:
                                       e.tensor_tensor(o, a, u_, ALU.mult)), [t2, U[c]], [HB[c]])
            if glu:
                wv = w.rearrange("(k p) n -> p k n", p=128)
                for s in range(4):
                    sa = C.next_slab()
                    sg = C.next_slab()
                    va = sa.ap[:, 0:KC * 512].rearrange("p (k n) -> p k n", k=KC)
                    vg = sg.ap[:, 0:KC * 512].rearrange("p (k n) -> p k n", k=KC)
                    P.dma("pool", sa, va, None, wv[:, :, s * 512:(s + 1) * 512])
                    P.dma("pool", sg, vg, None, wv[:, :, D + s * 512:D + (s + 1) * 512])
                    for j in range(4):
                        m = s * 4 + j
                        pa = C.next_ps()
                        pg = C.next_ps()
                        for k in range(KC):
                            P.add("pe", (lambda e, o=pa.ap[:], l=va[:, k, j * 128:(j + 1) * 128], r=HB[k].ap[:], st=(k == 0), sp=(k == KC - 1):
                                         e.matmul(o, l, r, start=st, stop=sp)), [sa, HB[k]], [pa])
                        for k in range(KC):
                            P.add("pe", (lambda e, o=pg.ap[:], l=vg[:, k, j * 128:(j + 1) * 128], r=HB[k].ap[:], st=(k == 0), sp=(k == KC - 1):
                                         e.matmul(o, l, r, start=st, stop=sp)), [sg, HB[k]], [pg])
                        t = C.next_tmp()
                        P.add("act", (lambda e, o=t.ap[:], i=pg.ap[:], m=m:
                                      e.activation(o, i, AF.Sigmoid, bias=c_b.ap[:, KC + m:KC + m + 1])), [pg, c_b], [t])
                        P.add("dve", (lambda e, o=t.ap[:], a=pa.ap[:], m=m:
                                      e.scalar_tensor_tensor(o, a, c_b.ap[:, m:m + 1], o, ALU.add, ALU.mult)), [pa, t, c_b], [t])
                        P.add("act", (lambda e, o=t.ap[:], m=m: e.activation(o, o, AF.Copy, scale=g1.ap[:, m:m + 1])), [t, g1], [t])
                        P.add("dve", (lambda e, o=X[m].ap[:], b_=t.ap[:]: e.scalar_tensor_tensor(o, o, ALPHA, b_, ALU.mult, ALU.add)), [X[m], t], [X[m]])
            else:
                def epi_out(mi, gi, ps):
                    t = C.next_tmp()
                    P.add("act", (lambda e, o=t.ap[:], i=ps.ap[:]: e.activation(o, i, AF.Identity, bias=g1b.ap[:, mi:mi + 1], scale=g1.ap[:, mi:mi + 1])), [ps, g1b, g1], [t])
                    P.add("dve", (lambda e, o=X[mi].ap[:], b_=t.ap[:]: e.scalar_tensor_tensor(o, o, ALPHA, b_, ALU.mult, ALU.add)), [X[mi], t], [X[mi]])
                linear(C, w, D, 0, D, 512, [(HB, lambda k: HB[k].ap[:])], epi_out)
            ln_inplace(C, X, NT, c_ln1g, c_ln1b, HB, sc2p, sh2)
            if DBG_TAIL:
                for k in range(KC):
                    P.dma("sp", DBG, dbgT[k * 128:(k + 1) * 128, it * NT:(it + 1) * NT], X[k], X[k].ap[:])
            if not moe:
                ffn_phase(C, HB, X, w1, w3, w2, G, g2, NT)
            else:
                def hf_out(k, t):
                    P.dma("sp", OUT, hfT[k * 128:(k + 1) * 128, it * NT:(it + 1) * NT], t, t.ap[:])
                Gm = moe_router(C, X, sc2p, sh2, wr_sb, NT, hf_out)
                P.dma("sp", OUT, gm_d[it * NT:(it + 1) * NT, :].rearrange("(j p) e -> p j e", p=128), Gm, Gm.ap[:])
                for k in range(KC):
                    P.dma("sp", OUT, x1T[k * 128:(k + 1) * 128, it * NT:(it + 1) * NT], X[k], X[k].ap[:])
                continue
            ln_inplace(C, X, NT, c_ln2g, c_ln2b)
            for k in range(KC):
                P.dma("sp", OUT, outT[k * 128:(k + 1) * 128, it * NT:(it + 1) * NT], X[k], X[k].ap[:])
        P.finish([OUT] + ([DBG] if DBG_TAIL else []))
        P.emit_all()
    return nc


def common_maps(inp, layer, core):
    b = core // 2
    return {"modc": cols(inp["_mod"][layer][b])}


def tok_T(x, core):
    b, h = core // 2, core % 2
    return np.ascontiguousarray(x[b, h * TC:(h + 1) * TC, :].T)


def from_T(res, key, width):
    out = np.empty((4, SEQ, width), np.float32)
    for core in range(NCORE):
        b, h = core // 2, core % 2
        out[b, h * TC:(h + 1) * TC, :] = res[core][key].T[:, :width]
    return out


def run_head(inp, x, layer, w, bvec):
    ncols = w.shape[1]
    nm = (ncols + 127) // 128
    wp = np.zeros((D, nm * 128), np.float32)
    wp[:, :ncols] = w
    bp = np.zeros((nm * 128,), np.float32)
    bp[:ncols] = bvec
    nc = build_head(ncols, layer)
    maps = []
    for core in range(NCORE):
        m = common_maps(inp, layer, core)
        m.update({"xT": tok_T(x, core), "w": shard_rows(wp, core), "b": cols(bp)})
        maps.append({k: np.ascontiguousarray(v, dtype=np.float32) for k, v in m.items()})
    res = _run(nc, maps)
    return from_T(res, "outT", ncols)


def run_tail(inp, x, y, layer, w, bvec, glu, moe, gmlp=False):
    nc = build_tail(glu, moe, gmlp)
    maps = []
    fi = layer // 2
    for core in range(NCORE):
        m = common_maps(inp, layer, core)
        if gmlp:
            m.update({"w_gin": shard_rows(inp["d_w_in"][0], core), "d_bi": cols(inp["d_b_in"][0][:D]),
                      "d_bv": np.tile(inp["d_b_in"][0][D:][None, :], (128, 1)),
                      "d_lng": cols(inp["d_ln_g"][0]), "d_lnb": cols(inp["d_ln_b"][0]),
                      "d_wspT": inp["d_w_sp"][0].transpose(2, 0, 1),
                      "d_mask": np.triu(np.ones((128, 128), np.float32)),
                      "d_bsp": np.tile(inp["d_b_sp"][0][None, :, :], (128, 1, 1))})
        else:
            m["yT"] = tok_T(y, core)
        m.update({"xT": tok_T(x, core), "w": shard_rows(w, core), "b": cols(bvec),
                  "ln1_g": cols(inp["ln1_g"][layer]), "ln1_b": cols(inp["ln1_b"][layer]),
                  "ln2_g": cols(inp["ln2_g"][layer]), "ln2_b": cols(inp["ln2_b"][layer])})
        if moe:
            m.update({"wr": inp["m_router"][fi].reshape(KC, 128, 8).transpose(1, 0, 2),
                      "idn": np.eye(128, dtype=np.float32)})
        else:
            m.update({"w1": shard_rows(inp["f_w1"][fi], core), "w3": shard_rows(inp["f_w3"][fi], core), "w2": shard_rows(inp["f_w2"][fi], core)})
        maps.append({k: np.ascontiguousarray(v, dtype=np.float32) for k, v in m.items()})
    res = _run(nc, maps)
    if moe:
        gm = np.empty((4, SEQ, 8), np.float32)
        for core in range(NCORE):
            b, h = core // 2, core % 2
            gm[b, h * TC:(h + 1) * TC] = res[core]["gm"]
        return from_T(res, "x1T", D), from_T(res, "hfT", D), gm
    return from_T(res, "outT", D)


S5_TB = 512
TWO_PI = 2.0 * math.pi

MAGIC = 12582912.0
INV2PI = 1.0 / (2.0 * math.pi)
PI_SAFE = 3.14159


def sincos(P, ang, sn, cs, w1, w2, aa, sa, ca):
    A = P.add
    for (dst, da, shift) in ((sn, sa, 0.0), (cs, ca, 0.25)):
        A("dve", lambda e, sh=shift: e.tensor_scalar(aa(w1), aa(ang), INV2PI, sh, ALU.mult, ALU.add), [ang], [w1])
        A("dve", lambda e: e.tensor_scalar(aa(w1), aa(w1), MAGIC, None, ALU.add), [w1], [w1])
        A("dve", lambda e: e.tensor_scalar(aa(w1), aa(w1), -MAGIC, None, ALU.add), [w1], [w1])
        A("dve", lambda e: e.scalar_tensor_tensor(aa(w2), aa(w1), -TWO_PI, aa(ang), ALU.mult, ALU.add), [w1, ang], [w2])
        A("dve", lambda e, sh=shift: e.tensor_scalar(aa(w2), aa(w2), sh * TWO_PI, PI_SAFE, ALU.add, ALU.min), [w2], [w2])
        A("dve", lambda e: e.tensor_scalar(aa(w2), aa(w2), -PI_SAFE, None, ALU.max), [w2], [w2])
        A("act", lambda e, d_=dst, da_=da: e.activation(da_(d_), aa(w2), AF.Sin), [w2], [dst])


def build_s5core():
    nc = bass.Bass("TRN2", target_bir_lowering=False)
    dr = lambda name, shape: nc.dram_tensor(name, list(shape), F32, kind="ExternalInput").ap()
    NP_ = 32
    NCH = 8
    uT = dr("uT", [NCH * 128, SEQ])
    are_c, aim_c, ldt_c = dr("are_c", [128, NP_]), dr("aim_c", [128, NP_]), dr("ldt_c", [128, NP_])
    bre_l, bim_l = dr("bre_l", [128, NP_, 128]), dr("bim_l", [128, NP_, 128])
    cre_l, cim_l = dr("cre_l", [128, NP_, 128]), dr("cim_l", [128, NP_, 128])
    d_c = dr("d_c", [128, NCH])
    iota_d = dr("iota", [128, S5_TB])
    yT = nc.dram_tensor("yT", [NCH * 128, SEQ], F32, kind="ExternalOutput").ap()
    TB = S5_TB
    NB = SEQ // TB
    with ExitStack() as es:
        P = Prog(nc, es)
        pss = [P.ps([128, 512], F32, "ps%d" % i) for i in range(8)]
        psi = [0]

        def nps():
            p = pss[psi[0] % 8]
            psi[0] += 1
            return p
        ld = lambda name, ap, shape: (lambda t: (P.dma("sp", t, t.ap[:], None, ap), t)[1])(P.sb(shape, F32, name))
        are = ld("are", are_c, [128, NP_])
        aim = ld("aim", aim_c, [128, NP_])
        ldt = ld("ldt", ldt_c, [128, NP_])
        dcol = ld("dcol", d_c, [128, NCH])
        iota = ld("iota_sb", iota_d, [128, TB])
        brel = P.sb([128, NP_, 128], BF16, "brel")
        biml = P.sb([128, NP_, 128], BF16, "biml")
        P.dma("pool", brel, brel.ap[:], None, bre_l)
        P.dma("pool", biml, biml.ap[:], None, bim_l)
        sm = lambda name: P.sb([128, NP_], F32, name)
        dt, rho, th, sn, cs, lr, li, den, zr, zi, t1, t2 = [sm(n) for n in ("dt", "rho", "th", "sn0", "cs0", "lr", "li", "den", "zr", "zi", "t1s", "t2s")]
        negpi = P.sb([128, 1], F32, "negpi")
        P.add("dve", lambda e: e.memset(negpi.ap[:], -math.pi), [], [negpi])
        A = lambda eng, f, r, w: P.add(eng, f, r, w)
        A("act", lambda e: e.activation(dt.ap[:], ldt.ap[:], AF.Exp), [ldt], [dt])
        A("dve", lambda e: e.tensor_tensor(t1.ap[:], are.ap[:], dt.ap[:], ALU.mult), [are, dt], [t1])
        A("act", lambda e: e.activation(rho.ap[:], t1.ap[:], AF.Exp), [t1], [rho])
        A("dve", lambda e: e.tensor_tensor(th.ap[:], aim.ap[:], dt.ap[:], ALU.mult), [aim, dt], [th])
        full = lambda t: t.ap[:]
        sincos(P, th, sn, cs, t1, t2, full, full, full)
        thb, snB, csB = sm("thb"), sm("snB"), sm("csB")
        A("dve", lambda e: e.tensor_scalar(thb.ap[:], th.ap[:], float(S5_TB), None, ALU.mult), [th], [thb])
        sincos(P, thb, snB, csB, t1, t2, full, full, full)
        A("dve", lambda e: e.tensor_tensor(lr.ap[:], rho.ap[:], cs.ap[:], ALU.mult), [rho, cs], [lr])
        A("dve", lambda e: e.tensor_tensor(li.ap[:], rho.ap[:], sn.ap[:], ALU.mult), [rho, sn], [li])
        A("dve", lambda e: e.tensor_tensor(den.ap[:], are.ap[:], are.ap[:], ALU.mult), [are], [den])
        A("dve", lambda e: e.tensor_tensor(t1.ap[:], aim.ap[:], aim.ap[:], ALU.mult), [aim], [t1])
        A("dve", lambda e: e.tensor_tensor(den.ap[:], den.ap[:], t1.ap[:], ALU.add), [den, t1], [den])
        A("dve", lambda e: e.reciprocal(den.ap[:], den.ap[:]), [den], [den])
        A("dve", lambda e: e.tensor_scalar(lr.ap[:], lr.ap[:], -1.0, None, ALU.add), [lr], [lr])
        A("dve", lambda e: e.tensor_tensor(t1.ap[:], lr.ap[:], are.ap[:], ALU.mult), [lr, are], [t1])
        A("dve", lambda e: e.tensor_tensor(t2.ap[:], li.ap[:], aim.ap[:], ALU.mult), [li, aim], [t2])
        A("dve", lambda e: e.tensor_tensor(zr.ap[:], t1.ap[:], t2.ap[:], ALU.add), [t1, t2], [zr])
        A("dve", lambda e: e.tensor_tensor(zr.ap[:], zr.ap[:], den.ap[:], ALU.mult), [zr, den], [zr])
        A("dve", lambda e: e.tensor_tensor(t1.ap[:], li.ap[:], are.ap[:], ALU.mult), [li, are], [t1])
        A("dve", lambda e: e.tensor_tensor(t2.ap[:], lr.ap[:], aim.ap[:], ALU.mult), [lr, aim], [t2])
        A("dve", lambda e: e.tensor_tensor(zi.ap[:], t1.ap[:], t2.ap[:], ALU.subtract), [t1, t2], [zi])
        A("dve", lambda e: e.tensor_tensor(zi.ap[:], zi.ap[:], den.ap[:], ALU.mult), [zi, den], [zi])
        nzi = sm("nzi")
        A("dve", lambda e: e.tensor_scalar(nzi.ap[:], zi.ap[:], -1.0, None, ALU.mult), [zi], [nzi])
        cpr = P.sb([128, NP_, 128], BF16, "cpr")
        ncpi = P.sb([128, NP_, 128], BF16, "ncpi")
        ctmp = [P.sb([128, 128], F32, "ctmp%d" % i) for i in range(4)]
        for j in range(NP_):
            cr_t, ci_t, w1_t, w2_t = ctmp
            P.dma("sp", cr_t, cr_t.ap[:], None, cre_l[:, j, :])
            P.dma("sp", ci_t, ci_t.ap[:], None, cim_l[:, j, :])
            A("dve", lambda e, j=j: e.tensor_scalar(w1_t.ap[:], ci_t.ap[:], nzi.ap[:, j:j + 1], None, ALU.mult), [ci_t, nzi], [w1_t])
            A("dve", lambda e, j=j: e.scalar_tensor_tensor(cpr.ap[:, j, :], cr_t.ap[:], zr.ap[:, j:j + 1], w1_t.ap[:], ALU.mult, ALU.add), [cr_t, zr, w1_t], [cpr])
            A("dve", lambda e, j=j: e.tensor_scalar(w2_t.ap[:], ci_t.ap[:], zr.ap[:, j:j + 1], None, ALU.mult), [ci_t, zr], [w2_t])
            A("dve", lambda e, j=j: e.scalar_tensor_tensor(w2_t.ap[:], cr_t.ap[:], zi.ap[:, j:j + 1], w2_t.ap[:], ALU.mult, ALU.add), [cr_t, zi, w2_t], [w2_t])
            A("dve", lambda e, j=j: e.tensor_scalar(ncpi.ap[:, j, :], w2_t.ap[:], -1.0, None, ALU.mult), [w2_t], [ncpi])
        W = lambda name: P.sb([128, TB], F32, name)
        ub = P.sb([128, SEQ], BF16, "ub")
        uf = P.sb([128, SEQ], F32, "uf")
        Sr = [P.sb([128, SEQ], BF16, "Sr%d" % i) for i in range(4)]
        Si = [P.sb([128, SEQ], BF16, "Si%d" % i) for i in range(4)]
        rho_t = W("rho_t")
        ang, r1, r2, sn_t, cs_t = W("ang"), W("r1"), W("r2"), W("sn_t"), W("cs_t")
        arS2, aiS2 = [W("arS0"), W("arS1")], [W("aiS0"), W("aiS1")]
        q1_2, q2_2, q3_2, q4_2 = [[W("q%d_%d" % (a_, b_)) for b_ in range(2)] for a_ in range(1, 5)]
        xr2, xi2 = [W("xr0"), W("xr1")], [W("xi0"), W("xi1")]
        sr2 = [W("sra"), W("srb")]
        si2 = [W("sia"), W("sib")]
        car = [P.sb([128, 1], F32, "car%d" % i) for i in range(4)]
        zero_c = P.sb([128, 1], F32, "zero_c")
        A("dve", lambda e: e.memset(zero_c.ap[:], 0.0), [], [zero_c])
        yo = [W("yo0"), W("yo1")]
        g1t, g2t = W("g1t"), W("g2t")
        OUT = Tl(None, "out")
        def do_block(c, j, jj, k):
            arS, aiS, q1, q2, q3, q4, xr, xi = arS2[k % 2], aiS2[k % 2], q1_2[k % 2], q2_2[k % 2], q3_2[k % 2], q4_2[k % 2], xr2[k % 2], xi2[k % 2]
            pr, pi_ = nps(), nps()
            A("pe", lambda e, o=pr.ap[:], l=brel.ap[:, j, :], r=ub.ap[:, k * TB:(k + 1) * TB]: e.matmul(o, l, r, start=True, stop=True), [brel, ub], [pr])
            A("pe", lambda e, o=pi_.ap[:], l=biml.ap[:, j, :], r=ub.ap[:, k * TB:(k + 1) * TB]: e.matmul(o, l, r, start=True, stop=True), [biml, ub], [pi_])
            A("act", lambda e, o=arS.ap[:], i=pr.ap[:]: e.activation(o, i, AF.Copy), [pr], [arS])
            A("act", lambda e, o=aiS.ap[:], i=pi_.ap[:]: e.activation(o, i, AF.Copy), [pi_], [aiS])
            A("dve", lambda e: e.tensor_tensor(q1.ap[:], arS.ap[:], cs_t.ap[:], ALU.mult), [arS, cs_t], [q1])
            A("dve", lambda e: e.tensor_tensor(q2.ap[:], aiS.ap[:], sn_t.ap[:], ALU.mult), [aiS, sn_t], [q2])
            A("pool", lambda e: e.tensor_tensor(q3.ap[:], aiS.ap[:], cs_t.ap[:], ALU.mult), [aiS, cs_t], [q3])
            A("pool", lambda e: e.tensor_tensor(q4.ap[:], arS.ap[:], sn_t.ap[:], ALU.mult), [arS, sn_t], [q4])
            A("dve", lambda e: e.tensor_tensor(xr.ap[:], q1.ap[:], q2.ap[:], ALU.add), [q1, q2], [xr])
            A("pool", lambda e: e.tensor_tensor(xi.ap[:], q3.ap[:], q4.ap[:], ALU.subtract), [q3, q4], [xi])
            srt, sit = sr2[k % 2], si2[k % 2]
            if k == 0:
                ir, ii_, rd = zero_c.ap[:, 0:1], zero_c.ap[:, 0:1], [zero_c]
            else:
                pr_ = sr2[(k - 1) % 2]
                pi2_ = si2[(k - 1) % 2]
                A("dve", lambda e, j=j, p_=pi2_: e.tensor_scalar(car[2].ap[:], p_.ap[:, TB - 1:TB], snB.ap[:, j:j + 1], None, ALU.mult), [pi2_, snB], [car[2]])
                A("dve", lambda e, j=j, p_=pr_: e.scalar_tensor_tensor(car[0].ap[:], p_.ap[:, TB - 1:TB], csB.ap[:, j:j + 1], car[2].ap[:], ALU.mult, ALU.subtract), [pr_, csB, car[2]], [car[0]])
                A("dve", lambda e, j=j, p_=pi2_: e.tensor_scalar(car[3].ap[:], p_.ap[:, TB - 1:TB], csB.ap[:, j:j + 1], None, ALU.mult), [pi2_, csB], [car[3]])
                A("dve", lambda e, j=j, p_=pr_: e.scalar_tensor_tensor(car[1].ap[:], p_.ap[:, TB - 1:TB], snB.ap[:, j:j + 1], car[3].ap[:], ALU.mult, ALU.add), [pr_, snB, car[3]], [car[1]])
                ir, ii_, rd = car[0].ap[:, 0:1], car[1].ap[:, 0:1], [car[0], car[1]]
            A("dve", lambda e, o=srt.ap[:], i0=ir: e.tensor_tensor_scan(o, rho_t.ap[:], xr.ap[:], i0, ALU.mult, ALU.add), [rho_t, xr] + rd, [srt])
            A("dve", lambda e, o=sit.ap[:], i0=ii_: e.tensor_tensor_scan(o, rho_t.ap[:], xi.ap[:], i0, ALU.mult, ALU.add), [rho_t, xi] + rd, [sit])
            A("dve", lambda e, s_=srt: e.tensor_tensor(q1.ap[:], s_.ap[:], cs_t.ap[:], ALU.mult), [srt, cs_t], [q1])
            A("dve", lambda e, s_=sit: e.tensor_tensor(q2.ap[:], s_.ap[:], sn_t.ap[:], ALU.mult), [sit, sn_t], [q2])
            A("pool", lambda e, s_=srt: e.tensor_tensor(q3.ap[:], s_.ap[:], sn_t.ap[:], ALU.mult), [srt, sn_t], [q3])
            A("pool", lambda e, s_=sit: e.tensor_tensor(q4.ap[:], s_.ap[:], cs_t.ap[:], ALU.mult), [sit, cs_t], [q4])
            A("dve", lambda e, o=Sr[jj].ap[:, k * TB:(k + 1) * TB]: e.tensor_tensor(o, q1.ap[:], q2.ap[:], ALU.subtract), [q1, q2], [Sr[jj]])
            A("pool", lambda e, o=Si[jj].ap[:, k * TB:(k + 1) * TB]: e.tensor_tensor(o, q3.ap[:], q4.ap[:], ALU.add), [q3, q4], [Si[jj]])

        for c in range(NCH):
            P.dma("sp", uf, uf.ap[:], None, uT[c * 128:(c + 1) * 128, :])
            P.dma("pool", ub, ub.ap[:], None, uT[c * 128:(c + 1) * 128, :])
            for jj in range(4):
                j = c * 4 + jj
                A("dve", lambda e, j=j: e.tensor_scalar(rho_t.ap[:], iota.ap[:], 0.0, rho.ap[:, j:j + 1], ALU.mult, ALU.add), [iota, rho], [rho_t])
                A("dve", lambda e, j=j: e.tensor_scalar(ang.ap[:], iota.ap[:], th.ap[:, j:j + 1], None, ALU.mult), [iota, th], [ang])
                sincos(P, ang, sn_t, cs_t, r1, r2, full, full, full)
                for k in range(NB):
                    do_block(c, j, jj, k)
            for k in range(NB):
                ps = nps()
                for jj in range(4):
                    j = c * 4 + jj
                    A("pe", lambda e, o=ps.ap[:], l=cpr.ap[:, j, :], r=Sr[jj].ap[:, k * TB:(k + 1) * TB], st=(jj == 0): e.matmul(o, l, r, start=st, stop=False), [cpr, Sr[jj]], [ps])
                    A("pe", lambda e, o=ps.ap[:], l=ncpi.ap[:, j, :], r=Si[jj].ap[:, k * TB:(k + 1) * TB], sp=(jj == 3): e.matmul(o, l, r, start=False, stop=sp), [ncpi, Si[jj]], [ps])
                y = yo[k % 2]
                A("dve", lambda e, o=y.ap[:], u_=uf.ap[:, k * TB:(k + 1) * TB], p_=ps.ap[:], c=c: e.scalar_tensor_tensor(o, u_, dcol.ap[:, c:c + 1], p_, ALU.mult, ALU.add), [uf, dcol, ps], [y])
                A("act", lambda e, o=g1t.ap[:], i=y.ap[:]: e.activation(o, i, AF.Square), [y], [g1t])
                A("dve", lambda e: e.tensor_scalar(g1t.ap[:], g1t.ap[:], 0.044715, 1.0, ALU.mult, ALU.add), [g1t], [g1t])
                A("pool", lambda e, i=y.ap[:]: e.tensor_tensor(g2t.ap[:], g1t.ap[:], i, ALU.mult), [g1t, y], [g2t])
                A("act", lambda e: e.activation(g2t.ap[:], g2t.ap[:], AF.Sigmoid, scale=2.0 * math.sqrt(2.0 / math.pi)), [g2t], [g2t])
                A("pool", lambda e, o=y.ap[:]: e.tensor_tensor(o, o, g2t.ap[:], ALU.mult), [y, g2t], [y])
                P.dma("sp", OUT, yT[c * 128:(c + 1) * 128, k * TB:(k + 1) * TB], y, y.ap[:])
        P.finish([OUT])
        P.emit_all()
    return nc


def run_s5core(inp, u):
    nc = build_s5core()
    are, aim, ldt = inp["b_a_re"][0], inp["b_a_im"][0], inp["b_log_dt"][0]
    bre, bim, cre, cim = inp["b_b_re"][0], inp["b_b_im"][0], inp["b_c_re"][0], inp["b_c_im"][0]
    dvec = inp["b_d"][0]
    maps = []
    for core in range(NCORE):
        b, gh = core // 2, core % 2
        g0 = gh * 64
        pc = lambda a: np.ascontiguousarray(a[g0:g0 + 64].reshape(32, 128).T)
        ldt_rep = np.repeat(ldt[:, None], 64, axis=1)
        bre_l = np.zeros((128, 32, 128), np.float32)
        bim_l = np.zeros((128, 32, 128), np.float32)
        cre_l = np.zeros((128, 32, 128), np.float32)
        cim_l = np.zeros((128, 32, 128), np.float32)
        for j in range(32):
            for gi in range(2):
                g = g0 + 2 * j + gi
                r0 = 16 * ((2 * j + gi) % 8)
                bre_l[r0:r0 + 16, j, gi * 64:(gi + 1) * 64] = bre[g].T
                bim_l[r0:r0 + 16, j, gi * 64:(gi + 1) * 64] = bim[g].T
                cre_l[gi * 64:(gi + 1) * 64, j, r0:r0 + 16] = cre[g].T
                cim_l[gi * 64:(gi + 1) * 64, j, r0:r0 + 16] = cim[g].T
        m = {"uT": np.ascontiguousarray(u[b, :, gh * 1024:(gh + 1) * 1024].T),
             "are_c": pc(are), "aim_c": pc(aim), "ldt_c": pc(ldt_rep),
             "bre_l": bre_l, "bim_l": bim_l, "cre_l": cre_l, "cim_l": cim_l,
             "d_c": np.ascontiguousarray(dvec[gh * 1024:(gh + 1) * 1024].reshape(8, 128).T),
             "iota": np.tile(np.arange(S5_TB, dtype=np.float32)[None, :], (128, 1))}
        maps.append({k: np.ascontiguousarray(v, dtype=np.float32) for k, v in m.items()})
    res = _run(nc, maps)
    y = np.empty((4, SEQ, D), np.float32)
    for core in range(NCORE):
        b, gh = core // 2, core % 2
        y[b, :, gh * 1024:(gh + 1) * 1024] = res[core]["yT"].T
    return y


ML_T = 128
DKS = 128.0 ** -0.5


def build_mlstm():
    nc = bass.Bass("TRN2", target_bir_lowering=False)
    dr = lambda name, shape: nc.dram_tensor(name, list(shape), F32, kind="ExternalInput").ap()
    qkT = dr("qkT", [8, 128, SEQ])
    wcv = dr("wcv", [128, 8, 4])
    bcv = dr("bcv", [128, 8])
    v_d = dr("v_tok", [SEQ, 4, 256])
    o_d = dr("o_tok", [SEQ, 1024])
    gi_d = dr("gi_tok", [SEQ, 4])
    gf_d = dr("gf_tok", [SEQ, 4])
    mhg_d = dr("mhg", [128, 1024])
    tri_d, blk_d, mkc_d, cm0_d, cm1_d, idn_d = [dr(n, [128, 128]) for n in ("tri", "blk", "mkc", "cm0", "cm1", "idn")]
    hs_d = nc.dram_tensor("hs", [SEQ, 1024], F32, kind="ExternalOutput").ap()
    NTL = SEQ // ML_T
    with ExitStack() as es:
        P = Prog(nc, es)
        A = P.add
        ldc = lambda name, ap, shape, dt=F32, q="sp": (lambda t: (P.dma(q, t, t.ap[:], None, ap), t)[1])(P.sb(shape, dt, name))
        TRI = ldc("TRI", tri_d, [128, 128])
        BLK = ldc("BLK", blk_d, [128, 128])
        MKC = ldc("MKC", mkc_d, [128, 128])
        CM0 = ldc("CM0", cm0_d, [128, 128])
        CM1 = ldc("CM1", cm1_d, [128, 128])
        IDB = ldc("IDB", idn_d, [128, 128], BF16, "pool")
        MHG = ldc("MHG", mhg_d, [128, 1024])
        WCV = ldc("WCV", wcv, [128, 8, 4])
        BCV = ldc("BCV", bcv, [128, 8])
        ONES = P.sb([128, 128], F32, "ONES")
        A("dve", lambda e: e.memset(ONES.ap[:], 1.0), [], [ONES])
        lnk = P.sb([128, 1], F32, "lnk")
        A("dve", lambda e: e.memset(lnk.ap[:], math.log(DKS)), [], [lnk])
        CF = [P.sb([128, 257], F32, "CF%d" % h) for h in range(4)]
        CA = [P.sb([128, 257], BF16, "CA%d" % h) for h in range(4)]
        CAm = [P.sb([128, 257], BF16, "CAm%d" % h) for h in range(4)]
        for h in range(4):
            A("dve", lambda e, h=h: e.memset(CF[h].ap[:], 0.0), [], [CF[h]])
            A("dve", lambda e, h=h: e.memset(CA[h].ap[:], 0.0), [], [CA[h]])
        QKR = [P.sb([128, 8, 3 + ML_T], F32, "QKR%d" % i) for i in range(2)]
        A("dve", lambda e: e.memset(QKR[1].ap[:], 0.0), [], [QKR[1]])
        QKC = [P.sb([128, 8, ML_T], BF16, "QKC%d" % i) for i in range(2)]
        cacc = [P.sb([128, ML_T], F32, "cacc%d" % i) for i in range(2)]
        VA = [P.sb([128, 4, 257], BF16, "VA%d" % i) for i in range(2)]
        for i in range(2):
            A("dve", lambda e, i=i: e.memset(VA[i].ap[:, :, 256:257], 1.0), [], [VA[i]])
        OT = [P.sb([128, 1024], F32, "OT%d" % i) for i in range(2)]
        GI = [P.sb([128, 4], F32, "GI%d" % i) for i in range(2)]
        GF = [P.sb([128, 4], F32, "GF%d" % i) for i in range(2)]
        LF = P.sb([128, 4], F32, "LF")
        BC, AC, BL, WK = [P.sb([128, 4], F32, n) for n in ("BCc", "ACc", "BLc", "WKc")]
        LFB = [P.sb([128, 128], F32, "LFB%d" % i) for i in range(2)]
        BBS = P.sb([128, 4, 128], F32, "BBS")
        EBC = P.sb([128, 4, 128], F32, "EBC")
        E0 = [P.sb([128, 128], F32, "E0_%d" % i) for i in range(2)]
        E1 = [P.sb([128, 128], F32, "E1_%d" % i) for i in range(2)]
        Q0 = [P.sb([128, 128], BF16, "Q0_%d" % i) for i in range(2)]
        Q1 = [P.sb([128, 128], BF16, "Q1_%d" % i) for i in range(2)]
        WTT = [P.sb([128, 128], F32, "WTT%d" % i) for i in range(2)]
        SB = [P.sb([128, 128], BF16, "SB%d" % i) for i in range(2)]
        KW = [P.sb([128, 128], BF16, "KW%d" % i) for i in range(2)]
        DN = [P.sb([128, 1], F32, "DN%d" % i) for i in range(2)]
        S1 = [P.sb([128, 1], F32, "S1_%d" % i) for i in range(2)]
        S2 = [P.sb([128, 1], F32, "S2_%d" % i) for i in range(2)]
        OG = [P.sb([128, 256], F32, "OG%d" % i) for i in range(2)]
        HG = [P.sb([128, 256], F32, "HG%d" % i) for i in range(2)]
        SQ = [P.sb([128, 256], F32, "SQ%d" % i) for i in range(2)]
        OUTT = [P.sb([128, 1024], F32, "OUTT%d" % i) for i in range(2)]
        p_bb = P.ps([128, 512], F32, "p_bb")
        p_st = P.ps([128, 512], F32, "p_st")
        p_kt = P.ps([128, 512], BF16, "p_kt")
        p_sm = P.ps([128, 512], F32, "p_sm")
        p_c = [P.ps([128, 512], F32, "p_c%d" % i) for i in range(2)]
        p_o = [P.ps([128, 512], F32, "p_o%d" % i) for i in range(2)]
        OUT = Tl(None, "out")
        pcs = [0]

        def do_tile(i):
            t0 = i * ML_T
            qr, qp = QKR[i % 2], QKR[(i + 1) % 2]
            qc = QKC[i % 2]
            va, ot, gi, gf = VA[i % 2], OT[i % 2], GI[i % 2], GF[i % 2]
            P.dma("sp", qr, qr.ap[:, :, 3:3 + ML_T], None, qkT[:, :, t0:t0 + ML_T].rearrange("g p t -> p g t"))
            P.dma("pool", va, va.ap[:, :, 0:256], None, v_d[t0:t0 + ML_T, :, :])
            P.dma("sp", ot, ot.ap[:], None, o_d[t0:t0 + ML_T, :])
            P.dma("sp", gi, gi.ap[:], None, gi_d[t0:t0 + ML_T, :])
            P.dma("sp", gf, gf.ap[:], None, gf_d[t0:t0 + ML_T, :])
            A("pool", lambda e, o=qr.ap[:, :, 0:3], s_=qp.ap[:, :, ML_T:ML_T + 3]: e.tensor_copy(o, s_), [qp], [qr])
            for g in range(8):
                ca = cacc[g % 2]
                A("dve", lambda e, g=g, o=ca.ap[:]: e.tensor_scalar(o, qr.ap[:, g, 0:ML_T], WCV.ap[:, g, 0:1], BCV.ap[:, g:g + 1], ALU.mult, ALU.add), [qr, WCV, BCV], [ca])
                for j in range(1, 4):
                    A("dve", lambda e, g=g, j=j, o=ca.ap[:]: e.scalar_tensor_tensor(o, qr.ap[:, g, j:j + ML_T], WCV.ap[:, g, j:j + 1], o, ALU.mult, ALU.add), [qr, WCV, ca], [ca])
                A("act", lambda e, g=g, i_=ca.ap[:]: e.activation(qc.ap[:, g, :], i_, AF.Silu), [ca], [qc])
            A("act", lambda e: e.activation(LF.ap[:], gf.ap[:], AF.Exp, scale=-1.0), [gf], [LF])
            A("act", lambda e: e.activation(LF.ap[:], LF.ap[:], AF.Ln, bias=1.0), [LF], [LF])
            A("dve", lambda e: e.tensor_scalar(LF.ap[:], LF.ap[:], -1.0, None, ALU.mult), [LF], [LF])
            A("pe", lambda e: e.matmul(p_sm.ap[:, 0:4], TRI.ap[:], LF.ap[:], start=True, stop=True), [TRI, LF], [p_sm])
            A("pe", lambda e: e.matmul(p_sm.ap[:, 4:8], BLK.ap[:], LF.ap[:], start=True, stop=True), [BLK, LF], [p_sm])
            A("dve", lambda e: e.tensor_copy(BC.ap[:], p_sm.ap[:, 0:4]), [p_sm], [BC])
            A("dve", lambda e: e.tensor_tensor(AC.ap[:], gi.ap[:], BC.ap[:], ALU.subtract), [gi, BC], [AC])
            A("dve", lambda e: e.tensor_tensor(BL.ap[:], p_sm.ap[:, 4:8], AC.ap[:], ALU.add), [p_sm, AC], [BL])
            A("act", lambda e: e.activation(WK.ap[:], BL.ap[:], AF.Exp, bias=lnk.ap[:, 0:1]), [BL, lnk], [WK])
            for h in range(4):
                lb = LFB[h % 2]
                A("dve", lambda e, h=h, o=lb.ap[:]: e.tensor_scalar(o, ONES.ap[:], LF.ap[:, h:h + 1], None, ALU.mult), [ONES, LF], [lb])
                A("pe", lambda e, h=h, l=lb.ap[:]: e.matmul(p_bb.ap[:, h * 128:(h + 1) * 128], l, TRI.ap[:], start=True, stop=True), [lb, TRI], [p_bb])
            A("act", lambda e: e.activation(BBS.ap[:], p_bb.ap[:].rearrange("p (h t) -> p h t", h=4), AF.Copy), [p_bb], [BBS])
            A("act", lambda e: e.activation(EBC.ap[:], BBS.ap[:], AF.Exp), [BBS], [EBC])
            for h in range(4):
                do_head(i, h, qc, va, ot)
            P.dma("sp", OUT, hs_d[t0:t0 + ML_T, :], OUTT[i % 2], OUTT[i % 2].ap[:])

        def do_head(i, h, qc, va, ot):
            if True:
                qh = qc.ap[:, h, :]
                kh = qc.ap[:, 4 + h, :]
                A("pe", lambda e, h=h, kh=kh, qh=qh: e.matmul(p_st.ap[:, h * 128:(h + 1) * 128], kh, qh, start=True, stop=True), [qc], [p_st])
                wt = WTT[h % 2]
                A("act", lambda e, h=h, o=wt.ap[:]: e.activation(o, BBS.ap[:, h, :], AF.Exp, bias=AC.ap[:, h:h + 1]), [BBS, AC], [wt])
                A("pool", lambda e, o=wt.ap[:]: e.tensor_tensor(o, o, MKC.ap[:], ALU.mult), [wt, MKC], [wt])
                sb = SB[h % 2]
                A("dve", lambda e, h=h, o=sb.ap[:], w_=wt.ap[:]: e.tensor_tensor(o, p_st.ap[:, h * 128:(h + 1) * 128], w_, ALU.mult), [p_st, wt], [sb])
                A("pe", lambda e, h=h, kh=kh: e.transpose(p_kt.ap[:, h * 128:(h + 1) * 128], kh, IDB.ap[:]), [qc, IDB], [p_kt])
                kw = KW[h % 2]
                A("act", lambda e, h=h, o=kw.ap[:]: e.activation(o, p_kt.ap[:, h * 128:(h + 1) * 128], AF.Copy, scale=WK.ap[:, h:h + 1]), [p_kt, WK], [kw])
                e0, e1, q0, q1 = E0[h % 2], E1[h % 2], Q0[h % 2], Q1[h % 2]
                A("pool", lambda e, h=h, o=e0.ap[:]: e.tensor_tensor(o, EBC.ap[:, h, :], CM0.ap[:], ALU.mult), [EBC, CM0], [e0])
                A("pool", lambda e, h=h, o=e1.ap[:]: e.tensor_tensor(o, EBC.ap[:, h, :], CM1.ap[:], ALU.mult), [EBC, CM1], [e1])
                A("dve", lambda e, o=q0.ap[:], qh=qh, e_=e0.ap[:]: e.tensor_tensor(o, qh, e_, ALU.mult), [qc, e0], [q0])
                A("dve", lambda e, o=q1.ap[:], qh=qh, e_=e1.ap[:]: e.tensor_tensor(o, qh, e_, ALU.mult), [qc, e1], [q1])
                po = p_o[h % 2]
                A("pe", lambda e, h=h, o=po.ap[:, 0:257], l=sb.ap[:]: e.matmul(o, l, va.ap[:, h, :], start=True, stop=False), [sb, va], [po])
                A("pe", lambda e, h=h, o=po.ap[:, 0:257], l=q0.ap[:]: e.matmul(o, l, CA[h].ap[:], start=False, stop=False), [q0, CA[h]], [po])
                for cc in range(2):
                    pc = p_c[pcs[0] % 2]
                    pcs[0] += 1
                    A("pe", lambda e, h=h, cc=cc, o=pc.ap[:, 0:257], l=kw.ap[64 * cc:64 * cc + 64, :]: e.matmul(o, l, va.ap[64 * cc:64 * cc + 64, h, :], start=True, stop=True), [kw, va], [pc])
                    A("dve", lambda e, h=h, cc=cc, p_=pc.ap[:, 0:257]: e.scalar_tensor_tensor(CF[h].ap[:], CF[h].ap[:], EBC.ap[:, h, 64 * cc + 63:64 * cc + 64], p_, ALU.mult, ALU.add), [CF[h], EBC, pc], [CF[h]])
                    if cc == 0:
                        A("act", lambda e, h=h: e.activation(CAm[h].ap[:], CF[h].ap[:], AF.Copy), [CF[h]], [CAm[h]])
                        A("pe", lambda e, h=h, o=po.ap[:, 0:257], l=q1.ap[:]: e.matmul(o, l, CAm[h].ap[:], start=False, stop=True), [q1, CAm[h]], [po])
                    else:
                        A("act", lambda e, h=h: e.activation(CA[h].ap[:], CF[h].ap[:], AF.Copy), [CF[h]], [CA[h]])
                dn, s1, s2, og, hg, sq = DN[h % 2], S1[h % 2], S2[h % 2], OG[h % 2], HG[h % 2], SQ[h % 2]
                A("act", lambda e, o=dn.ap[:], p_=po.ap[:, 256:257]: e.activation(o, p_, AF.Abs), [po], [dn])
                A("dve", lambda e, o=dn.ap[:]: e.tensor_scalar(o, o, 1.0, None, ALU.max), [dn], [dn])
                A("dve", lambda e, o=dn.ap[:]: e.reciprocal(o, o), [dn], [dn])
                A("act", lambda e, h=h, o=og.ap[:]: e.activation(o, ot.ap[:, h * 256:(h + 1) * 256], AF.Sigmoid), [ot], [og])
                A("dve", lambda e, o=hg.ap[:], p_=po.ap[:, 0:256], d_=dn.ap[:, 0:1], g_=og.ap[:]: e.scalar_tensor_tensor(o, p_, d_, g_, ALU.mult, ALU.mult), [po, dn, og], [hg])
                A("dve", lambda e, o=s1.ap[:], i_=hg.ap[:]: e.tensor_reduce(o, i_, mybir.AxisListType.X, ALU.add), [hg], [s1])
                A("dve", lambda e, o=s1.ap[:]: e.tensor_scalar(o, o, -1.0 / 256.0, None, ALU.mult), [s1], [s1])
                A("pool", lambda e, o=hg.ap[:], c_=s1.ap[:, 0:1]: e.tensor_scalar(o, o, c_, None, ALU.add), [hg, s1], [hg])
                A("act", lambda e, o=sq.ap[:], i_=hg.ap[:]: e.activation(o, i_, AF.Square), [hg], [sq])
                A("dve", lambda e, o=s2.ap[:], i_=sq.ap[:]: e.tensor_reduce(o, i_, mybir.AxisListType.X, ALU.add), [sq], [s2])
                A("dve", lambda e, o=s2.ap[:]: e.tensor_scalar(o, o, 1.0 / 256.0, LN_EPS, ALU.mult, ALU.add), [s2], [s2])
                A("act", lambda e, o=s2.ap[:]: e.activation(o, o, AF.Sqrt), [s2], [s2])
                A("dve", lambda e, o=s2.ap[:]: e.reciprocal(o, o), [s2], [s2])
                outt = OUTT[i % 2]
                A("dve", lambda e, h=h, o=outt.ap[:, h * 256:(h + 1) * 256], i_=hg.ap[:], c_=s2.ap[:, 0:1]: e.scalar_tensor_tensor(o, i_, c_, MHG.ap[:, h * 256:(h + 1) * 256], ALU.mult, ALU.mult), [hg, s2, MHG], [outt])
        for i in range(NTL):
            do_tile(i)
        P.finish([OUT])
        P.emit_all()
    return nc


def run_mlstm(inp, z):
    nc = build_mlstm()
    wc, bc, mhg = inp["c_w_conv"][0], inp["c_b_conv"][0], inp["c_mh_g"][0]
    ii = np.arange(128)
    same = (ii[:, None] // 64) == (ii[None, :] // 64)
    tri = (same & (ii[:, None] <= ii[None, :])).astype(np.float32)
    consts = {"tri": tri, "blk": same.astype(np.float32), "mkc": tri * np.float32(DKS),
              "cm0": np.tile((ii < 64).astype(np.float32)[None, :], (128, 1)),
              "cm1": np.tile((ii >= 64).astype(np.float32)[None, :], (128, 1)),
              "idn": np.eye(128, dtype=np.float32)}
    maps = []
    for core in range(NCORE):
        b, hh = core // 2, core % 2
        heads = [4 * hh + h for h in range(4)]
        chans = [np.arange(128 * h_, 128 * h_ + 128) for h_ in heads] + [np.arange(1024 + 128 * h_, 1024 + 128 * h_ + 128) for h_ in heads]
        qk = np.stack([z[b][:, ch].T for ch in chans], 0)
        wcv = np.stack([wc[:, ch].T for ch in chans], 1)
        bcv = np.stack([bc[ch] for ch in chans], 1)
        v = z[b][:, 2048 + 1024 * hh:2048 + 1024 * (hh + 1)].reshape(SEQ, 4, 256)
        o = z[b][:, 4096 + 1024 * hh:4096 + 1024 * (hh + 1)]
        gi = z[b][:, 6144 + 4 * hh:6144 + 4 * hh + 4]
        gf = z[b][:, 6152 + 4 * hh:6152 + 4 * hh + 4]
        m = {"qkT": qk, "wcv": wcv, "bcv": bcv, "v_tok": v, "o_tok": o, "gi_tok": gi, "gf_tok": gf,
             "mhg": np.tile(mhg[1024 * hh:1024 * (hh + 1)][None, :], (128, 1))}
        m.update(consts)
        maps.append({k: np.ascontiguousarray(v_, dtype=np.float32) for k, v_ in m.items()})
    res = _run(nc, maps)
    hs = np.empty((4, SEQ, D), np.float32)
    for core in range(NCORE):
        b, hh = core // 2, core % 2
        hs[b, :, 1024 * hh:1024 * (hh + 1)] = res[core]["hs"]
    return hs


def _dbg(name, arr):
    import os
    d = os.environ.get("KDEBUG_DIR")
    if d:
        np.save(os.path.join(d, name + ".npy"), arr[0])


def kernel(**inputs):
    inp = {k: np.asarray(v) for k, v in inputs.items()}
    inp["_mod"] = run_mod(inp)
    x = np.asarray(inp["x"], np.float32)
    x = run_layer0(inp, x)
    _dbg("x0", x)
    u = run_head(inp, x, 1, inp["b_w_in"][0], inp["b_b_in"][0])
    y = run_s5core(inp, u)
    _dbg("y1", y)
    del u
    x1, hf, gm = run_tail(inp, x, y, 1, inp["b_w_glu"][0], inp["b_b_glu"][0], True, True)
    del y, x
    ya, yb = run_experts(inp, 0, hf, gm)
    x = run_combine(inp, 1, x1, ya, yb)
    _dbg("x1", x)
    del x1, hf, ya, yb
    z = run_head(inp, x, 2, inp["c_w_in"][0], inp["c_b_in"][0])
    hs = run_mlstm(inp, z)
    _dbg("hs2", hs)
    del z
    x = run_tail(inp, x, hs, 2, inp["c_w_out"][0], inp["c_b_out"][0], False, False)
    _dbg("x2", x)
    del hs
    x1, hf, gm = run_tail(inp, x, None, 3, inp["d_w_out"][0], inp["d_b_out"][0], False, True, gmlp=True)
    del x
    ya, yb = run_experts(inp, 1, hf, gm)
    x = run_combine(inp, 3, x1, ya, yb)
    return np.ascontiguousarray(x, dtype=np.float32)


MODW = 6 * D // NCORE


def build_mod():
    nc = bass.Bass("TRN2", target_bir_lowering=False)
    dr = lambda name, shape: nc.dram_tensor(name, list(shape), F32, kind="ExternalInput").ap()
    c_d = dr("c_all", [128, KC, 4])
    aw = dr("adw", [4 * D, MODW])
    ab = dr("adb", [4, 4, MODW])
    out = nc.dram_tensor("modr", [4, 4, MODW], F32, kind="ExternalOutput").ap()
    with ExitStack() as es:
        P = Prog(nc, es)
        C = Ctx(P)
        craw = P.sb([128, KC, 4], F32, "craw")
        cb = P.sb([128, KC, 4], BF16, "cbf")
        P.dma("sp", craw, craw.ap[:], None, c_d)
        P.add("act", lambda e: e.activation(cb.ap[:], craw.ap[:], AF.Silu), [craw], [cb])
        bt = [P.sb([4, MODW], F32, "bt%d" % l) for l in range(4)]
        ot = [P.sb([4, 512], F32, "ot%d" % i) for i in range(2)]
        OUT = Tl(None, "out")
        i = 0
        for l in range(4):
            P.dma("sp", bt[l], bt[l].ap[:], None, ab[l])
            wv = aw[l * D:(l + 1) * D, :].rearrange("(k p) n -> p k n", p=128)
            for s_ in range(MODW // 512):
                slab = C.next_slab()
                sv = slab.ap[:, 0:KC * 512].rearrange("p (k n) -> p k n", k=KC)
                P.dma("pool", slab, sv, None, wv[:, :, s_ * 512:(s_ + 1) * 512])
                ps = C.next_ps()
                for k in range(KC):
                    P.add("pe", (lambda e, o=ps.ap[0:4, :], l_=cb.ap[:, k, :], r=sv[:, k, :], st=(k == 0), sp=(k == KC - 1):
                                 e.matmul(o, l_, r, start=st, stop=sp)), [slab, cb], [ps])
                o_ = ot[i % 2]
                i += 1
                P.add("dve", (lambda e, o=o_.ap[:], a=ps.ap[0:4, :], b_=bt[l].ap[:, s_ * 512:(s_ + 1) * 512]: e.tensor_tensor(o, a, b_, ALU.add)), [ps, bt[l]], [o_])
                P.dma("sp", OUT, out[l, :, s_ * 512:(s_ + 1) * 512], o_, o_.ap[:])
        P.finish([OUT])
        P.emit_all()
    return nc


def run_mod(inp):
    nc = build_mod()
    c_all = np.stack([cols(inp["c"][b]) for b in range(4)], -1)
    maps = []
    for core in range(NCORE):
        cs = slice(core * MODW, (core + 1) * MODW)
        m = {"c_all": c_all, "adw": inp["ada_w"][:, :, cs].reshape(4 * D, MODW),
             "adb": np.tile(inp["ada_b"][:, None, cs], (1, 4, 1))}
        maps.append({k: np.ascontiguousarray(v, dtype=np.float32) for k, v in m.items()})
    res = _run(nc, maps)
    mod = np.empty((4, 4, 6 * D), np.float32)
    for core in range(NCORE):
        mod[:, :, core * MODW:(core + 1) * MODW] = res[core]["modr"]
    return mod


def build_expert(ntile):
    n = ntile * NT
    nc = bass.Bass("TRN2", target_bir_lowering=False)
    dr = lambda name, shape: nc.dram_tensor(name, list(shape), F32, kind="ExternalInput").ap()
    xT = dr("xT", [D, n])
    grow = dr("grow", [1, n])
    w1, w3, w2 = dr("w1", [D, DFF]), dr("w3", [D, DFF]), dr("w2", [DFF, D])
    yT = nc.dram_tensor("yT", [D, n], F32, kind="ExternalOutput").ap()
    with ExitStack() as es:
        P = Prog(nc, es)
        C = Ctx(P, nslab=2, nsmall=6)
        X = [P.sb([128, NT], F32, "X%d" % k) for k in range(KC)]
        HB = [P.sb([128, NT], BF16, "HB%d" % k) for k in range(KC)]
        G = [P.sb([128, NT], BF16, "G%d" % m) for m in range(MC_FF)]
        onec = P.sb([128, KC], F32, "onec")
        P.add("dve", lambda e: e.memset(onec.ap[:], 1.0), [], [onec])
        gr = [P.sb([1, NT], F32, "gr%d" % i) for i in range(2)]
        OUT = Tl(None, "out")
        for it in range(ntile):
            g_ = gr[it % 2]
            P.dma("sp", g_, g_.ap[:], None, grow[:, it * NT:(it + 1) * NT])
            for k in range(KC):
                P.dma("pool", HB[k], HB[k].ap[:], None, xT[k * 128:(k + 1) * 128, it * NT:(it + 1) * NT])
                P.add("pool", (lambda e, o=X[k].ap[:]: e.memset(o, 0.0)), [], [X[k]])
            gate = C.pstat[1]
            P.add("pe", (lambda e, o=gate.ap[:], r=g_.ap[0:1, :]: e.matmul(o, C.ones.ap[0:1, :], r, start=True, stop=True)), [C.ones, g_], [gate])
            ffn_phase(C, HB, X, w1, w3, w2, G, onec, NT, gate=gate, ACC=1)
            for k in range(KC):
                P.dma("sp", OUT, yT[k * 128:(k + 1) * 128, it * NT:(it + 1) * NT], X[k], X[k].ap[:])
        P.finish([OUT])
        P.emit_all()
    return nc


def run_experts(inp, fi, hf, gm):
    hff = hf.reshape(-1, D)
    gmf = gm.reshape(-1, 8)
    idxs = [np.nonzero(gmf[:, e] != 0)[0] for e in range(8)]
    ntile = max(1, max((len(ix) + NT - 1) // NT for ix in idxs))
    n = ntile * NT
    nc = build_expert(ntile)
    maps = []
    for e in range(8):
        ix = idxs[e]
        xe = np.zeros((D, n), np.float32)
        xe[:, :len(ix)] = hff[ix].T
        ge = np.zeros((1, n), np.float32)
        ge[0, :len(ix)] = gmf[ix, e]
        maps.append({"xT": xe, "grow": ge, "w1": np.ascontiguousarray(inp["m_w1"][fi][e], dtype=np.float32),
                     "w3": np.ascontiguousarray(inp["m_w3"][fi][e], dtype=np.float32),
                     "w2": np.ascontiguousarray(inp["m_w2"][fi][e], dtype=np.float32)})
    res = _run(nc, maps)
    ya = np.zeros_like(hff)
    yb = np.zeros_like(hff)
    filled = np.zeros((hff.shape[0],), np.int32)
    for e in range(8):
        ix = idxs[e]
        ye = res[e]["yT"][:, :len(ix)].T
        first = filled[ix] == 0
        ya[ix[first]] = ye[first]
        yb[ix[~first]] = ye[~first]
        filled[ix] += 1
    return ya.reshape(hf.shape), yb.reshape(hf.shape)


def build_combine():
    nc = bass.Bass("TRN2", target_bir_lowering=False)
    dr = lambda name, shape: nc.dram_tensor(name, list(shape), F32, kind="ExternalInput").ap()
    x1T, yaT, ybT = dr("x1T", [D, TC]), dr("yaT", [D, TC]), dr("ybT", [D, TC])
    modc_d = dr("modc", [128, 6 * KC])
    ln2_g, ln2_b = dr("ln2_g", [128, KC]), dr("ln2_b", [128, KC])
    outT = nc.dram_tensor("outT", [D, TC], F32, kind="ExternalOutput").ap()
    with ExitStack() as es:
        P = Prog(nc, es)
        C = Ctx(P, nslab=1, slab_elems=64)
        sh1, sc1, g1, sh2, sc2, g2 = setup_common(P, C, modc_d)
        c_g, c_b = load_cols(P, "c_ln2g", ln2_g, KC), load_cols(P, "c_ln2b", ln2_b, KC)
        X = [P.sb([128, NT], F32, "X%d" % k) for k in range(KC)]
        YA = [P.sb([128, NT], F32, "YA%d" % k) for k in range(KC)]
        YB = [P.sb([128, NT], F32, "YB%d" % k) for k in range(KC)]
        OUT = Tl(None, "out")
        for it in range(TC // NT):
            sl = slice(it * NT, (it + 1) * NT)
            for k in range(KC):
                P.dma("sp", X[k], X[k].ap[:], None, x1T[k * 128:(k + 1) * 128, sl])
                P.dma("sp", YA[k], YA[k].ap[:], None, yaT[k * 128:(k + 1) * 128, sl])
                P.dma("sp", YB[k], YB[k].ap[:], None, ybT[k * 128:(k + 1) * 128, sl])
                P.add("pool", (lambda e, o=YA[k].ap[:], b_=YB[k].ap[:]: e.tensor_tensor(o, o, b_, ALU.add)), [YA[k], YB[k]], [YA[k]])
                P.add("act", (lambda e, o=YA[k].ap[:], k=k: e.activation(o, o, AF.Copy, scale=g2.ap[:, k:k + 1])), [YA[k], g2], [YA[k]])
                P.add("dve", (lambda e, o=X[k].ap[:], b_=YA[k].ap[:]: e.scalar_tensor_tensor(o, o, ALPHA, b_, ALU.mult, ALU.add)), [X[k], YA[k]], [X[k]])
            ln_inplace(C, X, NT, c_g, c_b)
            for k in range(KC):
                P.dma("sp", OUT, outT[k * 128:(k + 1) * 128, sl], X[k], X[k].ap[:])
        P.finish([OUT])
        P.emit_all()
    return nc


def run_combine(inp, layer, x1, ya, yb):
    nc = build_combine()
    maps = []
    for core in range(NCORE):
        m = common_maps(inp, layer, core)
        m.update({"x1T": tok_T(x1, core), "yaT": tok_T(ya, core), "ybT": tok_T(yb, core),
                  "ln2_g": cols(inp["ln2_g"][layer]), "ln2_b": cols(inp["ln2_b"][layer])})
        maps.append({k: np.ascontiguousarray(v, dtype=np.float32) for k, v in m.items()})
    res = _run(nc, maps)
    return from_T(res, "outT", D)
```

```python
import math
from contextlib import ExitStack
import numpy as np
import concourse.bass as bass
import concourse.mybir as mybir
from concourse.bass_utils import run_bass_kernel_spmd

AF = mybir.ActivationFunctionType
ALU = mybir.AluOpType
F32 = mybir.dt.float32
BF16 = mybir.dt.bfloat16

D = 2048
KC = 16
SEQ = 4096
NCORE = 8
TC = 2048
NT = 512
DFF = 5632
MC_FF = 44
ALPHA = 8.0 ** 0.25
LN_EPS = 1e-5
HALO = 32
ENGS = ["pe", "act", "dve", "pool", "sp"]


class Tl:
    __slots__ = ("ap", "name", "w", "rs", "rd", "dsem", "dcnt", "al")

    def __init__(self, ap, name):
        self.ap = ap
        self.name = name
        self.w = None
        self.rs = {}
        self.rd = []
        self.dsem = None
        self.dcnt = 0
        self.al = ()

    def __getitem__(self, idx):
        return self.ap[idx]


class Op:
    __slots__ = ("eng", "emit", "waits", "idx", "mark", "cnt", "is_dma", "dsem", "dval", "dtile", "inc")


class Prog:
    def __init__(self, nc, es):
        self.nc = nc
        self.es = es
        self.streams = {e: [] for e in ENGS}
        self.esem = {e: es.enter_context(nc.semaphore("S_" + e)) for e in ENGS}
        self.nsem = 5
        self.uid = 0
        self.wt = {}

    def sb(self, shape, dt, name):
        t = self.es.enter_context(self.nc.sbuf_tensor(name, list(shape), dt))
        return Tl(t, name)

    def ps(self, shape, dt, name):
        t = self.es.enter_context(self.nc.psum_tensor(name, list(shape), dt))
        return Tl(t, name)

    def view(self, ap, name):
        return Tl(ap, name)

    def _touch(self, tiles):
        out = []
        for t in tiles:
            out.append(t)
            out.extend(t.al)
        return out

    def add(self, eng, emit, reads=(), writes=(), dma_tile=None, inc=16):
        op = Op()
        op.inc = inc
        op.eng = eng
        op.emit = emit
        op.idx = len(self.streams[eng])
        op.mark = False
        op.cnt = 0
        op.is_dma = dma_tile is not None
        op.dtile = dma_tile
        op.dsem = None
        op.dval = 0
        reads = self._touch(reads)
        writes = self._touch(writes)
        deps = {}
        ddeps = []

        def dep(d):
            if d is None or d is op:
                return
            if d.is_dma:
                if op.is_dma and d.dtile is dma_tile:
                    return
                ddeps.append(d)
                return
            cur = deps.get(d.eng)
            if cur is None or cur.idx < d.idx:
                deps[d.eng] = d

        for t in reads:
            dep(t.w)
        for t in writes:
            dep(t.w)
            for r in t.rs.values():
                dep(r)
            for r in t.rd:
                dep(r)
        op.waits = []
        for d in ddeps:
            op.waits.append((d.dsem, d.dtile.dcnt))
        for e, d in deps.items():
            if e == eng and not op.is_dma:
                if eng == "pe":
                    continue
                if op.idx - d.idx >= 4:
                    continue
            d.mark = True
            op.waits.append(d)
        for t in reads:
            if op.is_dma:
                t.rd.append(op)
            else:
                t.rs[eng] = op
        for t in writes:
            t.w = op
            t.rs = {}
            t.rd = []
        if op.is_dma:
            if dma_tile.dsem is None:
                dma_tile.dsem = self.es.enter_context(self.nc.semaphore("D%d" % self.nsem))
                self.nsem += 1
            dma_tile.dcnt += inc
            op.dsem = dma_tile.dsem
            op.dval = dma_tile.dcnt
        self.streams[eng].append(op)
        return op

    def dma(self, q, out_t, out_ap, in_t, in_ap):
        if in_t is None:
            try:
                in_t = self.wt.get(in_ap.name)
            except Exception:
                in_t = None
        reads = [in_t] if in_t is not None else []
        writes = [out_t] if out_t is not None else []
        return self.add(q, lambda e: e.dma_start(out=out_ap, in_=in_ap), reads, writes, dma_tile=out_t)

    def gather_into(self, name, rows, cols, slot):
        nc = self.nc
        rs = rows // NCORE
        ext = nc.dram_tensor(name, [rs, cols], F32, kind="ExternalInput").ap()
        if "full" not in slot:
            slot["bnc"] = nc.dram_tensor(slot["nm"] + "_b", [rs, cols], F32)
            slot["full"] = nc.dram_tensor(slot["nm"] + "_f", [rows, cols], F32)
            slot["tb"] = Tl(None, slot["nm"] + "_b")
            slot["tf"] = Tl(None, slot["nm"] + "_f")
            self.wt[slot["full"].ap().name] = slot["tf"]
        bnc, full, tb, tf = slot["bnc"], slot["full"], slot["tb"], slot["tf"]
        step = max(1, (8 << 20) // (cols * 4))
        r = 0
        first = True
        while r < rs:
            r2 = min(rs, r + step)
            self.dma("pool", tb, bnc.ap()[r:r2, :], None, ext[r:r2, :])
            r = r2
        self.add("pool", lambda e: e.collective_compute("AllGather", ALU.bypass, replica_groups=[list(range(NCORE))],
                                                        ins=[bnc.ap()], outs=[full.ap()]),
                 [tb], [tf], dma_tile=tf, inc=1)
        return full.ap()

    def gathered(self, name, rows, cols):
        return self.nc.dram_tensor(name, [rows, cols], F32, kind="ExternalInput").ap()

    def gathered_cc(self, name, rows, cols):
        nc = self.nc
        rs = rows // NCORE
        ext = nc.dram_tensor(name, [rs, cols], F32, kind="ExternalInput").ap()
        bnc = nc.dram_tensor(name + "_b", [rs, cols], F32)
        full = nc.dram_tensor(name + "_f", [rows, cols], F32)
        tb = Tl(None, name + "_b")
        tf = Tl(None, name + "_f")
        step = max(1, (8 << 20) // (cols * 4))
        r = 0
        while r < rs:
            r2 = min(rs, r + step)
            self.dma("pool", tb, bnc.ap()[r:r2, :], None, ext[r:r2, :])
            r = r2
        self.add("pool", lambda e: e.collective_compute("AllGather", ALU.bypass, replica_groups=[list(range(NCORE))],
                                                        ins=[bnc.ap()], outs=[full.ap()]),
                 [tb], [tf], dma_tile=tf, inc=1)
        self.wt[full.ap().name] = tf
        return full.ap()

    def finish(self, tiles):
        self.add("sp", lambda e: e.nop(), reads=list(tiles), writes=[])

    def emit_all(self):
        nc = self.nc
        for e in ENGS:
            c = 0
            for op in self.streams[e]:
                if op.mark:
                    c += 1
                    op.cnt = c
        esem = self.esem
        streams = self.streams

        def run(eng_obj, e):
            waited = {}
            for op in streams[e]:
                for d in op.waits:
                    if isinstance(d, tuple):
                        sem, val = d
                    else:
                        sem, val = esem[d.eng], d.cnt
                    k = id(sem)
                    if waited.get(k, 0) >= val:
                        continue
                    eng_obj.wait_ge(sem, val)
                    waited[k] = val
                ins = op.emit(eng_obj)
                if op.is_dma:
                    ins.then_inc(op.dsem, op.inc)
                elif op.mark:
                    ins.then_inc(esem[e], 1)

        with nc.Block() as block:
            @block.tensor
            def _(x):
                run(x, "pe")

            @block.scalar
            def _(x):
                run(x, "act")

            @block.vector
            def _(x):
                run(x, "dve")

            @block.gpsimd
            def _(x):
                run(x, "pool")

            @block.sync
            def _(x):
                run(x, "sp")


class Ctx:
    def __init__(self, P, nslab=3, slab_elems=11264, ntmp=3, nsmall=0):
        self.P = P
        self.slabs = [P.sb([128, slab_elems], BF16, "slab%d" % i) for i in range(nslab)]
        self.slab_i = 0
        self.small = [P.sb([128, 4096], BF16, "sslab%d" % i) for i in range(nsmall)]
        self.small_i = 0
        self.psum = [P.ps([128, 512], F32, "ps%d" % i) for i in range(6)]
        self.ps_i = 0
        self.pstat = [P.ps([128, 512], F32, "pst%d" % i) for i in range(2)]
        self.ones = P.sb([128, 128], F32, "ones")
        P.add("dve", lambda e: e.memset(self.ones.ap[:], 1.0), [], [self.ones])
        self.tmp = [P.sb([128, 512], F32, "tmp%d" % i) for i in range(ntmp)]
        self.tmp_i = 0
        self.stat = [P.sb([128, 512], F32, "stat%d" % i) for i in range(3)]
        self.ev = 0

    def next_slab(self):
        s = self.slabs[self.slab_i % len(self.slabs)]
        self.slab_i += 1
        return s

    def next_small(self):
        if not self.small:
            return self.next_slab()
        s = self.small[self.small_i % len(self.small)]
        self.small_i += 1
        return s

    def next_ps(self):
        p = self.psum[self.ps_i % len(self.psum)]
        self.ps_i += 1
        return p

    def next_tmp(self):
        t = self.tmp[self.tmp_i % len(self.tmp)]
        self.tmp_i += 1
        return t


def linear(C, w_ap, K, col0, ncols, slabw, rhs_groups, epilogue, m_order=None):
    P = C.P
    kc = K // 128
    wv = w_ap.rearrange("(k p) n -> p k n", p=128)
    nslab = ncols // slabw
    for s in range(nslab):
        slab = C.next_slab()
        c0 = col0 + s * slabw
        sv = slab.ap[:, 0:kc * slabw].rearrange("p (k n) -> p k n", k=kc)
        P.dma("pool", slab, sv, None, wv[:, :, c0:c0 + slabw])
        for j in range(slabw // 128):
            mi = (c0 // 128) + j
            for gi, (rt, rf) in enumerate(rhs_groups):
                ps = C.next_ps()
                for k in range(kc):
                    lhs = sv[:, k, j * 128:(j + 1) * 128]
                    rhs = rf(k)
                    nn = rhs.shape[-1]
                    P.add("pe", (lambda e, o=ps.ap[:, 0:nn], l=lhs, r=rhs, st=(k == 0), sp=(k == kc - 1):
                                 e.matmul(o, l, r, start=st, stop=sp)),
                          [slab, rt[k]], [ps])
                epilogue(mi, gi, ps)


def ln_stats(C, ztiles, n, width_sel=None):
    P = C.P
    ps1, ps2 = C.pstat
    nk = len(ztiles)
    for k in range(nk):
        zt, za = ztiles[k]
        P.add("pe", (lambda e, o=ps1.ap[:, 0:n], r=za, st=(k == 0), sp=(k == nk - 1):
                     e.matmul(o, C.ones.ap[:], r, start=st, stop=sp)), [C.ones, zt], [ps1])
    for k in range(nk):
        zt, za = ztiles[k]
        sq = C.next_tmp()
        P.add("act", (lambda e, o=sq.ap[:, 0:n], i=za: e.activation(o, i, AF.Square)), [zt], [sq])
        P.add("pe", (lambda e, o=ps2.ap[:, 0:n], r=sq.ap[:, 0:n], st=(k == 0), sp=(k == nk - 1):
                     e.matmul(o, C.ones.ap[:], r, start=st, stop=sp)), [C.ones, sq], [ps2])
    mean, ex2, rstd = C.stat
    invd = 1.0 / (128.0 * nk)
    P.add("act", lambda e: e.activation(mean.ap[:, 0:n], ps1.ap[:, 0:n], AF.Copy, scale=invd), [ps1], [mean])
    P.add("act", lambda e: e.activation(ex2.ap[:, 0:n], ps2.ap[:, 0:n], AF.Copy, scale=invd), [ps2], [ex2])
    P.add("dve", lambda e: e.tensor_tensor(rstd.ap[:, 0:n], mean.ap[:, 0:n], mean.ap[:, 0:n], ALU.mult), [mean], [rstd])
    P.add("dve", lambda e: e.tensor_tensor(ex2.ap[:, 0:n], ex2.ap[:, 0:n], rstd.ap[:, 0:n], ALU.subtract), [ex2, rstd], [ex2])
    P.add("dve", lambda e: e.tensor_scalar(ex2.ap[:, 0:n], ex2.ap[:, 0:n], LN_EPS, None, ALU.add), [ex2], [ex2])
    P.add("act", lambda e: e.activation(ex2.ap[:, 0:n], ex2.ap[:, 0:n], AF.Sqrt), [ex2], [ex2])
    P.add("dve", lambda e: e.reciprocal(rstd.ap[:, 0:n], ex2.ap[:, 0:n]), [ex2], [rstd])
    return mean, rstd


def ln_apply(C, zt, za, outs, n, mean, rstd, gcol, bcol, func=AF.Identity, extra=None):
    P = C.P
    P.add("dve", lambda e: e.tensor_tensor(za, za, mean.ap[:, 0:n], ALU.subtract), [zt, mean], [zt])
    P.add("dve", lambda e: e.tensor_tensor(za, za, rstd.ap[:, 0:n], ALU.mult), [zt, rstd], [zt])
    ot, oa = outs
    P.add("act", lambda e: e.activation(oa, za, func, bias=bcol, scale=gcol), [zt], [ot])
    if extra is not None:
        xt, xa, sc, bc = extra
        P.add("act", lambda e: e.activation(xa, oa, AF.Identity, bias=bc, scale=sc), [ot], [xt])


def compute_mod(C, cvec_ap, ada_w_ap, ada_b_ap, which, modc):
    P = C.P
    craw = P.sb([128, KC], F32, "craw")
    cb = P.sb([128, KC], BF16, "cbf")
    P.dma("sp", craw, craw.ap[:], None, cvec_ap)
    P.add("act", lambda e: e.activation(cb.ap[:], craw.ap[:], AF.Silu), [craw], [cb])
    bcols = P.sb([128, 6 * KC], F32, "adab_cols")
    P.dma("sp", bcols, bcols.ap[:], None, ada_b_ap)
    one11 = P.sb([1, 1], F32, "one11")
    P.add("dve", lambda e: e.memset(one11.ap[:], 1.0), [], [one11])
    wv = ada_w_ap.rearrange("(k p) n -> p k n", p=128)
    i = 0
    for v in which:
        for s in range(4):
            slab = C.next_slab()
            c0 = v * D + s * 512
            sv = slab.ap[:, 0:KC * 512].rearrange("p (k n) -> p k n", k=KC)
            P.dma("pool", slab, sv, None, wv[:, :, c0:c0 + 512])
            ps = C.next_ps()
            for k in range(KC):
                P.add("pe", (lambda e, o=ps.ap[0:1, :], l=cb.ap[:, k:k + 1], r=sv[:, k, :], st=(k == 0), sp=(k == KC - 1):
                             e.matmul(o, l, r, start=st, stop=sp)), [slab, cb], [ps])
            mr = C.next_tmp()
            P.add("dve", (lambda e, o=mr.ap[0:1, :], a=ps.ap[0:1, :]: e.tensor_copy(o, a)), [ps], [mr])
            ps2 = C.next_ps()
            for k in range(4):
                P.add("pe", (lambda e, o=ps2.ap[:, k:k + 1], l=mr.ap[0:1, k * 128:(k + 1) * 128]:
                             e.matmul(o, l, one11.ap[0:1, 0:1], start=True, stop=True)), [mr, one11], [ps2])
            cc = v * KC + s * 4
            P.add("dve", (lambda e, o=modc.ap[:, cc:cc + 4], i_=ps2.ap[:, 0:4], b=bcols.ap[:, cc:cc + 4]:
                          e.tensor_tensor(o, i_, b, ALU.add)), [ps2, bcols], [modc])


def load_cols(P, name, ap_1d, n):
    t = P.sb([128, n], F32, name)
    P.dma("sp", t, t.ap[:], None, ap_1d)
    return t


def ffn_phase(C, HB, X, w1_ap, w3_ap, w2_ap, G, g2col, n, gate=None, ACC=None, last=True):
    P = C.P
    SW = 256
    for s in range(DFF // SW):
        sl1 = C.next_small()
        sl3 = C.next_small()
        c0 = s * SW
        v1 = sl1.ap[:, 0:KC * SW].rearrange("p (k n) -> p k n", k=KC)
        v3 = sl3.ap[:, 0:KC * SW].rearrange("p (k n) -> p k n", k=KC)
        P.dma("pool", sl1, v1, None, w1_ap.rearrange("(k p) n -> p k n", p=128)[:, :, c0:c0 + SW])
        P.dma("pool", sl3, v3, None, w3_ap.rearrange("(k p) n -> p k n", p=128)[:, :, c0:c0 + SW])
        for j in range(SW // 128):
            m = c0 // 128 + j
            p1 = C.next_ps()
            p3 = C.next_ps()
            for k in range(KC):
                P.add("pe", (lambda e, o=p1.ap[:, 0:n], l=v1[:, k, j * 128:(j + 1) * 128], r=HB[k].ap[:, 0:n], st=(k == 0), sp=(k == KC - 1):
                             e.matmul(o, l, r, start=st, stop=sp)), [sl1, HB[k]], [p1])
            for k in range(KC):
                P.add("pe", (lambda e, o=p3.ap[:, 0:n], l=v3[:, k, j * 128:(j + 1) * 128], r=HB[k].ap[:, 0:n], st=(k == 0), sp=(k == KC - 1):
                             e.matmul(o, l, r, start=st, stop=sp)), [sl3, HB[k]], [p3])
            t = C.next_tmp()
            P.add("act", (lambda e, o=t.ap[:, 0:n], i=p1.ap[:, 0:n]: e.activation(o, i, AF.Silu)), [p1], [t])
            if gate is None:
                P.add("dve", (lambda e, o=G[m].ap[:, 0:n], a=t.ap[:, 0:n], b=p3.ap[:, 0:n]: e.tensor_tensor(o, a, b, ALU.mult)), [t, p3], [G[m]])
            else:
                P.add("dve", (lambda e, o=t.ap[:, 0:n], a=t.ap[:, 0:n], b=p3.ap[:, 0:n]: e.tensor_tensor(o, a, b, ALU.mult)), [t, p3], [t])
                P.add("dve", (lambda e, o=G[m].ap[:, 0:n], a=t.ap[:, 0:n], b=gate.ap[:, 0:n]: e.tensor_tensor(o, a, b, ALU.mult)), [t, gate], [G[m]])
    SW2 = 256
    for s in range(D // SW2):
        sl = C.next_slab()
        c0 = s * SW2
        v2 = sl.ap[:, 0:MC_FF * SW2].rearrange("p (k n) -> p k n", k=MC_FF)
        P.dma("pool", sl, v2, None, w2_ap.rearrange("(k p) n -> p k n", p=128)[:, :, c0:c0 + SW2])
        for j in range(SW2 // 128):
            c = c0 // 128 + j
            ps = C.next_ps()
            for m in range(MC_FF):
                P.add("pe", (lambda e, o=ps.ap[:, 0:n], l=v2[:, m, j * 128:(j + 1) * 128], r=G[m].ap[:, 0:n], st=(m == 0), sp=(m == MC_FF - 1):
                             e.matmul(o, l, r, start=st, stop=sp)), [sl, G[m]], [ps])
            if gate is None:
                t = C.next_tmp()
                P.add("act", (lambda e, o=t.ap[:, 0:n], i=ps.ap[:, 0:n], c=c: e.activation(o, i, AF.Copy, scale=g2col.ap[:, c:c + 1])), [ps, g2col], [t])
                P.add("dve", (lambda e, o=X[c].ap[:, 0:n], a=X[c].ap[:, 0:n], b=t.ap[:, 0:n]:
                              e.scalar_tensor_tensor(o, a, ALPHA, b, ALU.mult, ALU.add)), [X[c], t], [X[c]])
            else:
                P.add("dve", (lambda e, o=X[c].ap[:, 0:n], p_=ps.ap[:, 0:n], c=c:
                              e.scalar_tensor_tensor(o, p_, g2col.ap[:, c:c + 1], o, ALU.mult, ALU.add)), [X[c], ps, g2col], [X[c]])


def ln_inplace(C, X, n, gcol, bcol, HB=None, sccol=None, shcol=None):
    mean, rstd = ln_stats(C, [(X[c], X[c].ap[:, 0:n]) for c in range(KC)], n)
    for c in range(KC):
        extra = None
        if HB is not None:
            extra = (HB[c], HB[c].ap[:, 0:n], sccol.ap[:, c:c + 1], shcol.ap[:, c:c + 1])
        ln_apply(C, X[c], X[c].ap[:, 0:n], (X[c], X[c].ap[:, 0:n]), n, mean, rstd,
                 gcol.ap[:, c:c + 1], bcol.ap[:, c:c + 1], AF.Identity, extra)


def build_layer0():
    nc = bass.Bass("TRN2", target_bir_lowering=False)
    dr = lambda name, shape: nc.dram_tensor(name, list(shape), F32, kind="ExternalInput").ap()
    xT = dr("xT", [D, HALO + TC])
    modc_d = dr("modc", [128, 6 * KC])
    hmask = dr("hmask", [128, 1])
    CV = [128, KC]
    ln1_g, ln1_b, ln2_g, ln2_b = dr("ln1_g", CV), dr("ln1_b", CV), dr("ln2_g", CV), dr("ln2_b", CV)
    b_in = dr("a_b_in", [128, 2 * KC])
    w_dw, b_dw = dr("a_w_dw", [128, KC, 31]), dr("a_b_dw", CV)
    aln_g, aln_b = dr("a_ln_g", CV), dr("a_ln_b", CV)
    b_out = dr("a_b_out", CV)
    outT = nc.dram_tensor("outT", [D, TC], F32, kind="ExternalOutput").ap()
    with ExitStack() as es:
        P = Prog(nc, es)
        w_in = P.gathered("a_w_in", D, 2 * D)
        w_out = P.gathered("a_w_out", D, D)
        w1, w3, w2 = P.gathered("f_w1", D, DFF), P.gathered("f_w3", D, DFF), P.gathered("f_w2", DFF, D)
        C = Ctx(P)
        sh1, sc1, g1, sh2, sc2, g2 = setup_common(P, C, modc_d)
        sc1p = P.sb([128, KC], F32, "sc1p")
        sc2p = P.sb([128, KC], F32, "sc2p")
        P.add("dve", lambda e: e.tensor_scalar(sc1p.ap[:], sc1.ap[:], 1.0, None, ALU.add), [sc1], [sc1p])
        P.add("dve", lambda e: e.tensor_scalar(sc2p.ap[:], sc2.ap[:], 1.0, None, ALU.add), [sc2], [sc2p])
        c_ln1g, c_ln1b = load_cols(P, "c_ln1g", ln1_g, KC), load_cols(P, "c_ln1b", ln1_b, KC)
        c_ln2g, c_ln2b = load_cols(P, "c_ln2g", ln2_g, KC), load_cols(P, "c_ln2b", ln2_b, KC)
        c_bin = load_cols(P, "c_bin", b_in, 2 * KC)
        c_bdw = load_cols(P, "c_bdw", b_dw, KC)
        c_alng, c_alnb = load_cols(P, "c_alng", aln_g, KC), load_cols(P, "c_alnb", aln_b, KC)
        c_bout = load_cols(P, "c_bout", b_out, KC)
        c_wdw = P.sb([128, KC, 31], F32, "c_wdw")
        P.dma("sp", c_wdw, c_wdw.ap[:], None, w_dw)
        c_mask = P.sb([128, 1], F32, "c_mask")
        P.dma("sp", c_mask, c_mask.ap[:], None, hmask)
        g1b = P.sb([128, KC], F32, "g1b")
        P.add("dve", lambda e: e.tensor_tensor(g1b.ap[:], g1.ap[:], c_bout.ap[:], ALU.mult), [g1, c_bout], [g1b])

        X = [P.sb([128, NT], F32, "X%d" % k) for k in range(KC)]
        XH = [P.sb([128, HALO], F32, "XH%d" % k) for k in range(KC)]
        HB = [P.sb([128, NT], BF16, "HB%d" % k) for k in range(KC)]
        HBH = [P.sb([128, HALO], BF16, "HBH%d" % k) for k in range(KC)]
        arena = P.sb([128, 16 * (HALO + NT) + 16 * NT], F32, "arena")
        Pp, U = [], []
        for k in range(KC):
            t = Tl(arena.ap[:, k * (HALO + NT):(k + 1) * (HALO + NT)], "Pp%d" % k)
            Pp.append(t)
        off = KC * (HALO + NT)
        for k in range(KC):
            t = Tl(arena.ap[:, off + k * NT:off + (k + 1) * NT], "U%d" % k)
            U.append(t)
        gview = arena.ap[:, 0:MC_FF * NT // 2].bitcast(BF16)
        G = []
        for m in range(MC_FF):
            t = Tl(gview[:, m * NT:(m + 1) * NT], "G%d" % m)
            G.append(t)
        spans = [(Pp[k], k * (HALO + NT), (k + 1) * (HALO + NT)) for k in range(KC)] + \
                [(U[k], off + k * NT, off + (k + 1) * NT) for k in range(KC)]
        for m in range(MC_FF):
            lo, hi = m * NT // 2, (m + 1) * NT // 2
            al = [t for (t, a, b) in spans if a < hi and lo < b]
            G[m].al = tuple(al)
            for t in al:
                t.al = tuple(list(t.al) + [G[m]])
        HALOS = [P.sb([128, HALO], F32, "HL%d" % k) for k in range(KC)]

        ntile = TC // NT
        for it in range(ntile):
            t0 = HALO + it * NT
            for k in range(KC):
                P.dma("sp", X[k], X[k].ap[:], None, xT[k * 128:(k + 1) * 128, t0:t0 + NT])
                P.add("act", (lambda e, o=HB[k].ap[:], i=X[k].ap[:], k=k:
                              e.activation(o, i, AF.Identity, bias=sh1.ap[:, k:k + 1], scale=sc1p.ap[:, k:k + 1])), [X[k], sh1, sc1p], [HB[k]])
            groups = [(HB, lambda k: HB[k].ap[:])]
            if it == 0:
                for k in range(KC):
                    P.dma("sp", XH[k], XH[k].ap[:], None, xT[k * 128:(k + 1) * 128, 0:HALO])
                    P.add("act", (lambda e, o=HBH[k].ap[:], i=XH[k].ap[:], k=k:
                                  e.activation(o, i, AF.Identity, bias=sh1.ap[:, k:k + 1], scale=sc1p.ap[:, k:k + 1])), [XH[k], sh1, sc1p], [HBH[k]])
                groups.append((HBH, lambda k: HBH[k].ap[:]))
            else:
                for k in range(KC):
                    P.add("pool", (lambda e, o=Pp[k].ap[:, 0:HALO], i=HALOS[k].ap[:]: e.tensor_copy(o, i)), [HALOS[k]], [Pp[k]])
            wv = w_in.rearrange("(k p) n -> p k n", p=128)
            for s in range(4):
                sa = C.next_slab()
                sg = C.next_slab()
                va = sa.ap[:, 0:KC * 512].rearrange("p (k n) -> p k n", k=KC)
                vg = sg.ap[:, 0:KC * 512].rearrange("p (k n) -> p k n", k=KC)
                P.dma("pool", sa, va, None, wv[:, :, s * 512:(s + 1) * 512])
                P.dma("pool", sg, vg, None, wv[:, :, D + s * 512:D + (s + 1) * 512])
                for j in range(4):
                    m = s * 4 + j
                    for gi, (rt, rf) in enumerate(groups):
                        nn = NT if gi == 0 else HALO
                        pa = C.next_ps()
                        pg = C.next_ps()
                        for k in range(KC):
                            P.add("pe", (lambda e, o=pa.ap[:, 0:nn], l=va[:, k, j * 128:(j + 1) * 128], r=rf(k), st=(k == 0), sp=(k == KC - 1):
                                         e.matmul(o, l, r, start=st, stop=sp)), [sa, rt[k]], [pa])
                        for k in range(KC):
                            P.add("pe", (lambda e, o=pg.ap[:, 0:nn], l=vg[:, k, j * 128:(j + 1) * 128], r=rf(k), st=(k == 0), sp=(k == KC - 1):
                                         e.matmul(o, l, r, start=st, stop=sp)), [sg, rt[k]], [pg])
                        t = C.next_tmp()
                        P.add("act", (lambda e, o=t.ap[:, 0:nn], i=pg.ap[:, 0:nn], m=m:
                                      e.activation(o, i, AF.Sigmoid, bias=c_bin.ap[:, KC + m:KC + m + 1])), [pg, c_bin], [t])
                        dst = Pp[m].ap[:, HALO:] if gi == 0 else Pp[m].ap[:, 0:HALO]
                        P.add("dve", (lambda e, o=dst, a=pa.ap[:, 0:nn], b=t.ap[:, 0:nn], m=m:
                                      e.scalar_tensor_tensor(o, a, c_bin.ap[:, m:m + 1], b, ALU.add, ALU.mult)), [pa, t, c_bin], [Pp[m]])
                        if gi == 1:
                            P.add("dve", (lambda e, o=dst: e.tensor_scalar(o, o, c_mask.ap[:, 0:1], None, ALU.mult)), [Pp[m], c_mask], [Pp[m]])
            for m in range(KC):
                eng = "dve"
                P.add(eng, (lambda e, o=U[m].ap[:], i=Pp[m].ap[:, 2:2 + NT], m=m:
                            e.tensor_scalar(o, i, c_wdw.ap[:, m, 0:1], c_bdw.ap[:, m:m + 1], ALU.mult, ALU.add)), [Pp[m], c_wdw, c_bdw], [U[m]])
                for j in range(1, 31):
                    P.add(eng, (lambda e, o=U[m].ap[:], i=Pp[m].ap[:, 2 + j:2 + j + NT], m=m, j=j:
                                e.scalar_tensor_tensor(o, i, c_wdw.ap[:, m, j:j + 1], o, ALU.mult, ALU.add)), [Pp[m], U[m], c_wdw], [U[m]])
                P.add("pool", (lambda e, o=HALOS[m].ap[:], i=Pp[m].ap[:, NT:NT + HALO]: e.tensor_copy(o, i)), [Pp[m]], [HALOS[m]])
            mean, rstd = ln_stats(C, [(U[m], U[m].ap[:]) for m in range(KC)], NT)
            for m in range(KC):
                ln_apply(C, U[m], U[m].ap[:], (HB[m], HB[m].ap[:]), NT, mean, rstd,
                         c_alng.ap[:, m:m + 1], c_alnb.ap[:, m:m + 1], AF.Silu)
            def epi_out(mi, gi, ps):
                t = C.next_tmp()
                P.add("act", (lambda e, o=t.ap[:], i=ps.ap[:]: e.activation(o, i, AF.Identity, bias=g1b.ap[:, mi:mi + 1], scale=g1.ap[:, mi:mi + 1])), [ps, g1b, g1], [t])
                P.add("dve", (lambda e, o=X[mi].ap[:], b=t.ap[:]: e.scalar_tensor_tensor(o, o, ALPHA, b, ALU.mult, ALU.add)), [X[mi], t], [X[mi]])
            linear(C, w_out, D, 0, D, 512, [(HB, lambda k: HB[k].ap[:])], epi_out)
            ln_inplace(C, X, NT, c_ln1g, c_ln1b, HB, sc2p, sh2)
            ffn_phase(C, HB, X, w1, w3, w2, G, g2, NT)
            ln_inplace(C, X, NT, c_ln2g, c_ln2b)
            outt = Tl(None, "out")
            if it == 0:
                OUT = outt
            for k in range(KC):
                P.dma("sp", OUT, outT[k * 128:(k + 1) * 128, it * NT:(it + 1) * NT], X[k], X[k].ap[:])
        P.finish([OUT])
        P.emit_all()
    return nc


def cols(v):
    v = np.asarray(v, np.float32).reshape(-1)
    return np.ascontiguousarray(v.reshape(-1, 128).T)


def shard_rows(w2d, core):
    w2d = np.asarray(w2d, np.float32)
    return w2d.reshape(-1, w2d.shape[-1])


def _run(nc, in_maps):
    res = run_bass_kernel_spmd(nc, in_maps, core_ids=list(range(NCORE)))
    return res.results


def run_layer0(inp, x):
    nc = build_layer0()
    in_maps = []
    for core in range(NCORE):
        b, h = core // 2, core % 2
        xT = np.zeros((D, HALO + TC), np.float32)
        t0 = h * TC
        xT[:, HALO:] = x[b, t0:t0 + TC, :].T
        if h == 1:
            xT[:, :HALO] = x[b, t0 - HALO:t0, :].T
        m = {
            "xT": xT, "modc": cols(inp["_mod"][0][b]),
            "hmask": np.full((128, 1), float(h), np.float32),
            "ln1_g": cols(inp["ln1_g"][0]), "ln1_b": cols(inp["ln1_b"][0]), "ln2_g": cols(inp["ln2_g"][0]), "ln2_b": cols(inp["ln2_b"][0]),
            "a_w_in": shard_rows(inp["a_w_in"][0], core), "a_b_in": cols(inp["a_b_in"][0]),
            "a_w_dw": inp["a_w_dw"][0].T.reshape(KC, 128, 31).transpose(1, 0, 2), "a_b_dw": cols(inp["a_b_dw"][0]),
            "a_ln_g": cols(inp["a_ln_g"][0]), "a_ln_b": cols(inp["a_ln_b"][0]), "a_w_out": shard_rows(inp["a_w_out"][0], core), "a_b_out": cols(inp["a_b_out"][0]),
            "f_w1": shard_rows(inp["f_w1"][0], core), "f_w3": shard_rows(inp["f_w3"][0], core), "f_w2": shard_rows(inp["f_w2"][0], core),
        }
        in_maps.append({k: np.ascontiguousarray(v, dtype=np.float32) for k, v in m.items()})
    res = _run(nc, in_maps)
    out = np.empty_like(x)
    for core in range(NCORE):
        b, h = core // 2, core % 2
        out[b, h * TC:(h + 1) * TC, :] = res[core]["outT"].T
    return out


def setup_common(P, C, modc_d):
    modc = P.sb([128, 6 * KC], F32, "modc_sb")
    P.dma("sp", modc, modc.ap[:], None, modc_d)
    vs = [Tl(modc.ap[:, v * KC:(v + 1) * KC], "m%d" % v) for v in range(6)]
    for t in vs:
        t.al = (modc,)
    return vs


def plus_one(P, t, name):
    o = P.sb([128, KC], F32, name)
    P.add("dve", lambda e: e.tensor_scalar(o.ap[:], t.ap[:], 1.0, None, ALU.add), [t], [o])
    return o


def build_head(ncols, layer_tag):
    nc = bass.Bass("TRN2", target_bir_lowering=False)
    dr = lambda name, shape: nc.dram_tensor(name, list(shape), F32, kind="ExternalInput").ap()
    xT = dr("xT", [D, TC])
    modc_d = dr("modc", [128, 6 * KC])
    nm = (ncols + 127) // 128
    ncp = nm * 128
    b = dr("b", [128, nm])
    outT = nc.dram_tensor("outT", [ncp, TC], F32, kind="ExternalOutput").ap()
    with ExitStack() as es:
        P = Prog(nc, es)
        w = P.gathered("w", D, ncp)
        C = Ctx(P)
        sh1, sc1, g1, sh2, sc2, g2 = setup_common(P, C, modc_d)
        sc1p = plus_one(P, sc1, "sc1p")
        c_b = load_cols(P, "c_b", b, nm)
        X = [P.sb([128, NT], F32, "X%d" % k) for k in range(KC)]
        HB = [P.sb([128, NT], BF16, "HB%d" % k) for k in range(KC)]
        O = [P.sb([128, NT], F32, "O%d" % k) for k in range(4)]
        OUT = Tl(None, "out")
        oi = [0]
        for it in range(TC // NT):
            for k in range(KC):
                P.dma("sp", X[k], X[k].ap[:], None, xT[k * 128:(k + 1) * 128, it * NT:(it + 1) * NT])
                P.add("act", (lambda e, o=HB[k].ap[:], i=X[k].ap[:], k=k:
                              e.activation(o, i, AF.Identity, bias=sh1.ap[:, k:k + 1], scale=sc1p.ap[:, k:k + 1])), [X[k], sh1, sc1p], [HB[k]])

            def epi(mi, gi, ps):
                o = O[oi[0] % 4]
                oi[0] += 1
                P.add("act", (lambda e, o_=o.ap[:], i=ps.ap[:]: e.activation(o_, i, AF.Identity, bias=c_b.ap[:, mi:mi + 1])), [ps, c_b], [o])
                P.dma("sp", OUT, outT[mi * 128:(mi + 1) * 128, it * NT:(it + 1) * NT], o, o.ap[:])
            full = (ncp // 512) * 512
            if full:
                linear(C, w, D, 0, full, 512, [(HB, lambda k: HB[k].ap[:])], epi)
            if ncp - full:
                linear(C, w, D, full, ncp - full, ncp - full, [(HB, lambda k: HB[k].ap[:])], epi)
        P.finish([OUT])
        P.emit_all()
    return nc


def moe_router(C, X, sc2p, sh2, wr_sb, n, hf_cb=None):
    P = C.P
    nch = n // 128
    pls = [C.next_ps() for _ in range(nch)]
    for k in range(KC):
        t = C.next_tmp()
        P.add("act", (lambda e, o=t.ap[:, 0:n], i=X[k].ap[:, 0:n], k=k:
                      e.activation(o, i, AF.Identity, bias=sh2.ap[:, k:k + 1], scale=sc2p.ap[:, k:k + 1])), [X[k], sh2, sc2p], [t])
        for j in range(nch):
            P.add("pe", (lambda e, o=pls[j].ap[:, 0:8], l=t.ap[:, j * 128:(j + 1) * 128], r=wr_sb.ap[:, k, :], st=(k == 0), sp=(k == KC - 1):
                         e.matmul(o, l, r, start=st, stop=sp)), [t, wr_sb], [pls[j]])
        if hf_cb is not None:
            hf_cb(k, t)
    if not hasattr(C, "rt"):
        C.rt = [P.sb([128, 4, 8], F32, nm) for nm in ("lg", "l2", "eq1", "eq2")] + [P.sb([128, 4], F32, nm) for nm in ("m1", "m2", "ga", "gb")]
    lg, l2, eq1, eq2, m1, m2, ga, gb = C.rt
    v3 = lambda t: t.ap[:, 0:nch, :]
    bc = lambda t: t.ap[:, 0:nch].unsqueeze(2).to_broadcast([128, nch, 8])
    for j in range(nch):
        P.add("dve", (lambda e, j=j: e.tensor_copy(lg.ap[:, j, :], pls[j].ap[:, 0:8])), [pls[j]], [lg])
    P.add("dve", lambda e: e.tensor_reduce(m1.ap[:, 0:nch], v3(lg), mybir.AxisListType.X, ALU.max), [lg], [m1])
    P.add("dve", lambda e: e.tensor_tensor(v3(eq1), v3(lg), bc(m1), ALU.is_equal), [lg, m1], [eq1])
    P.add("dve", lambda e: e.scalar_tensor_tensor(v3(l2), v3(eq1), -1e30, v3(lg), ALU.mult, ALU.add), [eq1, lg], [l2])
    P.add("dve", lambda e: e.tensor_reduce(m2.ap[:, 0:nch], v3(l2), mybir.AxisListType.X, ALU.max), [l2], [m2])
    P.add("dve", lambda e: e.tensor_tensor(v3(eq2), v3(l2), bc(m2), ALU.is_equal), [l2, m2], [eq2])
    P.add("dve", lambda e: e.tensor_tensor(gb.ap[:, 0:nch], m2.ap[:, 0:nch], m1.ap[:, 0:nch], ALU.subtract), [m1, m2], [gb])
    P.add("act", lambda e: e.activation(gb.ap[:, 0:nch], gb.ap[:, 0:nch], AF.Exp), [gb], [gb])
    P.add("dve", lambda e: e.tensor_scalar(ga.ap[:, 0:nch], gb.ap[:, 0:nch], 1.0, None, ALU.add), [gb], [ga])
    P.add("dve", lambda e: e.reciprocal(ga.ap[:, 0:nch], ga.ap[:, 0:nch]), [ga], [ga])
    P.add("dve", lambda e: e.tensor_tensor(gb.ap[:, 0:nch], gb.ap[:, 0:nch], ga.ap[:, 0:nch], ALU.mult), [ga, gb], [gb])
    P.add("dve", lambda e: e.tensor_tensor(v3(eq1), v3(eq1), bc(ga), ALU.mult), [eq1, ga], [eq1])
    P.add("dve", lambda e: e.tensor_tensor(v3(eq2), v3(eq2), bc(gb), ALU.mult), [eq2, gb], [eq2])
    P.add("dve", lambda e: e.tensor_tensor(v3(eq1), v3(eq1), v3(eq2), ALU.add), [eq1, eq2], [eq1])
    return eq1

GELU_C = 2.0 * math.sqrt(2.0 / math.pi)


def gelu_ops(C, xb_t, xb, out_t, out, n):
    P = C.P
    g = C.next_tmp()
    ga = g.ap[:, 0:n]
    P.add("act", lambda e: e.activation(ga, xb, AF.Square), [xb_t], [g])
    P.add("dve", lambda e: e.tensor_scalar(ga, ga, 0.044715, 1.0, ALU.mult, ALU.add), [g], [g])
    P.add("pool", lambda e: e.tensor_tensor(ga, ga, xb, ALU.mult), [g, xb_t], [g])
    P.add("act", lambda e: e.activation(ga, ga, AF.Sigmoid, scale=GELU_C), [g], [g])
    P.add("pool", lambda e: e.tensor_tensor(out, xb, ga, ALU.mult), [g, xb_t], [out_t])


DBG_TAIL = False


def build_tail(glu, moe, gmlp=False):
    nc = bass.Bass("TRN2", target_bir_lowering=False)
    dr = lambda name, shape: nc.dram_tensor(name, list(shape), F32, kind="ExternalInput").ap()
    xT = dr("xT", [D, TC])
    if not gmlp:
        yT = dr("yT", [D, TC])
    modc_d = dr("modc", [128, 6 * KC])
    CV = [128, KC]
    ln1_g, ln1_b, ln2_g, ln2_b = dr("ln1_g", CV), dr("ln1_b", CV), dr("ln2_g", CV), dr("ln2_b", CV)
    if gmlp:
        d_bi = dr("d_bi", [128, KC])
        d_bv = dr("d_bv", [128, D])
        d_lng, d_lnb = dr("d_lng", CV), dr("d_lnb", CV)
        d_wspT = dr("d_wspT", [128, 8, 128])
        d_mask = dr("d_mask", [128, 128])
        d_bsp = dr("d_bsp", [128, 8, 128])
    ncol = 2 * D if glu else D
    b = dr("b", [128, ncol // 128])
    if moe:
        wr = dr("wr", [128, KC, 8])
        idn = dr("idn", [128, 128])
    if not moe:
        outT = nc.dram_tensor("outT", [D, TC], F32, kind="ExternalOutput").ap()
    with ExitStack() as es:
        P = Prog(nc, es)
        w = P.gathered("w", D, ncol)
        if gmlp:
            w_gin = P.gathered("w_gin", D, 2 * D)
        if moe:
            x1T = nc.dram_tensor("x1T", [D, TC], F32, kind="ExternalOutput").ap()
            hfT = nc.dram_tensor("hfT", [D, TC], F32, kind="ExternalOutput").ap()
            gm_d = nc.dram_tensor("gm", [TC, 8], F32, kind="ExternalOutput").ap()
        else:
            w1, w3, w2 = P.gathered("w1", D, DFF), P.gathered("w3", D, DFF), P.gathered("w2", DFF, D)
        C = Ctx(P, nslab=(2 if (gmlp or not moe) else 3), ntmp=(4 if gmlp else 3), nsmall=(6 if (not moe and not gmlp) else 0))
        sh1, sc1, g1, sh2, sc2, g2 = setup_common(P, C, modc_d)
        sc2p = plus_one(P, sc2, "sc2p")
        if gmlp:
            sc1p = plus_one(P, sc1, "sc1p")
        c_ln1g, c_ln1b = load_cols(P, "c_ln1g", ln1_g, KC), load_cols(P, "c_ln1b", ln1_b, KC)
        c_ln2g, c_ln2b = load_cols(P, "c_ln2g", ln2_g, KC), load_cols(P, "c_ln2b", ln2_b, KC)
        c_b = load_cols(P, "c_b", b, ncol // 128)
        X = [P.sb([128, NT], F32, "X%d" % k) for k in range(KC)]
        HB = [P.sb([128, NT], BF16, "HB%d" % k) for k in range(KC)]
        if not gmlp:
            G = [P.sb([128, NT], BF16, "G%d" % m) for m in range(MC_FF)]
        else:
            arena = P.sb([128, MC_FF * NT], BF16, "arena")
            G = [Tl(arena.ap[:, m * NT:(m + 1) * NT], "G%d" % m) for m in range(MC_FF)]
            vview = arena.ap[:, 0:16384].bitcast(F32)
            V = [Tl(vview[:, j * D:(j + 1) * D], "V%d" % j) for j in range(4)]
            VB = [Tl(arena.ap[:, 16384 + j * D:16384 + (j + 1) * D], "VB%d" % j) for j in range(3)] + [Tl(arena.ap[:, 0:D], "VB3")]
            for j in range(4):
                V[j].al = tuple(G[8 * j:8 * j + 8]) + ((VB[3],) if j == 0 else ())
            for j in range(3):
                VB[j].al = tuple(G[32 + 4 * j:32 + 4 * j + 4])
            VB[3].al = tuple(G[0:4]) + (V[0],)
            for m in range(MC_FF):
                al = []
                if m < 32:
                    al.append(V[m // 8])
                    if m < 4:
                        al.append(VB[3])
                else:
                    al.append(VB[(m - 32) // 4])
                G[m].al = tuple(al)
            U = [P.sb([128, NT], BF16, "U%d" % k) for k in range(KC)]
            c_dbi = load_cols(P, "c_dbi", d_bi, KC)
            c_dlng, c_dlnb = load_cols(P, "c_dlng", d_lng, KC), load_cols(P, "c_dlnb", d_lnb, KC)
            BR = P.sb([128, D], F32, "BR")
            P.dma("sp", BR, BR.ap[:], None, d_bv)
            WTf = P.sb([128, 8, 128], F32, "WTf")
            P.dma("sp", WTf, WTf.ap[:], None, d_wspT)
            MK = P.sb([128, 128], F32, "MK")
            P.dma("sp", MK, MK.ap[:], None, d_mask)
            BSP = P.sb([128, 8, 128], F32, "BSP")
            P.dma("sp", BSP, BSP.ap[:], None, d_bsp)
            WT = P.sb([128, 8, 128], BF16, "WT")
            RS = P.sb([128, 8, 128], F32, "RS")
            for g_ in range(8):
                P.add("dve", (lambda e, g_=g_: e.tensor_tensor(WTf.ap[:, g_, :], WTf.ap[:, g_, :], MK.ap[:], ALU.mult)), [WTf, MK], [WTf])
            P.add("dve", lambda e: e.tensor_copy(WT.ap[:], WTf.ap[:]), [WTf], [WT])
            for g_ in range(0, 8, 4):
                pr_ = C.next_ps()
                P.add("pe", (lambda e, o=pr_.ap[:], r=WTf.ap[:, g_:g_ + 4, :]: e.matmul(o, C.ones.ap[:], r, start=True, stop=True)), [C.ones, WTf], [pr_])
                P.add("dve", (lambda e, o=RS.ap[:, g_:g_ + 4, :], i=pr_.ap[:].rearrange("p (g t) -> p g t", g=4): e.tensor_copy(o, i)), [pr_], [RS])
            lnc = [P.sb([128, 4], F32, "lnc%d" % i) for i in range(3)]
            lns = [P.sb([128, 1], F32, "lns%d" % i) for i in range(3)]
            t1s = [P.sb([128, 128], F32, "t1s%d" % i) for i in range(2)]
            t2s = [P.sb([128, 128], F32, "t2s%d" % i) for i in range(2)]
        if moe:
            ACC = None
            wr_sb = P.sb([128, KC, 8], F32, "wr_sb")
            P.dma("sp", wr_sb, wr_sb.ap[:], None, wr)
            ident = P.sb([128, 128], F32, "ident")
            P.dma("sp", ident, ident.ap[:], None, idn)
            dg = [P.sb([128, 128], F32, "dg%d" % i) for i in range(2)]
        if not glu:
            g1b = P.sb([128, KC], F32, "g1b")
            P.add("dve", lambda e: e.tensor_tensor(g1b.ap[:], g1.ap[:], c_b.ap[:], ALU.mult), [g1, c_b], [g1b])
        OUT = Tl(None, "out")
        if DBG_TAIL:
            dbgT = nc.dram_tensor("dbgT", [D, TC], F32, kind="ExternalOutput").ap()
            DBG = Tl(None, "dbg")
        for it in range(TC // NT):
            for k in range(KC):
                P.dma("sp", X[k], X[k].ap[:], None, xT[k * 128:(k + 1) * 128, it * NT:(it + 1) * NT])
                if not gmlp:
                    P.dma("pool", HB[k], HB[k].ap[:], None, yT[k * 128:(k + 1) * 128, it * NT:(it + 1) * NT])
                else:
                    P.add("act", (lambda e, o=HB[k].ap[:], i=X[k].ap[:], k=k:
                                  e.activation(o, i, AF.Identity, bias=sh1.ap[:, k:k + 1], scale=sc1p.ap[:, k:k + 1])), [X[k], sh1, sc1p], [HB[k]])
            if gmlp:
                def epi_u(mi, gi, ps):
                    xb = C.next_tmp()
                    P.add("act", (lambda e, o=xb.ap[:], i=ps.ap[:]: e.activation(o, i, AF.Identity, bias=c_dbi.ap[:, mi:mi + 1])), [ps, c_dbi], [xb])
                    gelu_ops(C, xb, xb.ap[:], U[mi], U[mi].ap[:], NT)
                linear(C, w_gin, D, 0, D, 512, [(HB, lambda k: HB[k].ap[:])], epi_u)
                wvv = w_gin.rearrange("(k p) n -> p k n", p=128)
                for s_ in range(4):
                    slab = C.next_slab()
                    sv = slab.ap[:, 0:KC * 512].rearrange("p (k n) -> p k n", k=KC)
                    P.dma("pool", slab, sv, None, wvv[:, :, D + s_ * 512:D + (s_ + 1) * 512])
                    for j in range(4):
                        ps = C.next_ps()
                        for k in range(KC):
                            P.add("pe", (lambda e, o=ps.ap[:], l=HB[k].ap[:, j * 128:(j + 1) * 128], r=sv[:, k, :], st=(k == 0), sp=(k == KC - 1):
                                         e.matmul(o, l, r, start=st, stop=sp)), [slab, HB[k]], [ps])
                        xb = C.next_tmp()
                        P.add("dve", (lambda e, o=xb.ap[:], a=ps.ap[:], b_=BR.ap[:, s_ * 512:(s_ + 1) * 512]: e.tensor_tensor(o, a, b_, ALU.add)), [ps, BR], [xb])
                        gelu_ops(C, xb, xb.ap[:], V[j], V[j].ap[:, s_ * 512:(s_ + 1) * 512], NT)
                for j in range(4):
                    c4, s1, s2 = lnc[j % 3], lns[j % 3], lns[(j + 1) % 3]
                    P.add("dve", (lambda e, o=s1.ap[:], i=V[j].ap[:]: e.tensor_reduce(o, i, mybir.AxisListType.X, ALU.add)), [V[j]], [s1])
                    P.add("dve", (lambda e, o=s1.ap[:]: e.tensor_scalar(o, o, -1.0 / D, None, ALU.mult)), [s1], [s1])
                    P.add("dve", (lambda e, o=V[j].ap[:], sc_=s1.ap[:, 0:1]: e.tensor_scalar(o, o, sc_, None, ALU.add)), [V[j], s1], [V[j]])
                    for s_ in range(4):
                        sq = C.next_tmp()
                        P.add("act", (lambda e, o=sq.ap[:], i=V[j].ap[:, s_ * 512:(s_ + 1) * 512]: e.activation(o, i, AF.Square)), [V[j]], [sq])
                        P.add("dve", (lambda e, o=c4.ap[:, s_:s_ + 1], i=sq.ap[:]: e.tensor_reduce(o, i, mybir.AxisListType.X, ALU.add)), [sq], [c4])
                    P.add("dve", (lambda e, o=s2.ap[:], i=c4.ap[:]: e.tensor_reduce(o, i, mybir.AxisListType.X, ALU.add)), [c4], [s2])
                    P.add("dve", (lambda e, o=s2.ap[:]: e.tensor_scalar(o, o, 1.0 / D, LN_EPS, ALU.mult, ALU.add)), [s2], [s2])
                    P.add("act", (lambda e, o=s2.ap[:]: e.activation(o, o, AF.Sqrt)), [s2], [s2])
                    P.add("dve", (lambda e, o=s2.ap[:]: e.reciprocal(o, o)), [s2], [s2])
                    P.add("dve", (lambda e, o=VB[j].ap[:], i=V[j].ap[:], sc_=s2.ap[:, 0:1]: e.tensor_scalar(o, i, sc_, None, ALU.mult)), [V[j], s2], [VB[j]])
                for c in range(KC):
                    g_ = c // 2
                    t1 = t1s[c % 2]
                    P.add("dve", (lambda e, o=t1.ap[:], r=RS.ap[:, g_, :], b_=BSP.ap[:, g_, :], c=c:
                                  e.scalar_tensor_tensor(o, r, c_dlnb.ap[:, c:c + 1], b_, ALU.mult, ALU.add)), [RS, BSP, c_dlnb], [t1])
                    for j in range(4):
                        ps = C.next_ps()
                        P.add("pe", (lambda e, o=ps.ap[:, 0:128], l=VB[j].ap[:, c * 128:(c + 1) * 128], r=WT.ap[:, g_, :]:
                                     e.matmul(o, l, r, start=True, stop=True)), [VB[j], WT], [ps])
                        t2 = t2s[j % 2]
                        P.add("dve", (lambda e, o=t2.ap[:], p_=ps.ap[:, 0:128], t1_=t1.ap[:], c=c:
                                      e.scalar_tensor_tensor(o, p_, c_dlng.ap[:, c:c + 1], t1_, ALU.mult, ALU.add)), [ps, t1, c_dlng], [t2])
                        P.add("pool", (lambda e, o=HB[c].ap[:, j * 128:(j + 1) * 128], a=t2.ap[:], u_=U[c].ap[:, j * 128:(j + 1) * 128]:
                                       e.tensor_tensor(o, a, u_, ALU.mult)), [t2, U[c]], [HB[c]])
            if glu:
                wv = w.rearrange("(k p) n -> p k n", p=128)
                for s in range(4):
                    sa = C.next_slab()
                    sg = C.next_slab()
                    va = sa.ap[:, 0:KC * 512].rearrange("p (k n) -> p k n", k=KC)
                    vg = sg.ap[:, 0:KC * 512].rearrange("p (k n) -> p k n", k=KC)
                    P.dma("pool", sa, va, None, wv[:, :, s * 512:(s + 1) * 512])
                    P.dma("pool", sg, vg, None, wv[:, :, D + s * 512:D + (s + 1) * 512])
                    for j in range(4):
                        m = s * 4 + j
                        pa = C.next_ps()
                        pg = C.next_ps()
                        for k in range(KC):
                            P.add("pe", (lambda e, o=pa.ap[:], l=va[:, k, j * 128:(j + 1) * 128], r=HB[k].ap[:], st=(k == 0), sp=(k == KC - 1):
                                         e.matmul(o, l, r, start=st, stop=sp)), [sa, HB[k]], [pa])
                        for k in range(KC):
                            P.add("pe", (lambda e, o=pg.ap[:], l=vg[:, k, j * 128:(j + 1) * 128], r=HB[k].ap[:], st=(k == 0), sp=(k == KC - 1):
                                         e.matmul(o, l, r, start=st, stop=sp)), [sg, HB[k]], [pg])
                        t = C.next_tmp()
                        P.add("act", (lambda e, o=t.ap[:], i=pg.ap[:], m=m:
                                      e.activation(o, i, AF.Sigmoid, bias=c_b.ap[:, KC + m:KC + m + 1])), [pg, c_b], [t])
                        P.add("dve", (lambda e, o=t.ap[:], a=pa.ap[:], m=m:
                                      e.scalar_tensor_tensor(o, a, c_b.ap[:, m:m + 1], o, ALU.add, ALU.mult)), [pa, t, c_b], [t])
                        P.add("act", (lambda e, o=t.ap[:], m=m: e.activation(o, o, AF.Copy, scale=g1.ap[:, m:m + 1])), [t, g1], [t])
                        P.add("dve", (lambda e, o=X[m].ap[:], b_=t.ap[:]: e.scalar_tensor_tensor(o, o, ALPHA, b_, ALU.mult, ALU.add)), [X[m], t], [X[m]])
            else:
                def epi_out(mi, gi, ps):
                    t = C.next_tmp()
                    P.add("act", (lambda e, o=t.ap[:], i=ps.ap[:]: e.activation(o, i, AF.Identity, bias=g1b.ap[:, mi:mi + 1], scale=g1.ap[:, mi:mi + 1])), [ps, g1b, g1], [t])
                    P.add("dve", (lambda e, o=X[mi].ap[:], b_=t.ap[:]: e.scalar_tensor_tensor(o, o, ALPHA, b_, ALU.mult, ALU.add)), [X[mi], t], [X[mi]])
                linear(C, w, D, 0, D, 512, [(HB, lambda k: HB[k].ap[:])], epi_out)
            ln_inplace(C, X, NT, c_ln1g, c_ln1b, HB, sc2p, sh2)
            if DBG_TAIL:
                for k in range(KC):
                    P.dma("sp", DBG, dbgT[k * 128:(k + 1) * 128, it * NT:(it + 1) * NT], X[k], X[k].ap[:])
            if not moe:
                ffn_phase(C, HB, X, w1, w3, w2, G, g2, NT)
            else:
                def hf_out(k, t):
                    P.dma("sp", OUT, hfT[k * 128:(k + 1) * 128, it * NT:(it + 1) * NT], t, t.ap[:])
                Gm = moe_router(C, X, sc2p, sh2, wr_sb, NT, hf_out)
                P.dma("sp", OUT, gm_d[it * NT:(it + 1) * NT, :].rearrange("(j p) e -> p j e", p=128), Gm, Gm.ap[:])
                for k in range(KC):
                    P.dma("sp", OUT, x1T[k * 128:(k + 1) * 128, it * NT:(it + 1) * NT], X[k], X[k].ap[:])
                continue
            ln_inplace(C, X, NT, c_ln2g, c_ln2b)
            for k in range(KC):
                P.dma("sp", OUT, outT[k * 128:(k + 1) * 128, it * NT:(it + 1) * NT], X[k], X[k].ap[:])
        P.finish([OUT] + ([DBG] if DBG_TAIL else []))
        P.emit_all()
    return nc


def common_maps(inp, layer, core):
    b = core // 2
    return {"modc": cols(inp["_mod"][layer][b])}


def tok_T(x, core):
    b, h = core // 2, core % 2
    return np.ascontiguousarray(x[b, h * TC:(h + 1) * TC, :].T)


def from_T(res, key, width):
    out = np.empty((4, SEQ, width), np.float32)
    for core in range(NCORE):
        b, h = core // 2, core % 2
        out[b, h * TC:(h + 1) * TC, :] = res[core][key].T[:, :width]
    return out


def run_head(inp, x, layer, w, bvec):
    ncols = w.shape[1]
    nm = (ncols + 127) // 128
    wp = np.zeros((D, nm * 128), np.float32)
    wp[:, :ncols] = w
    bp = np.zeros((nm * 128,), np.float32)
    bp[:ncols] = bvec
    nc = build_head(ncols, layer)
    maps = []
    for core in range(NCORE):
        m = common_maps(inp, layer, core)
        m.update({"xT": tok_T(x, core), "w": shard_rows(wp, core), "b": cols(bp)})
        maps.append({k: np.ascontiguousarray(v, dtype=np.float32) for k, v in m.items()})
    res = _run(nc, maps)
    return from_T(res, "outT", ncols)


def run_tail(inp, x, y, layer, w, bvec, glu, moe, gmlp=False):
    nc = build_tail(glu, moe, gmlp)
    maps = []
    fi = layer // 2
    for core in range(NCORE):
        m = common_maps(inp, layer, core)
        if gmlp:
            m.update({"w_gin": shard_rows(inp["d_w_in"][0], core), "d_bi": cols(inp["d_b_in"][0][:D]),
                      "d_bv": np.tile(inp["d_b_in"][0][D:][None, :], (128, 1)),
                      "d_lng": cols(inp["d_ln_g"][0]), "d_lnb": cols(inp["d_ln_b"][0]),
                      "d_wspT": inp["d_w_sp"][0].transpose(2, 0, 1),
                      "d_mask": np.triu(np.ones((128, 128), np.float32)),
                      "d_bsp": np.tile(inp["d_b_sp"][0][None, :, :], (128, 1, 1))})
        else:
            m["yT"] = tok_T(y, core)
        m.update({"xT": tok_T(x, core), "w": shard_rows(w, core), "b": cols(bvec),
                  "ln1_g": cols(inp["ln1_g"][layer]), "ln1_b": cols(inp["ln1_b"][layer]),
                  "ln2_g": cols(inp["ln2_g"][layer]), "ln2_b": cols(inp["ln2_b"][layer])})
        if moe:
            m.update({"wr": inp["m_router"][fi].reshape(KC, 128, 8).transpose(1, 0, 2),
                      "idn": np.eye(128, dtype=np.float32)})
        else:
            m.update({"w1": shard_rows(inp["f_w1"][fi], core), "w3": shard_rows(inp["f_w3"][fi], core), "w2": shard_rows(inp["f_w2"][fi], core)})
        maps.append({k: np.ascontiguousarray(v, dtype=np.float32) for k, v in m.items()})
    res = _run(nc, maps)
    if moe:
        gm = np.empty((4, SEQ, 8), np.float32)
        for core in range(NCORE):
            b, h = core // 2, core % 2
            gm[b, h * TC:(h + 1) * TC] = res[core]["gm"]
        return from_T(res, "x1T", D), from_T(res, "hfT", D), gm
    return from_T(res, "outT", D)


S5_TB = 512
TWO_PI = 2.0 * math.pi

MAGIC = 12582912.0
INV2PI = 1.0 / (2.0 * math.pi)
PI_SAFE = 3.14159


def sincos(P, ang, sn, cs, w1, w2, aa, sa, ca):
    A = P.add
    for (dst, da, shift) in ((sn, sa, 0.0), (cs, ca, 0.25)):
        A("dve", lambda e, sh=shift: e.tensor_scalar(aa(w1), aa(ang), INV2PI, sh, ALU.mult, ALU.add), [ang], [w1])
        A("dve", lambda e: e.tensor_scalar(aa(w1), aa(w1), MAGIC, None, ALU.add), [w1], [w1])
        A("dve", lambda e: e.tensor_scalar(aa(w1), aa(w1), -MAGIC, None, ALU.add), [w1], [w1])
        A("dve", lambda e: e.scalar_tensor_tensor(aa(w2), aa(w1), -TWO_PI, aa(ang), ALU.mult, ALU.add), [w1, ang], [w2])
        A("dve", lambda e, sh=shift: e.tensor_scalar(aa(w2), aa(w2), sh * TWO_PI, PI_SAFE, ALU.add, ALU.min), [w2], [w2])
        A("dve", lambda e: e.tensor_scalar(aa(w2), aa(w2), -PI_SAFE, None, ALU.max), [w2], [w2])
        A("act", lambda e, d_=dst, da_=da: e.activation(da_(d_), aa(w2), AF.Sin), [w2], [dst])


def build_s5core():
    nc = bass.Bass("TRN2", target_bir_lowering=False)
    dr = lambda name, shape: nc.dram_tensor(name, list(shape), F32, kind="ExternalInput").ap()
    NP_ = 32
    NCH = 8
    uT = dr("uT", [NCH * 128, SEQ])
    are_c, aim_c, ldt_c = dr("are_c", [128, NP_]), dr("aim_c", [128, NP_]), dr("ldt_c", [128, NP_])
    bre_l, bim_l = dr("bre_l", [128, NP_, 128]), dr("bim_l", [128, NP_, 128])
    cre_l, cim_l = dr("cre_l", [128, NP_, 128]), dr("cim_l", [128, NP_, 128])
    d_c = dr("d_c", [128, NCH])
    iota_d = dr("iota", [128, S5_TB])
    yT = nc.dram_tensor("yT", [NCH * 128, SEQ], F32, kind="ExternalOutput").ap()
    TB = S5_TB
    NB = SEQ // TB
    with ExitStack() as es:
        P = Prog(nc, es)
        pss = [P.ps([128, 512], F32, "ps%d" % i) for i in range(8)]
        psi = [0]

        def nps():
            p = pss[psi[0] % 8]
            psi[0] += 1
            return p
        ld = lambda name, ap, shape: (lambda t: (P.dma("sp", t, t.ap[:], None, ap), t)[1])(P.sb(shape, F32, name))
        are = ld("are", are_c, [128, NP_])
        aim = ld("aim", aim_c, [128, NP_])
        ldt = ld("ldt", ldt_c, [128, NP_])
        dcol = ld("dcol", d_c, [128, NCH])
        iota = ld("iota_sb", iota_d, [128, TB])
        brel = P.sb([128, NP_, 128], BF16, "brel")
        biml = P.sb([128, NP_, 128], BF16, "biml")
        P.dma("pool", brel, brel.ap[:], None, bre_l)
        P.dma("pool", biml, biml.ap[:], None, bim_l)
        sm = lambda name: P.sb([128, NP_], F32, name)
        dt, rho, th, sn, cs, lr, li, den, zr, zi, t1, t2 = [sm(n) for n in ("dt", "rho", "th", "sn0", "cs0", "lr", "li", "den", "zr", "zi", "t1s", "t2s")]
        negpi = P.sb([128, 1], F32, "negpi")
        P.add("dve", lambda e: e.memset(negpi.ap[:], -math.pi), [], [negpi])
        A = lambda eng, f, r, w: P.add(eng, f, r, w)
        A("act", lambda e: e.activation(dt.ap[:], ldt.ap[:], AF.Exp), [ldt], [dt])
        A("dve", lambda e: e.tensor_tensor(t1.ap[:], are.ap[:], dt.ap[:], ALU.mult), [are, dt], [t1])
        A("act", lambda e: e.activation(rho.ap[:], t1.ap[:], AF.Exp), [t1], [rho])
        A("dve", lambda e: e.tensor_tensor(th.ap[:], aim.ap[:], dt.ap[:], ALU.mult), [aim, dt], [th])
        full = lambda t: t.ap[:]
        sincos(P, th, sn, cs, t1, t2, full, full, full)
        thb, snB, csB = sm("thb"), sm("snB"), sm("csB")
        A("dve", lambda e: e.tensor_scalar(thb.ap[:], th.ap[:], float(S5_TB), None, ALU.mult), [th], [thb])
        sincos(P, thb, snB, csB, t1, t2, full, full, full)
        A("dve", lambda e: e.tensor_tensor(lr.ap[:], rho.ap[:], cs.ap[:], ALU.mult), [rho, cs], [lr])
        A("dve", lambda e: e.tensor_tensor(li.ap[:], rho.ap[:], sn.ap[:], ALU.mult), [rho, sn], [li])
        A("dve", lambda e: e.tensor_tensor(den.ap[:], are.ap[:], are.ap[:], ALU.mult), [are], [den])
        A("dve", lambda e: e.tensor_tensor(t1.ap[:], aim.ap[:], aim.ap[:], ALU.mult), [aim], [t1])
        A("dve", lambda e: e.tensor_tensor(den.ap[:], den.ap[:], t1.ap[:], ALU.add), [den, t1], [den])
        A("dve", lambda e: e.reciprocal(den.ap[:], den.ap[:]), [den], [den])
        A("dve", lambda e: e.tensor_scalar(lr.ap[:], lr.ap[:], -1.0, None, ALU.add), [lr], [lr])
        A("dve", lambda e: e.tensor_tensor(t1.ap[:], lr.ap[:], are.ap[:], ALU.mult), [lr, are], [t1])
        A("dve", lambda e: e.tensor_tensor(t2.ap[:], li.ap[:], aim.ap[:], ALU.mult), [li, aim], [t2])
        A("dve", lambda e: e.tensor_tensor(zr.ap[:], t1.ap[:], t2.ap[:], ALU.add), [t1, t2], [zr])
        A("dve", lambda e: e.tensor_tensor(zr.ap[:], zr.ap[:], den.ap[:], ALU.mult), [zr, den], [zr])
        A("dve", lambda e: e.tensor_tensor(t1.ap[:], li.ap[:], are.ap[:], ALU.mult), [li, are], [t1])
        A("dve", lambda e: e.tensor_tensor(t2.ap[:], lr.ap[:], aim.ap[:], ALU.mult), [lr, aim], [t2])
        A("dve", lambda e: e.tensor_tensor(zi.ap[:], t1.ap[:], t2.ap[:], ALU.subtract), [t1, t2], [zi])
        A("dve", lambda e: e.tensor_tensor(zi.ap[:], zi.ap[:], den.ap[:], ALU.mult), [zi, den], [zi])
        nzi = sm("nzi")
        A("dve", lambda e: e.tensor_scalar(nzi.ap[:], zi.ap[:], -1.0, None, ALU.mult), [zi], [nzi])
        cpr = P.sb([128, NP_, 128], BF16, "cpr")
        ncpi = P.sb([128, NP_, 128], BF16, "ncpi")
        ctmp = [P.sb([128, 128], F32, "ctmp%d" % i) for i in range(4)]
        for j in range(NP_):
            cr_t, ci_t, w1_t, w2_t = ctmp
            P.dma("sp", cr_t, cr_t.ap[:], None, cre_l[:, j, :])
            P.dma("sp", ci_t, ci_t.ap[:], None, cim_l[:, j, :])
            A("dve", lambda e, j=j: e.tensor_scalar(w1_t.ap[:], ci_t.ap[:], nzi.ap[:, j:j + 1], None, ALU.mult), [ci_t, nzi], [w1_t])
            A("dve", lambda e, j=j: e.scalar_tensor_tensor(cpr.ap[:, j, :], cr_t.ap[:], zr.ap[:, j:j + 1], w1_t.ap[:], ALU.mult, ALU.add), [cr_t, zr, w1_t], [cpr])
            A("dve", lambda e, j=j: e.tensor_scalar(w2_t.ap[:], ci_t.ap[:], zr.ap[:, j:j + 1], None, ALU.mult), [ci_t, zr], [w2_t])
            A("dve", lambda e, j=j: e.scalar_tensor_tensor(w2_t.ap[:], cr_t.ap[:], zi.ap[:, j:j + 1], w2_t.ap[:], ALU.mult, ALU.add), [cr_t, zi, w2_t], [w2_t])
            A("dve", lambda e, j=j: e.tensor_scalar(ncpi.ap[:, j, :], w2_t.ap[:], -1.0, None, ALU.mult), [w2_t], [ncpi])
        W = lambda name: P.sb([128, TB], F32, name)
        ub = P.sb([128, SEQ], BF16, "ub")
        uf = P.sb([128, SEQ], F32, "uf")
        Sr = [P.sb([128, SEQ], BF16, "Sr%d" % i) for i in range(4)]
        Si = [P.sb([128, SEQ], BF16, "Si%d" % i) for i in range(4)]
        rho_t = W("rho_t")
        ang, r1, r2, sn_t, cs_t = W("ang"), W("r1"), W("r2"), W("sn_t"), W("cs_t")
        arS2, aiS2 = [W("arS0"), W("arS1")], [W("aiS0"), W("aiS1")]
        q1_2, q2_2, q3_2, q4_2 = [[W("q%d_%d" % (a_, b_)) for b_ in range(2)] for a_ in range(1, 5)]
        xr2, xi2 = [W("xr0"), W("xr1")], [W("xi0"), W("xi1")]
        sr2 = [W("sra"), W("srb")]
        si2 = [W("sia"), W("sib")]
        car = [P.sb([128, 1], F32, "car%d" % i) for i in range(4)]
        zero_c = P.sb([128, 1], F32, "zero_c")
        A("dve", lambda e: e.memset(zero_c.ap[:], 0.0), [], [zero_c])
        yo = [W("yo0"), W("yo1")]
        g1t, g2t = W("g1t"), W("g2t")
        OUT = Tl(None, "out")
        def do_block(c, j, jj, k):
            arS, aiS, q1, q2, q3, q4, xr, xi = arS2[k % 2], aiS2[k % 2], q1_2[k % 2], q2_2[k % 2], q3_2[k % 2], q4_2[k % 2], xr2[k % 2], xi2[k % 2]
            pr, pi_ = nps(), nps()
            A("pe", lambda e, o=pr.ap[:], l=brel.ap[:, j, :], r=ub.ap[:, k * TB:(k + 1) * TB]: e.matmul(o, l, r, start=True, stop=True), [brel, ub], [pr])
            A("pe", lambda e, o=pi_.ap[:], l=biml.ap[:, j, :], r=ub.ap[:, k * TB:(k + 1) * TB]: e.matmul(o, l, r, start=True, stop=True), [biml, ub], [pi_])
            A("act", lambda e, o=arS.ap[:], i=pr.ap[:]: e.activation(o, i, AF.Copy), [pr], [arS])
            A("act", lambda e, o=aiS.ap[:], i=pi_.ap[:]: e.activation(o, i, AF.Copy), [pi_], [aiS])
            A("dve", lambda e: e.tensor_tensor(q1.ap[:], arS.ap[:], cs_t.ap[:], ALU.mult), [arS, cs_t], [q1])
            A("dve", lambda e: e.tensor_tensor(q2.ap[:], aiS.ap[:], sn_t.ap[:], ALU.mult), [aiS, sn_t], [q2])
            A("pool", lambda e: e.tensor_tensor(q3.ap[:], aiS.ap[:], cs_t.ap[:], ALU.mult), [aiS, cs_t], [q3])
            A("pool", lambda e: e.tensor_tensor(q4.ap[:], arS.ap[:], sn_t.ap[:], ALU.mult), [arS, sn_t], [q4])
            A("dve", lambda e: e.tensor_tensor(xr.ap[:], q1.ap[:], q2.ap[:], ALU.add), [q1, q2], [xr])
            A("pool", lambda e: e.tensor_tensor(xi.ap[:], q3.ap[:], q4.ap[:], ALU.subtract), [q3, q4], [xi])
            srt, sit = sr2[k % 2], si2[k % 2]
            if k == 0:
                ir, ii_, rd = zero_c.ap[:, 0:1], zero_c.ap[:, 0:1], [zero_c]
            else:
                pr_ = sr2[(k - 1) % 2]
                pi2_ = si2[(k - 1) % 2]
                A("dve", lambda e, j=j, p_=pi2_: e.tensor_scalar(car[2].ap[:], p_.ap[:, TB - 1:TB], snB.ap[:, j:j + 1], None, ALU.mult), [pi2_, snB], [car[2]])
                A("dve", lambda e, j=j, p_=pr_: e.scalar_tensor_tensor(car[0].ap[:], p_.ap[:, TB - 1:TB], csB.ap[:, j:j + 1], car[2].ap[:], ALU.mult, ALU.subtract), [pr_, csB, car[2]], [car[0]])
                A("dve", lambda e, j=j, p_=pi2_: e.tensor_scalar(car[3].ap[:], p_.ap[:, TB - 1:TB], csB.ap[:, j:j + 1], None, ALU.mult), [pi2_, csB], [car[3]])
                A("dve", lambda e, j=j, p_=pr_: e.scalar_tensor_tensor(car[1].ap[:], p_.ap[:, TB - 1:TB], snB.ap[:, j:j + 1], car[3].ap[:], ALU.mult, ALU.add), [pr_, snB, car[3]], [car[1]])
                ir, ii_, rd = car[0].ap[:, 0:1], car[1].ap[:, 0:1], [car[0], car[1]]
            A("dve", lambda e, o=srt.ap[:], i0=ir: e.tensor_tensor_scan(o, rho_t.ap[:], xr.ap[:], i0, ALU.mult, ALU.add), [rho_t, xr] + rd, [srt])
            A("dve", lambda e, o=sit.ap[:], i0=ii_: e.tensor_tensor_scan(o, rho_t.ap[:], xi.ap[:], i0, ALU.mult, ALU.add), [rho_t, xi] + rd, [sit])
            A("dve", lambda e, s_=srt: e.tensor_tensor(q1.ap[:], s_.ap[:], cs_t.ap[:], ALU.mult), [srt, cs_t], [q1])
            A("dve", lambda e, s_=sit: e.tensor_tensor(q2.ap[:], s_.ap[:], sn_t.ap[:], ALU.mult), [sit, sn_t], [q2])
            A("pool", lambda e, s_=srt: e.tensor_tensor(q3.ap[:], s_.ap[:], sn_t.ap[:], ALU.mult), [srt, sn_t], [q3])
            A("pool", lambda e, s_=sit: e.tensor_tensor(q4.ap[:], s_.ap[:], cs_t.ap[:], ALU.mult), [sit, cs_t], [q4])
            A("dve", lambda e, o=Sr[jj].ap[:, k * TB:(k + 1) * TB]: e.tensor_tensor(o, q1.ap[:], q2.ap[:], ALU.subtract), [q1, q2], [Sr[jj]])
            A("pool", lambda e, o=Si[jj].ap[:, k * TB:(k + 1) * TB]: e.tensor_tensor(o, q3.ap[:], q4.ap[:], ALU.add), [q3, q4], [Si[jj]])

        for c in range(NCH):
            P.dma("sp", uf, uf.ap[:], None, uT[c * 128:(c + 1) * 128, :])
            P.dma("pool", ub, ub.ap[:], None, uT[c * 128:(c + 1) * 128, :])
            for jj in range(4):
                j = c * 4 + jj
                A("dve", lambda e, j=j: e.tensor_scalar(rho_t.ap[:], iota.ap[:], 0.0, rho.ap[:, j:j + 1], ALU.mult, ALU.add), [iota, rho], [rho_t])
                A("dve", lambda e, j=j: e.tensor_scalar(ang.ap[:], iota.ap[:], th.ap[:, j:j + 1], None, ALU.mult), [iota, th], [ang])
                sincos(P, ang, sn_t, cs_t, r1, r2, full, full, full)
                for k in range(NB):
                    do_block(c, j, jj, k)
            for k in range(NB):
                ps = nps()
                for jj in range(4):
                    j = c * 4 + jj
                    A("pe", lambda e, o=ps.ap[:], l=cpr.ap[:, j, :], r=Sr[jj].ap[:, k * TB:(k + 1) * TB], st=(jj == 0): e.matmul(o, l, r, start=st, stop=False), [cpr, Sr[jj]], [ps])
                    A("pe", lambda e, o=ps.ap[:], l=ncpi.ap[:, j, :], r=Si[jj].ap[:, k * TB:(k + 1) * TB], sp=(jj == 3): e.matmul(o, l, r, start=False, stop=sp), [ncpi, Si[jj]], [ps])
                y = yo[k % 2]
                A("dve", lambda e, o=y.ap[:], u_=uf.ap[:, k * TB:(k + 1) * TB], p_=ps.ap[:], c=c: e.scalar_tensor_tensor(o, u_, dcol.ap[:, c:c + 1], p_, ALU.mult, ALU.add), [uf, dcol, ps], [y])
                A("act", lambda e, o=g1t.ap[:], i=y.ap[:]: e.activation(o, i, AF.Square), [y], [g1t])
                A("dve", lambda e: e.tensor_scalar(g1t.ap[:], g1t.ap[:], 0.044715, 1.0, ALU.mult, ALU.add), [g1t], [g1t])
                A("pool", lambda e, i=y.ap[:]: e.tensor_tensor(g2t.ap[:], g1t.ap[:], i, ALU.mult), [g1t, y], [g2t])
                A("act", lambda e: e.activation(g2t.ap[:], g2t.ap[:], AF.Sigmoid, scale=2.0 * math.sqrt(2.0 / math.pi)), [g2t], [g2t])
                A("pool", lambda e, o=y.ap[:]: e.tensor_tensor(o, o, g2t.ap[:], ALU.mult), [y, g2t], [y])
                P.dma("sp", OUT, yT[c * 128:(c + 1) * 128, k * TB:(k + 1) * TB], y, y.ap[:])
        P.finish([OUT])
        P.emit_all()
    return nc


def run_s5core(inp, u):
    nc = build_s5core()
    are, aim, ldt = inp["b_a_re"][0], inp["b_a_im"][0], inp["b_log_dt"][0]
    bre, bim, cre, cim = inp["b_b_re"][0], inp["b_b_im"][0], inp["b_c_re"][0], inp["b_c_im"][0]
    dvec = inp["b_d"][0]
    maps = []
    for core in range(NCORE):
        b, gh = core // 2, core % 2
        g0 = gh * 64
        pc = lambda a: np.ascontiguousarray(a[g0:g0 + 64].reshape(32, 128).T)
        ldt_rep = np.repeat(ldt[:, None], 64, axis=1)
        bre_l = np.zeros((128, 32, 128), np.float32)
        bim_l = np.zeros((128, 32, 128), np.float32)
        cre_l = np.zeros((128, 32, 128), np.float32)
        cim_l = np.zeros((128, 32, 128), np.float32)
        for j in range(32):
            for gi in range(2):
                g = g0 + 2 * j + gi
                r0 = 16 * ((2 * j + gi) % 8)
                bre_l[r0:r0 + 16, j, gi * 64:(gi + 1) * 64] = bre[g].T
                bim_l[r0:r0 + 16, j, gi * 64:(gi + 1) * 64] = bim[g].T
                cre_l[gi * 64:(gi + 1) * 64, j, r0:r0 + 16] = cre[g].T
                cim_l[gi * 64:(gi + 1) * 64, j, r0:r0 + 16] = cim[g].T
        m = {"uT": np.ascontiguousarray(u[b, :, gh * 1024:(gh + 1) * 1024].T),
             "are_c": pc(are), "aim_c": pc(aim), "ldt_c": pc(ldt_rep),
             "bre_l": bre_l, "bim_l": bim_l, "cre_l": cre_l, "cim_l": cim_l,
             "d_c": np.ascontiguousarray(dvec[gh * 1024:(gh + 1) * 1024].reshape(8, 128).T),
             "iota": np.tile(np.arange(S5_TB, dtype=np.float32)[None, :], (128, 1))}
        maps.append({k: np.ascontiguousarray(v, dtype=np.float32) for k, v in m.items()})
    res = _run(nc, maps)
    y = np.empty((4, SEQ, D), np.float32)
    for core in range(NCORE):
        b, gh = core // 2, core % 2
        y[b, :, gh * 1024:(gh + 1) * 1024] = res[core]["yT"].T
    return y


ML_T = 128
DKS = 128.0 ** -0.5


def build_mlstm():
    nc = bass.Bass("TRN2", target_bir_lowering=False)
    dr = lambda name, shape: nc.dram_tensor(name, list(shape), F32, kind="ExternalInput").ap()
    qkT = dr("qkT", [8, 128, SEQ])
    wcv = dr("wcv", [128, 8, 4])
    bcv = dr("bcv", [128, 8])
    v_d = dr("v_tok", [SEQ, 4, 256])
    o_d = dr("o_tok", [SEQ, 1024])
    gi_d = dr("gi_tok", [SEQ, 4])
    gf_d = dr("gf_tok", [SEQ, 4])
    mhg_d = dr("mhg", [128, 1024])
    tri_d, blk_d, mkc_d, cm0_d, cm1_d, idn_d = [dr(n, [128, 128]) for n in ("tri", "blk", "mkc", "cm0", "cm1", "idn")]
    hs_d = nc.dram_tensor("hs", [SEQ, 1024], F32, kind="ExternalOutput").ap()
    NTL = SEQ // ML_T
    with ExitStack() as es:
        P = Prog(nc, es)
        A = P.add
        ldc = lambda name, ap, shape, dt=F32, q="sp": (lambda t: (P.dma(q, t, t.ap[:], None, ap), t)[1])(P.sb(shape, dt, name))
        TRI = ldc("TRI", tri_d, [128, 128])
        BLK = ldc("BLK", blk_d, [128, 128])
        MKC = ldc("MKC", mkc_d, [128, 128])
        CM0 = ldc("CM0", cm0_d, [128, 128])
        CM1 = ldc("CM1", cm1_d, [128, 128])
        IDB = ldc("IDB", idn_d, [128, 128], BF16, "pool")
        MHG = ldc("MHG", mhg_d, [128, 1024])
        WCV = ldc("WCV", wcv, [128, 8, 4])
        BCV = ldc("BCV", bcv, [128, 8])
        ONES = P.sb([128, 128], F32, "ONES")
        A("dve", lambda e: e.memset(ONES.ap[:], 1.0), [], [ONES])
        lnk = P.sb([128, 1], F32, "lnk")
        A("dve", lambda e: e.memset(lnk.ap[:], math.log(DKS)), [], [lnk])
        CF = [P.sb([128, 257], F32, "CF%d" % h) for h in range(4)]
        CA = [P.sb([128, 257], BF16, "CA%d" % h) for h in range(4)]
        CAm = [P.sb([128, 257], BF16, "CAm%d" % h) for h in range(4)]
        for h in range(4):
            A("dve", lambda e, h=h: e.memset(CF[h].ap[:], 0.0), [], [CF[h]])
            A("dve", lambda e, h=h: e.memset(CA[h].ap[:], 0.0), [], [CA[h]])
        QKR = [P.sb([128, 8, 3 + ML_T], F32, "QKR%d" % i) for i in range(2)]
        A("dve", lambda e: e.memset(QKR[1].ap[:], 0.0), [], [QKR[1]])
        QKC = [P.sb([128, 8, ML_T], BF16, "QKC%d" % i) for i in range(2)]
        cacc = [P.sb([128, ML_T], F32, "cacc%d" % i) for i in range(2)]
        VA = [P.sb([128, 4, 257], BF16, "VA%d" % i) for i in range(2)]
        for i in range(2):
            A("dve", lambda e, i=i: e.memset(VA[i].ap[:, :, 256:257], 1.0), [], [VA[i]])
        OT = [P.sb([128, 1024], F32, "OT%d" % i) for i in range(2)]
        GI = [P.sb([128, 4], F32, "GI%d" % i) for i in range(2)]
        GF = [P.sb([128, 4], F32, "GF%d" % i) for i in range(2)]
        LF = P.sb([128, 4], F32, "LF")
        BC, AC, BL, WK = [P.sb([128, 4], F32, n) for n in ("BCc", "ACc", "BLc", "WKc")]
        LFB = [P.sb([128, 128], F32, "LFB%d" % i) for i in range(2)]
        BBS = P.sb([128, 4, 128], F32, "BBS")
        EBC = P.sb([128, 4, 128], F32, "EBC")
        E0 = [P.sb([128, 128], F32, "E0_%d" % i) for i in range(2)]
        E1 = [P.sb([128, 128], F32, "E1_%d" % i) for i in range(2)]
        Q0 = [P.sb([128, 128], BF16, "Q0_%d" % i) for i in range(2)]
        Q1 = [P.sb([128, 128], BF16, "Q1_%d" % i) for i in range(2)]
        WTT = [P.sb([128, 128], F32, "WTT%d" % i) for i in range(2)]
        SB = [P.sb([128, 128], BF16, "SB%d" % i) for i in range(2)]
        KW = [P.sb([128, 128], BF16, "KW%d" % i) for i in range(2)]
        DN = [P.sb([128, 1], F32, "DN%d" % i) for i in range(2)]
        S1 = [P.sb([128, 1], F32, "S1_%d" % i) for i in range(2)]
        S2 = [P.sb([128, 1], F32, "S2_%d" % i) for i in range(2)]
        OG = [P.sb([128, 256], F32, "OG%d" % i) for i in range(2)]
        HG = [P.sb([128, 256], F32, "HG%d" % i) for i in range(2)]
        SQ = [P.sb([128, 256], F32, "SQ%d" % i) for i in range(2)]
        OUTT = [P.sb([128, 1024], F32, "OUTT%d" % i) for i in range(2)]
        p_bb = P.ps([128, 512], F32, "p_bb")
        p_st = P.ps([128, 512], F32, "p_st")
        p_kt = P.ps([128, 512], BF16, "p_kt")
        p_sm = P.ps([128, 512], F32, "p_sm")
        p_c = [P.ps([128, 512], F32, "p_c%d" % i) for i in range(2)]
        p_o = [P.ps([128, 512], F32, "p_o%d" % i) for i in range(2)]
        OUT = Tl(None, "out")
        pcs = [0]

        def do_tile(i):
            t0 = i * ML_T
            qr, qp = QKR[i % 2], QKR[(i + 1) % 2]
            qc = QKC[i % 2]
            va, ot, gi, gf = VA[i % 2], OT[i % 2], GI[i % 2], GF[i % 2]
            P.dma("sp", qr, qr.ap[:, :, 3:3 + ML_T], None, qkT[:, :, t0:t0 + ML_T].rearrange("g p t -> p g t"))
            P.dma("pool", va, va.ap[:, :, 0:256], None, v_d[t0:t0 + ML_T, :, :])
            P.dma("sp", ot, ot.ap[:], None, o_d[t0:t0 + ML_T, :])
            P.dma("sp", gi, gi.ap[:], None, gi_d[t0:t0 + ML_T, :])
            P.dma("sp", gf, gf.ap[:], None, gf_d[t0:t0 + ML_T, :])
            A("pool", lambda e, o=qr.ap[:, :, 0:3], s_=qp.ap[:, :, ML_T:ML_T + 3]: e.tensor_copy(o, s_), [qp], [qr])
            for g in range(8):
                ca = cacc[g % 2]
                A("dve", lambda e, g=g, o=ca.ap[:]: e.tensor_scalar(o, qr.ap[:, g, 0:ML_T], WCV.ap[:, g, 0:1], BCV.ap[:, g:g + 1], ALU.mult, ALU.add), [qr, WCV, BCV], [ca])
                for j in range(1, 4):
                    A("dve", lambda e, g=g, j=j, o=ca.ap[:]: e.scalar_tensor_tensor(o, qr.ap[:, g, j:j + ML_T], WCV.ap[:, g, j:j + 1], o, ALU.mult, ALU.add), [qr, WCV, ca], [ca])
                A("act", lambda e, g=g, i_=ca.ap[:]: e.activation(qc.ap[:, g, :], i_, AF.Silu), [ca], [qc])
            A("act", lambda e: e.activation(LF.ap[:], gf.ap[:], AF.Exp, scale=-1.0), [gf], [LF])
            A("act", lambda e: e.activation(LF.ap[:], LF.ap[:], AF.Ln, bias=1.0), [LF], [LF])
            A("dve", lambda e: e.tensor_scalar(LF.ap[:], LF.ap[:], -1.0, None, ALU.mult), [LF], [LF])
            A("pe", lambda e: e.matmul(p_sm.ap[:, 0:4], TRI.ap[:], LF.ap[:], start=True, stop=True), [TRI, LF], [p_sm])
            A("pe", lambda e: e.matmul(p_sm.ap[:, 4:8], BLK.ap[:], LF.ap[:], start=True, stop=True), [BLK, LF], [p_sm])
            A("dve", lambda e: e.tensor_copy(BC.ap[:], p_sm.ap[:, 0:4]), [p_sm], [BC])
            A("dve", lambda e: e.tensor_tensor(AC.ap[:], gi.ap[:], BC.ap[:], ALU.subtract), [gi, BC], [AC])
            A("dve", lambda e: e.tensor_tensor(BL.ap[:], p_sm.ap[:, 4:8], AC.ap[:], ALU.add), [p_sm, AC], [BL])
            A("act", lambda e: e.activation(WK.ap[:], BL.ap[:], AF.Exp, bias=lnk.ap[:, 0:1]), [BL, lnk], [WK])
            for h in range(4):
                lb = LFB[h % 2]
                A("dve", lambda e, h=h, o=lb.ap[:]: e.tensor_scalar(o, ONES.ap[:], LF.ap[:, h:h + 1], None, ALU.mult), [ONES, LF], [lb])
                A("pe", lambda e, h=h, l=lb.ap[:]: e.matmul(p_bb.ap[:, h * 128:(h + 1) * 128], l, TRI.ap[:], start=True, stop=True), [lb, TRI], [p_bb])
            A("act", lambda e: e.activation(BBS.ap[:], p_bb.ap[:].rearrange("p (h t) -> p h t", h=4), AF.Copy), [p_bb], [BBS])
            A("act", lambda e: e.activation(EBC.ap[:], BBS.ap[:], AF.Exp), [BBS], [EBC])
            for h in range(4):
                do_head(i, h, qc, va, ot)
            P.dma("sp", OUT, hs_d[t0:t0 + ML_T, :], OUTT[i % 2], OUTT[i % 2].ap[:])

        def do_head(i, h, qc, va, ot):
            if True:
                qh = qc.ap[:, h, :]
                kh = qc.ap[:, 4 + h, :]
                A("pe", lambda e, h=h, kh=kh, qh=qh: e.matmul(p_st.ap[:, h * 128:(h + 1) * 128], kh, qh, start=True, stop=True), [qc], [p_st])
                wt = WTT[h % 2]
                A("act", lambda e, h=h, o=wt.ap[:]: e.activation(o, BBS.ap[:, h, :], AF.Exp, bias=AC.ap[:, h:h + 1]), [BBS, AC], [wt])
                A("pool", lambda e, o=wt.ap[:]: e.tensor_tensor(o, o, MKC.ap[:], ALU.mult), [wt, MKC], [wt])
                sb = SB[h % 2]
                A("dve", lambda e, h=h, o=sb.ap[:], w_=wt.ap[:]: e.tensor_tensor(o, p_st.ap[:, h * 128:(h + 1) * 128], w_, ALU.mult), [p_st, wt], [sb])
                A("pe", lambda e, h=h, kh=kh: e.transpose(p_kt.ap[:, h * 128:(h + 1) * 128], kh, IDB.ap[:]), [qc, IDB], [p_kt])
                kw = KW[h % 2]
                A("act", lambda e, h=h, o=kw.ap[:]: e.activation(o, p_kt.ap[:, h * 128:(h + 1) * 128], AF.Copy, scale=WK.ap[:, h:h + 1]), [p_kt, WK], [kw])
                e0, e1, q0, q1 = E0[h % 2], E1[h % 2], Q0[h % 2], Q1[h % 2]
                A("pool", lambda e, h=h, o=e0.ap[:]: e.tensor_tensor(o, EBC.ap[:, h, :], CM0.ap[:], ALU.mult), [EBC, CM0], [e0])
                A("pool", lambda e, h=h, o=e1.ap[:]: e.tensor_tensor(o, EBC.ap[:, h, :], CM1.ap[:], ALU.mult), [EBC, CM1], [e1])
                A("dve", lambda e, o=q0.ap[:], qh=qh, e_=e0.ap[:]: e.tensor_tensor(o, qh, e_, ALU.mult), [qc, e0], [q0])
                A("dve", lambda e, o=q1.ap[:], qh=qh, e_=e1.ap[:]: e.tensor_tensor(o, qh, e_, ALU.mult), [qc, e1], [q1])
                po = p_o[h % 2]
                A("pe", lambda e, h=h, o=po.ap[:, 0:257], l=sb.ap[:]: e.matmul(o, l, va.ap[:, h, :], start=True, stop=False), [sb, va], [po])
                A("pe", lambda e, h=h, o=po.ap[:, 0:257], l=q0.ap[:]: e.matmul(o, l, CA[h].ap[:], start=False, stop=False), [q0, CA[h]], [po])
                for cc in range(2):
                    pc = p_c[pcs[0] % 2]
                    pcs[0] += 1
                    A("pe", lambda e, h=h, cc=cc, o=pc.ap[:, 0:257], l=kw.ap[64 * cc:64 * cc + 64, :]: e.matmul(o, l, va.ap[64 * cc:64 * cc + 64, h, :], start=True, stop=True), [kw, va], [pc])
                    A("dve", lambda e, h=h, cc=cc, p_=pc.ap[:, 0:257]: e.scalar_tensor_tensor(CF[h].ap[:], CF[h].ap[:], EBC.ap[:, h, 64 * cc + 63:64 * cc + 64], p_, ALU.mult, ALU.add), [CF[h], EBC, pc], [CF[h]])
                    if cc == 0:
                        A("act", lambda e, h=h: e.activation(CAm[h].ap[:], CF[h].ap[:], AF.Copy), [CF[h]], [CAm[h]])
                        A("pe", lambda e, h=h, o=po.ap[:, 0:257], l=q1.ap[:]: e.matmul(o, l, CAm[h].ap[:], start=False, stop=True), [q1, CAm[h]], [po])
                    else:
                        A("act", lambda e, h=h: e.activation(CA[h].ap[:], CF[h].ap[:], AF.Copy), [CF[h]], [CA[h]])
                dn, s1, s2, og, hg, sq = DN[h % 2], S1[h % 2], S2[h % 2], OG[h % 2], HG[h % 2], SQ[h % 2]
                A("act", lambda e, o=dn.ap[:], p_=po.ap[:, 256:257]: e.activation(o, p_, AF.Abs), [po], [dn])
                A("dve", lambda e, o=dn.ap[:]: e.tensor_scalar(o, o, 1.0, None, ALU.max), [dn], [dn])
                A("dve", lambda e, o=dn.ap[:]: e.reciprocal(o, o), [dn], [dn])
                A("act", lambda e, h=h, o=og.ap[:]: e.activation(o, ot.ap[:, h * 256:(h + 1) * 256], AF.Sigmoid), [ot], [og])
                A("dve", lambda e, o=hg.ap[:], p_=po.ap[:, 0:256], d_=dn.ap[:, 0:1], g_=og.ap[:]: e.scalar_tensor_tensor(o, p_, d_, g_, ALU.mult, ALU.mult), [po, dn, og], [hg])
                A("dve", lambda e, o=s1.ap[:], i_=hg.ap[:]: e.tensor_reduce(o, i_, mybir.AxisListType.X, ALU.add), [hg], [s1])
                A("dve", lambda e, o=s1.ap[:]: e.tensor_scalar(o, o, -1.0 / 256.0, None, ALU.mult), [s1], [s1])
                A("pool", lambda e, o=hg.ap[:], c_=s1.ap[:, 0:1]: e.tensor_scalar(o, o, c_, None, ALU.add), [hg, s1], [hg])
                A("act", lambda e, o=sq.ap[:], i_=hg.ap[:]: e.activation(o, i_, AF.Square), [hg], [sq])
                A("dve", lambda e, o=s2.ap[:], i_=sq.ap[:]: e.tensor_reduce(o, i_, mybir.AxisListType.X, ALU.add), [sq], [s2])
                A("dve", lambda e, o=s2.ap[:]: e.tensor_scalar(o, o, 1.0 / 256.0, LN_EPS, ALU.mult, ALU.add), [s2], [s2])
                A("act", lambda e, o=s2.ap[:]: e.activation(o, o, AF.Sqrt), [s2], [s2])
                A("dve", lambda e, o=s2.ap[:]: e.reciprocal(o, o), [s2], [s2])
                outt = OUTT[i % 2]
                A("dve", lambda e, h=h, o=outt.ap[:, h * 256:(h + 1) * 256], i_=hg.ap[:], c_=s2.ap[:, 0:1]: e.scalar_tensor_tensor(o, i_, c_, MHG.ap[:, h * 256:(h + 1) * 256], ALU.mult, ALU.mult), [hg, s2, MHG], [outt])
        for i in range(NTL):
            do_tile(i)
        P.finish([OUT])
        P.emit_all()
    return nc


def run_mlstm(inp, z):
    nc = build_mlstm()
    wc, bc, mhg = inp["c_w_conv"][0], inp["c_b_conv"][0], inp["c_mh_g"][0]
    ii = np.arange(128)
    same = (ii[:, None] // 64) == (ii[None, :] // 64)
    tri = (same & (ii[:, None] <= ii[None, :])).astype(np.float32)
    consts = {"tri": tri, "blk": same.astype(np.float32), "mkc": tri * np.float32(DKS),
              "cm0": np.tile((ii < 64).astype(np.float32)[None, :], (128, 1)),
              "cm1": np.tile((ii >= 64).astype(np.float32)[None, :], (128, 1)),
              "idn": np.eye(128, dtype=np.float32)}
    maps = []
    for core in range(NCORE):
        b, hh = core // 2, core % 2
        heads = [4 * hh + h for h in range(4)]
        chans = [np.arange(128 * h_, 128 * h_ + 128) for h_ in heads] + [np.arange(1024 + 128 * h_, 1024 + 128 * h_ + 128) for h_ in heads]
        qk = np.stack([z[b][:, ch].T for ch in chans], 0)
        wcv = np.stack([wc[:, ch].T for ch in chans], 1)
        bcv = np.stack([bc[ch] for ch in chans], 1)
        v = z[b][:, 2048 + 1024 * hh:2048 + 1024 * (hh + 1)].reshape(SEQ, 4, 256)
        o = z[b][:, 4096 + 1024 * hh:4096 + 1024 * (hh + 1)]
        gi = z[b][:, 6144 + 4 * hh:6144 + 4 * hh + 4]
        gf = z[b][:, 6152 + 4 * hh:6152 + 4 * hh + 4]
        m = {"qkT": qk, "wcv": wcv, "bcv": bcv, "v_tok": v, "o_tok": o, "gi_tok": gi, "gf_tok": gf,
             "mhg": np.tile(mhg[1024 * hh:1024 * (hh + 1)][None, :], (128, 1))}
        m.update(consts)
        maps.append({k: np.ascontiguousarray(v_, dtype=np.float32) for k, v_ in m.items()})
    res = _run(nc, maps)
    hs = np.empty((4, SEQ, D), np.float32)
    for core in range(NCORE):
        b, hh = core // 2, core % 2
        hs[b, :, 1024 * hh:1024 * (hh + 1)] = res[core]["hs"]
    return hs


def _dbg(name, arr):
    import os
    d = os.environ.get("KDEBUG_DIR")
    if d:
        np.save(os.path.join(d, name + ".npy"), arr[0])


def kernel(**inputs):
    inp = {k: np.asarray(v) for k, v in inputs.items()}
    inp["_mod"] = run_mod(inp)
    x = np.asarray(inp["x"], np.float32)
    x = run_layer0(inp, x)
    _dbg("x0", x)
    u = run_head(inp, x, 1, inp["b_w_in"][0], inp["b_b_in"][0])
    y = run_s5core(inp, u)
    _dbg("y1", y)
    del u
    x1, hf, gm = run_tail(inp, x, y, 1, inp["b_w_glu"][0], inp["b_b_glu"][0], True, True)
    del y, x
    ya, yb = run_experts(inp, 0, hf, gm)
    x, z = run_combine(inp, 1, x1, ya, yb, head=(2, inp["c_w_in"][0], inp["c_b_in"][0]))
    _dbg("x1", x)
    del x1, hf, ya, yb
    hs = run_mlstm(inp, z)
    _dbg("hs2", hs)
    del z
    x = run_tail(inp, x, hs, 2, inp["c_w_out"][0], inp["c_b_out"][0], False, False)
    _dbg("x2", x)
    del hs
    x1, hf, gm = run_tail(inp, x, None, 3, inp["d_w_out"][0], inp["d_b_out"][0], False, True, gmlp=True)
    del x
    ya, yb = run_experts(inp, 1, hf, gm)
    x = run_combine(inp, 3, x1, ya, yb)
    return np.ascontiguousarray(x, dtype=np.float32)


MODW = 6 * D // NCORE


def build_mod():
    nc = bass.Bass("TRN2", target_bir_lowering=False)
    dr = lambda name, shape: nc.dram_tensor(name, list(shape), F32, kind="ExternalInput").ap()
    c_d = dr("c_all", [128, KC, 4])
    aw = dr("adw", [4 * D, MODW])
    ab = dr("adb", [4, 4, MODW])
    out = nc.dram_tensor("modr", [4, 4, MODW], F32, kind="ExternalOutput").ap()
    with ExitStack() as es:
        P = Prog(nc, es)
        C = Ctx(P)
        craw = P.sb([128, KC, 4], F32, "craw")
        cb = P.sb([128, KC, 4], BF16, "cbf")
        P.dma("sp", craw, craw.ap[:], None, c_d)
        P.add("act", lambda e: e.activation(cb.ap[:], craw.ap[:], AF.Silu), [craw], [cb])
        bt = [P.sb([4, MODW], F32, "bt%d" % l) for l in range(4)]
        ot = [P.sb([4, 512], F32, "ot%d" % i) for i in range(2)]
        OUT = Tl(None, "out")
        i = 0
        for l in range(4):
            P.dma("sp", bt[l], bt[l].ap[:], None, ab[l])
            wv = aw[l * D:(l + 1) * D, :].rearrange("(k p) n -> p k n", p=128)
            for s_ in range(MODW // 512):
                slab = C.next_slab()
                sv = slab.ap[:, 0:KC * 512].rearrange("p (k n) -> p k n", k=KC)
                P.dma("pool", slab, sv, None, wv[:, :, s_ * 512:(s_ + 1) * 512])
                ps = C.next_ps()
                for k in range(KC):
                    P.add("pe", (lambda e, o=ps.ap[0:4, :], l_=cb.ap[:, k, :], r=sv[:, k, :], st=(k == 0), sp=(k == KC - 1):
                                 e.matmul(o, l_, r, start=st, stop=sp)), [slab, cb], [ps])
                o_ = ot[i % 2]
                i += 1
                P.add("dve", (lambda e, o=o_.ap[:], a=ps.ap[0:4, :], b_=bt[l].ap[:, s_ * 512:(s_ + 1) * 512]: e.tensor_tensor(o, a, b_, ALU.add)), [ps, bt[l]], [o_])
                P.dma("sp", OUT, out[l, :, s_ * 512:(s_ + 1) * 512], o_, o_.ap[:])
        P.finish([OUT])
        P.emit_all()
    return nc


def run_mod(inp):
    nc = build_mod()
    c_all = np.stack([cols(inp["c"][b]) for b in range(4)], -1)
    maps = []
    for core in range(NCORE):
        cs = slice(core * MODW, (core + 1) * MODW)
        m = {"c_all": c_all, "adw": inp["ada_w"][:, :, cs].reshape(4 * D, MODW),
             "adb": np.tile(inp["ada_b"][:, None, cs], (1, 4, 1))}
        maps.append({k: np.ascontiguousarray(v, dtype=np.float32) for k, v in m.items()})
    res = _run(nc, maps)
    mod = np.empty((4, 4, 6 * D), np.float32)
    for core in range(NCORE):
        mod[:, :, core * MODW:(core + 1) * MODW] = res[core]["modr"]
    return mod


def build_expert(ntile):
    n = ntile * NT
    nc = bass.Bass("TRN2", target_bir_lowering=False)
    dr = lambda name, shape: nc.dram_tensor(name, list(shape), F32, kind="ExternalInput").ap()
    xT = dr("xT", [D, n])
    grow = dr("grow", [1, n])
    w1, w3, w2 = dr("w1", [D, DFF]), dr("w3", [D, DFF]), dr("w2", [DFF, D])
    yT = nc.dram_tensor("yT", [D, n], F32, kind="ExternalOutput").ap()
    with ExitStack() as es:
        P = Prog(nc, es)
        C = Ctx(P, nslab=2, nsmall=6)
        X = [P.sb([128, NT], F32, "X%d" % k) for k in range(KC)]
        HB = [P.sb([128, NT], BF16, "HB%d" % k) for k in range(KC)]
        G = [P.sb([128, NT], BF16, "G%d" % m) for m in range(MC_FF)]
        onec = P.sb([128, KC], F32, "onec")
        P.add("dve", lambda e: e.memset(onec.ap[:], 1.0), [], [onec])
        gr = [P.sb([1, NT], F32, "gr%d" % i) for i in range(2)]
        OUT = Tl(None, "out")
        for it in range(ntile):
            g_ = gr[it % 2]
            P.dma("sp", g_, g_.ap[:], None, grow[:, it * NT:(it + 1) * NT])
            for k in range(KC):
                P.dma("pool", HB[k], HB[k].ap[:], None, xT[k * 128:(k + 1) * 128, it * NT:(it + 1) * NT])
                P.add("pool", (lambda e, o=X[k].ap[:]: e.memset(o, 0.0)), [], [X[k]])
            gate = C.pstat[1]
            P.add("pe", (lambda e, o=gate.ap[:], r=g_.ap[0:1, :]: e.matmul(o, C.ones.ap[0:1, :], r, start=True, stop=True)), [C.ones, g_], [gate])
            ffn_phase(C, HB, X, w1, w3, w2, G, onec, NT, gate=gate, ACC=1)
            for k in range(KC):
                P.dma("sp", OUT, yT[k * 128:(k + 1) * 128, it * NT:(it + 1) * NT], X[k], X[k].ap[:])
        P.finish([OUT])
        P.emit_all()
    return nc


def run_experts(inp, fi, hf, gm):
    hff = hf.reshape(-1, D)
    gmf = gm.reshape(-1, 8)
    idxs = [np.nonzero(gmf[:, e] != 0)[0] for e in range(8)]
    ntile = max(1, max((len(ix) + NT - 1) // NT for ix in idxs))
    n = ntile * NT
    nc = build_expert(ntile)
    maps = []
    for e in range(8):
        ix = idxs[e]
        xe = np.zeros((D, n), np.float32)
        xe[:, :len(ix)] = hff[ix].T
        ge = np.zeros((1, n), np.float32)
        ge[0, :len(ix)] = gmf[ix, e]
        maps.append({"xT": xe, "grow": ge, "w1": np.ascontiguousarray(inp["m_w1"][fi][e], dtype=np.float32),
                     "w3": np.ascontiguousarray(inp["m_w3"][fi][e], dtype=np.float32),
                     "w2": np.ascontiguousarray(inp["m_w2"][fi][e], dtype=np.float32)})
    res = _run(nc, maps)
    ya = np.zeros_like(hff)
    yb = np.zeros_like(hff)
    filled = np.zeros((hff.shape[0],), np.int32)
    for e in range(8):
        ix = idxs[e]
        ye = res[e]["yT"][:, :len(ix)].T
        first = filled[ix] == 0
        ya[ix[first]] = ye[first]
        yb[ix[~first]] = ye[~first]
        filled[ix] += 1
    return ya.reshape(hf.shape), yb.reshape(hf.shape)


def build_combine(head_cols=0):
    nc = bass.Bass("TRN2", target_bir_lowering=False)
    dr = lambda name, shape: nc.dram_tensor(name, list(shape), F32, kind="ExternalInput").ap()
    x1T, yaT, ybT = dr("x1T", [D, TC]), dr("yaT", [D, TC]), dr("ybT", [D, TC])
    modc_d = dr("modc", [128, 6 * KC])
    ln2_g, ln2_b = dr("ln2_g", [128, KC]), dr("ln2_b", [128, KC])
    outT = nc.dram_tensor("outT", [D, TC], F32, kind="ExternalOutput").ap()
    if head_cols:
        nm = (head_cols + 127) // 128
        ncp = nm * 128
        modn_d = dr("modn", [128, 6 * KC])
        hw = dr("hw", [D, ncp])
        hb_d = dr("hb", [128, nm])
        zT = nc.dram_tensor("zT", [ncp, TC], F32, kind="ExternalOutput").ap()
    with ExitStack() as es:
        P = Prog(nc, es)
        C = Ctx(P, nslab=1, slab_elems=64) if not head_cols else Ctx(P)
        sh1, sc1, g1, sh2, sc2, g2 = setup_common(P, C, modc_d)
        if head_cols:
            modn = P.sb([128, 6 * KC], F32, "modn_sb")
            P.dma("sp", modn, modn.ap[:], None, modn_d)
            nsc1p = P.sb([128, KC], F32, "nsc1p")
            P.add("dve", lambda e: e.tensor_scalar(nsc1p.ap[:], modn.ap[:, KC:2 * KC], 1.0, None, ALU.add), [modn], [nsc1p])
            c_hb = load_cols(P, "c_hb", hb_d, nm)
            HB = [P.sb([128, NT], BF16, "HB%d" % k) for k in range(KC)]
            O = [P.sb([128, NT], F32, "O%d" % k) for k in range(4)]
            oi = [0]
        c_g, c_b = load_cols(P, "c_ln2g", ln2_g, KC), load_cols(P, "c_ln2b", ln2_b, KC)
        X = [P.sb([128, NT], F32, "X%d" % k) for k in range(KC)]
        YA = [P.sb([128, NT], F32, "YA%d" % k) for k in range(KC)]
        YB = [P.sb([128, NT], F32, "YB%d" % k) for k in range(KC)]
        OUT = Tl(None, "out")
        for it in range(TC // NT):
            sl = slice(it * NT, (it + 1) * NT)
            for k in range(KC):
                P.dma("sp", X[k], X[k].ap[:], None, x1T[k * 128:(k + 1) * 128, sl])
                P.dma("sp", YA[k], YA[k].ap[:], None, yaT[k * 128:(k + 1) * 128, sl])
                P.dma("sp", YB[k], YB[k].ap[:], None, ybT[k * 128:(k + 1) * 128, sl])
                P.add("pool", (lambda e, o=YA[k].ap[:], b_=YB[k].ap[:]: e.tensor_tensor(o, o, b_, ALU.add)), [YA[k], YB[k]], [YA[k]])
                P.add("act", (lambda e, o=YA[k].ap[:], k=k: e.activation(o, o, AF.Copy, scale=g2.ap[:, k:k + 1])), [YA[k], g2], [YA[k]])
                P.add("dve", (lambda e, o=X[k].ap[:], b_=YA[k].ap[:]: e.scalar_tensor_tensor(o, o, ALPHA, b_, ALU.mult, ALU.add)), [X[k], YA[k]], [X[k]])
            ln_inplace(C, X, NT, c_g, c_b)
            for k in range(KC):
                P.dma("sp", OUT, outT[k * 128:(k + 1) * 128, sl], X[k], X[k].ap[:])
            if head_cols:
                for k in range(KC):
                    P.add("act", (lambda e, o=HB[k].ap[:], i=X[k].ap[:], k=k:
                                  e.activation(o, i, AF.Identity, bias=modn.ap[:, k:k + 1], scale=nsc1p.ap[:, k:k + 1])), [X[k], modn, nsc1p], [HB[k]])

                def epi(mi, gi, ps, it=it):
                    o = O[oi[0] % 4]
                    oi[0] += 1
                    P.add("act", (lambda e, o_=o.ap[:], i=ps.ap[:]: e.activation(o_, i, AF.Identity, bias=c_hb.ap[:, mi:mi + 1])), [ps, c_hb], [o])
                    P.dma("sp", OUT, zT[mi * 128:(mi + 1) * 128, it * NT:(it + 1) * NT], o, o.ap[:])
                full = (ncp // 512) * 512
                if full:
                    linear(C, hw, D, 0, full, 512, [(HB, lambda k: HB[k].ap[:])], epi)
                if ncp - full:
                    linear(C, hw, D, full, ncp - full, ncp - full, [(HB, lambda k: HB[k].ap[:])], epi)
        P.finish([OUT])
        P.emit_all()
    return nc


def run_combine(inp, layer, x1, ya, yb, head=None):
    head_cols = 0
    if head is not None:
        nlayer, w, bvec = head
        head_cols = w.shape[1]
        nm = (head_cols + 127) // 128
        wp = np.zeros((D, nm * 128), np.float32)
        wp[:, :head_cols] = w
        bp = np.zeros((nm * 128,), np.float32)
        bp[:head_cols] = bvec
    nc = build_combine(head_cols)
    maps = []
    for core in range(NCORE):
        m = common_maps(inp, layer, core)
        m.update({"x1T": tok_T(x1, core), "yaT": tok_T(ya, core), "ybT": tok_T(yb, core),
                  "ln2_g": cols(inp["ln2_g"][layer]), "ln2_b": cols(inp["ln2_b"][layer])})
        if head is not None:
            m.update({"modn": cols(inp["_mod"][nlayer][core // 2]), "hw": wp, "hb": cols(bp)})
        maps.append({k: np.ascontiguousarray(v, dtype=np.float32) for k, v in m.items()})
    res = _run(nc, maps)
    if head is not None:
        return from_T(res, "outT", D), from_T(res, "zT", head_cols)
    return from_T(res, "outT", D)
```
